# Optimizing a Trainium2 kernel written in Bass

```python
import jax, jax.numpy as jnp
from jax import lax
import numpy as np

D_MODEL = 1024
BATCH = 16
SEQ = 2048
DEPTH = 2

HEAD_DIM = 64
N_HEADS = D_MODEL // HEAD_DIM
FOX_HEADS = N_HEADS // 2
SWA_HEADS = N_HEADS - FOX_HEADS
SWA_KV_HEADS = max(1, SWA_HEADS // 4)
SWA_WINDOW = 128
Q_BLOCK = 128
DSA_HEADS = N_HEADS
DSA_KV_HEADS = max(1, DSA_HEADS // 4)
IDX_HEADS = 8
IDX_DIM = 64
DSA_TOPK_MAX = 256
D_FF = 2816
N_EXPERTS = 8
TOP_K = 2
D_FF_EXPERT = 1408
ROPE_THETA = 10000.0
NORM_EPS = 1e-6
N_EVEN = (DEPTH + 1) // 2
N_ODD = DEPTH // 2

EVEN_SPLITS = (FOX_HEADS * HEAD_DIM, FOX_HEADS * HEAD_DIM, FOX_HEADS * HEAD_DIM, FOX_HEADS,
               SWA_HEADS * HEAD_DIM, SWA_KV_HEADS * HEAD_DIM, SWA_KV_HEADS * HEAD_DIM)
EVEN_IN = sum(EVEN_SPLITS)
ODD_SPLITS = (DSA_HEADS * HEAD_DIM, DSA_KV_HEADS * HEAD_DIM, DSA_KV_HEADS * HEAD_DIM,
              IDX_HEADS * IDX_DIM, IDX_DIM, IDX_HEADS)
ODD_IN = sum(ODD_SPLITS)

kernel_name = 'hybrid_fox_swa_dsa_moe'


def _split(t, sizes):
    return jnp.split(t, np.cumsum(sizes)[:-1].tolist(), axis=-1)


def rms_norm(x, g):
    xf = x.astype(jnp.float32)
    y = xf * lax.rsqrt(jnp.mean(xf * xf, axis=-1, keepdims=True) + NORM_EPS)
    return (y * g.astype(jnp.float32)).astype(x.dtype)


def ada_modulation(c, w, b):
    mod = jax.nn.silu(c) @ w + b
    return jnp.split(mod[:, None, :], 6, axis=-1)


def rope_tables(positions, dim):
    half = dim // 2
    inv_freq = ROPE_THETA ** (-jnp.arange(half, dtype=jnp.float32) / half)
    ang = positions.astype(jnp.float32)[..., None] * inv_freq
    return jnp.cos(ang)[:, :, None, :], jnp.sin(ang)[:, :, None, :]


def apply_rope(t, cos, sin):
    half = t.shape[-1] // 2
    tf = t.astype(jnp.float32)
    t1, t2 = tf[..., :half], tf[..., half:]
    return jnp.concatenate([t1 * cos - t2 * sin, t2 * cos + t1 * sin], axis=-1).astype(t.dtype)


def swiglu(h, w_gate, w_up, w_down):
    return (jax.nn.silu(h @ w_gate) * (h @ w_up)) @ w_down


def forgetting_attention(q, k, v, log_f):
    B, S, H, D = q.shape
    cum = jnp.cumsum(log_f.astype(jnp.float32), axis=1).transpose(0, 2, 1)
    kpos = jnp.arange(S)
    scale = D ** -0.5

    def block(i):
        start = i * Q_BLOCK
        qi = lax.dynamic_slice_in_dim(q, start, Q_BLOCK, axis=1)
        ci = lax.dynamic_slice_in_dim(cum, start, Q_BLOCK, axis=2)
        s = jnp.einsum('bqhd,bkhd->bhqk', qi, k, preferred_element_type=jnp.float32) * scale
        s = s + ci[..., :, None] - cum[..., None, :]
        qpos = start + jnp.arange(Q_BLOCK)
        s = jnp.where(kpos[None, :] <= qpos[:, None], s, -jnp.inf)
        p = jax.nn.softmax(s, axis=-1)
        return jnp.einsum('bhqk,bkhd->bqhd', p.astype(v.dtype), v)

    out = lax.map(block, jnp.arange(S // Q_BLOCK))
    return jnp.moveaxis(out, 0, 1).reshape(B, S, H, D)


def sliding_window_sink_attention(q, k, v, sinks):
    B, S, H, D = q.shape
    KVH = k.shape[2]
    G = H // KVH
    W = SWA_WINDOW
    nb = S // W
    qb = q.reshape(B, nb, W, KVH, G, D)

    def band_keys(t):
        tb = t.reshape(B, nb, W, KVH, D)
        prev = jnp.concatenate([jnp.zeros_like(tb[:, :1]), tb[:, :-1]], axis=1)
        return jnp.concatenate([prev, tb], axis=2)

    kk, vv = band_keys(k), band_keys(v)
    s = jnp.einsum('bnqhgd,bnshd->bnhgqs', qb, kk, preferred_element_type=jnp.float32) * D ** -0.5
    rel = (jnp.arange(W)[:, None] + W) - jnp.arange(2 * W)[None, :]
    band = (rel >= 0) & (rel < W)
    not_pad = (jnp.arange(nb)[:, None, None] > 0) | (jnp.arange(2 * W) >= W)[None, None, :]
    valid = band[None] & not_pad
    s = jnp.where(valid[None, :, None, None], s, -jnp.inf)
    sink = sinks.astype(jnp.float32).reshape(KVH, G)[None, None, :, :, None, None]
    m = jnp.maximum(jnp.max(s, axis=-1, keepdims=True), sink)
    e = jnp.exp(s - m)
    p = e / (jnp.sum(e, axis=-1, keepdims=True) + jnp.exp(sink - m))
    out = jnp.einsum('bnhgqs,bnshd->bnqhgd', p.astype(v.dtype), vv)
    return out.reshape(B, S, H, D)


def dsa_attention(q, k, v, iq, ik, iw):
    B, S, H, D = q.shape
    KVH = k.shape[2]
    G = H // KVH
    topk = min(DSA_TOPK_MAX, S // 4)
    kpos = jnp.arange(S)
    scale = D ** -0.5
    gather = jax.vmap(lambda tb, ib: tb[ib])

    def block(i):
        start = i * Q_BLOCK
        qi = lax.dynamic_slice_in_dim(q, start, Q_BLOCK, axis=1).reshape(B, Q_BLOCK, KVH, G, D)
        iqi = lax.dynamic_slice_in_dim(iq, start, Q_BLOCK, axis=1)
        iwi = lax.dynamic_slice_in_dim(iw, start, Q_BLOCK, axis=1).astype(jnp.float32)
        qpos = start + jnp.arange(Q_BLOCK)
        logits = jnp.einsum('bqhd,bsd->bqhs', iqi, ik, preferred_element_type=jnp.float32)
        score = jnp.einsum('bqh,bqhs->bqs', iwi, jax.nn.relu(logits))
        score = jnp.where((kpos[None, :] <= qpos[:, None])[None], score, -jnp.inf)
        _, idx = lax.top_k(score, topk)
        sel_valid = idx <= qpos[None, :, None]
        ks = gather(k, idx)
        vs = gather(v, idx)
        s = jnp.einsum('bqhgd,bqthd->bhgqt', qi, ks, preferred_element_type=jnp.float32) * scale
        s = jnp.where(sel_valid[:, None, None], s, -jnp.inf)
        p = jax.nn.softmax(s, axis=-1)
        o = jnp.einsum('bhgqt,bqthd->bqhgd', p.astype(v.dtype), vs)
        return o.reshape(B, Q_BLOCK, H, D)

    out = lax.map(block, jnp.arange(S // Q_BLOCK))
    return jnp.moveaxis(out, 0, 1).reshape(B, S, H, D)


def moe_swiglu(h, router_w, router_b, w_gate, w_up, w_down):
    B, S, Dm = h.shape
    t = h.reshape(B * S, Dm)
    logits = (t @ router_w).astype(jnp.float32) + router_b.astype(jnp.float32)
    top_vals, top_idx = lax.top_k(logits, TOP_K)
    gates = jax.nn.softmax(top_vals, axis=-1)
    combine = jnp.sum(jax.nn.one_hot(top_idx, N_EXPERTS, dtype=jnp.float32) * gates[..., None], axis=1)
    combine = combine.astype(h.dtype)
    out = jnp.zeros_like(t)
    for e in range(N_EXPERTS):
        out = out + combine[:, e:e + 1] * swiglu(t, w_gate[e], w_up[e], w_down[e])
    return out.reshape(B, S, Dm)


def even_layer(x, c, cos, sin, ada_w, ada_b, norm_mix, norm_ffn, w_in, forget_b, sinks, w_out,
               ffn_gate, ffn_up, ffn_down):
    B, S, _ = x.shape
    sh_m, sc_m, g_m, sh_f, sc_f, g_f = ada_modulation(c, ada_w, ada_b)
    h = rms_norm(x, norm_mix) * (1 + sc_m) + sh_m
    fq, fk, fv, fgl, sq, sk, sv = _split(h @ w_in, EVEN_SPLITS)
    heads = lambda t, n: t.reshape(B, S, n, HEAD_DIM)
    log_f = jax.nn.log_sigmoid((fgl + forget_b).astype(jnp.float32))
    o_fox = forgetting_attention(heads(fq, FOX_HEADS), heads(fk, FOX_HEADS), heads(fv, FOX_HEADS), log_f)
    o_swa = sliding_window_sink_attention(apply_rope(heads(sq, SWA_HEADS), cos, sin),
                                          apply_rope(heads(sk, SWA_KV_HEADS), cos, sin),
                                          heads(sv, SWA_KV_HEADS), sinks)
    mix = jnp.concatenate([o_fox.reshape(B, S, -1), o_swa.reshape(B, S, -1)], axis=-1) @ w_out
    x = x + g_m * mix
    h = rms_norm(x, norm_ffn) * (1 + sc_f) + sh_f
    return x + g_f * swiglu(h, ffn_gate, ffn_up, ffn_down)


def odd_layer(x, c, cos, sin, ada_w, ada_b, norm_mix, norm_ffn, w_in, w_out,
              router_w, router_b, exp_gate, exp_up, exp_down):
    B, S, _ = x.shape
    sh_m, sc_m, g_m, sh_f, sc_f, g_f = ada_modulation(c, ada_w, ada_b)
    h = rms_norm(x, norm_mix) * (1 + sc_m) + sh_m
    q, k, v, iq, ik, iw = _split(h @ w_in, ODD_SPLITS)
    q = apply_rope(q.reshape(B, S, DSA_HEADS, HEAD_DIM), cos, sin)
    k = apply_rope(k.reshape(B, S, DSA_KV_HEADS, HEAD_DIM), cos, sin)
    v = v.reshape(B, S, DSA_KV_HEADS, HEAD_DIM)
    iq = apply_rope(iq.reshape(B, S, IDX_HEADS, IDX_DIM), cos, sin)
    ik = apply_rope(ik.reshape(B, S, 1, IDX_DIM), cos, sin)[:, :, 0]
    iw = iw * (IDX_HEADS ** -0.5 * IDX_DIM ** -0.5)
    o = dsa_attention(q, k, v, iq, ik, iw)
    x = x + g_m * (o.reshape(B, S, -1) @ w_out)
    h = rms_norm(x, norm_ffn) * (1 + sc_f) + sh_f
    return x + g_f * moe_swiglu(h, router_w, router_b, exp_gate, exp_up, exp_down)


def setup_inputs(seed: int = 0) -> dict:
    key = jax.random.key(seed)
    ks = iter(jax.random.split(key, 40))
    D = D_MODEL

    def nrm(shape, s):
        return jax.random.normal(next(ks), shape, jnp.float32) * s

    x = nrm((BATCH, SEQ, D), 1.0)
    c = nrm((BATCH, D), 1.0)
    offset = jax.random.randint(next(ks), (BATCH, 1), 0, 4096, dtype=jnp.int32)
    positions = offset + jnp.arange(SEQ, dtype=jnp.int32)[None, :]
    return {
        'x': x, 'c': c, 'positions': positions,
        'e_ada_w': nrm((N_EVEN, D, 6 * D), 0.5 * D ** -0.5),
        'e_ada_b': nrm((N_EVEN, 6 * D), 0.02),
        'e_norm_mix': 1.0 + nrm((N_EVEN, D), 0.05),
        'e_norm_ffn': 1.0 + nrm((N_EVEN, D), 0.05),
        'e_w_in': nrm((N_EVEN, D, EVEN_IN), D ** -0.5),
        'e_forget_b': 3.0 + nrm((N_EVEN, FOX_HEADS), 1.0),
        'e_sinks': nrm((N_EVEN, SWA_HEADS), 1.0),
        'e_w_out': nrm((N_EVEN, D, D), D ** -0.5),
        'e_ffn_gate': nrm((N_EVEN, D, D_FF), D ** -0.5),
        'e_ffn_up': nrm((N_EVEN, D, D_FF), D ** -0.5),
        'e_ffn_down': nrm((N_EVEN, D_FF, D), D_FF ** -0.5),
        'o_ada_w': nrm((N_ODD, D, 6 * D), 0.5 * D ** -0.5),
        'o_ada_b': nrm((N_ODD, 6 * D), 0.02),
        'o_norm_mix': 1.0 + nrm((N_ODD, D), 0.05),
        'o_norm_ffn': 1.0 + nrm((N_ODD, D), 0.05),
        'o_w_in': nrm((N_ODD, D, ODD_IN), D ** -0.5),
        'o_w_out': nrm((N_ODD, D, D), D ** -0.5),
        'o_router_w': nrm((N_ODD, D, N_EXPERTS), D ** -0.5),
        'o_router_b': nrm((N_ODD, N_EXPERTS), 0.01),
        'o_exp_gate': nrm((N_ODD, N_EXPERTS, D, D_FF_EXPERT), D ** -0.5),
        'o_exp_up': nrm((N_ODD, N_EXPERTS, D, D_FF_EXPERT), D ** -0.5),
        'o_exp_down': nrm((N_ODD, N_EXPERTS, D_FF_EXPERT, D), D_FF_EXPERT ** -0.5),
        'final_norm': 1.0 + nrm((D,), 0.05),
    }


def reference(x, c, positions,
              e_ada_w, e_ada_b, e_norm_mix, e_norm_ffn, e_w_in, e_forget_b, e_sinks, e_w_out,
              e_ffn_gate, e_ffn_up, e_ffn_down,
              o_ada_w, o_ada_b, o_norm_mix, o_norm_ffn, o_w_in, o_w_out,
              o_router_w, o_router_b, o_exp_gate, o_exp_up, o_exp_down,
              final_norm):
    cos, sin = rope_tables(positions, HEAD_DIM)
    for layer in range(DEPTH):
        i = layer // 2
        if layer % 2 == 0:
            x = even_layer(x, c, cos, sin, e_ada_w[i], e_ada_b[i], e_norm_mix[i], e_norm_ffn[i],
                           e_w_in[i], e_forget_b[i], e_sinks[i], e_w_out[i],
                           e_ffn_gate[i], e_ffn_up[i], e_ffn_down[i])
        else:
            x = odd_layer(x, c, cos, sin, o_ada_w[i], o_ada_b[i], o_norm_mix[i], o_norm_ffn[i],
                          o_w_in[i], o_w_out[i], o_router_w[i], o_router_b[i],
                          o_exp_gate[i], o_exp_up[i], o_exp_down[i])
    return rms_norm(x, final_norm)
```

```python
import math
from contextlib import ExitStack

import numpy as np
import concourse.bass as bass
import concourse.mybir as mybir
from concourse.bass_utils import run_bass_kernel_spmd

F32 = mybir.dt.float32
BF16 = mybir.dt.bfloat16
I32 = mybir.dt.int32
ALU = mybir.AluOpType
AF = mybir.ActivationFunctionType
AX = mybir.AxisListType

CELL = 512
PSUM_BANK = 2048


class Buf:
    def __init__(self, space, off, nbytes, ap):
        self.space = space
        self.off = off
        self.nbytes = nbytes
        self.ap = ap

    def cells(self, lo=None, hi=None):
        lo = self.off if lo is None else self.off + lo
        hi = self.off + self.nbytes if hi is None else self.off + hi
        g = CELL if self.space != 'psum' else PSUM_BANK
        return [(self.space, c) for c in range(lo // g, (hi - 1) // g + 1)]


class Sched:
    COMPUTE = ('pe', 'act', 'dve', 'pool')
    QUEUES = ('pe', 'act', 'dve', 'pool', 'sp')

    def __init__(self, nc):
        self.nc = nc
        self.ops = {e: [] for e in self.QUEUES}
        self.cnt = {e: 0 for e in self.COMPUTE}
        self.waited = {e: {} for e in self.QUEUES}
        self.cell = {}
        self.dma_slots = {'sp': 8, 'pool': 2, 'act': 4}
        self.dma_rr = {q: 0 for q in self.dma_slots}
        self.dma_val = {}
        self.sems = {}
        self.nops = 0

    def sem_keys(self):
        keys = list(self.COMPUTE)
        for q, n in self.dma_slots.items():
            keys += [('dma', q, i) for i in range(n)]
        return keys

    def _deps(self, eng, reads, writes):
        deps = {}

        def add(tok):
            if tok is None:
                return
            k, v = tok
            if eng == 'pe' and k == 'pe':
                return
            if deps.get(k, 0) < v:
                deps[k] = v

        for c in reads:
            st = self.cell.get(c)
            if st:
                add(st[0])
        for c in writes:
            st = self.cell.get(c)
            if st:
                add(st[0])
                for k, v in st[1].items():
                    add((k, v))
        out = []
        w = self.waited[eng]
        for k, v in deps.items():
            if w.get(k, 0) < v:
                w[k] = v
                out.append((k, v))
        return out

    def _commit(self, tok, reads, writes):
        k, v = tok
        for c in reads:
            st = self.cell.setdefault(c, [None, {}])
            if st[1].get(k, 0) < v:
                st[1][k] = v
        for c in writes:
            self.cell[c] = [tok, {}]

    @staticmethod
    def _cells(lst):
        out = []
        for b in lst:
            if isinstance(b, Buf):
                out += b.cells()
            elif isinstance(b, tuple) and isinstance(b[0], Buf):
                out += b[0].cells(b[1], b[2])
            else:
                out.append(b)
        return out

    def op(self, eng, fn, reads=(), writes=()):
        reads = self._cells(reads)
        writes = self._cells(writes)
        writes = writes + [c for c in reads if c[0] == 'psum' and c not in writes]
        waits = self._deps(eng, reads, writes)
        self.cnt[eng] += 1
        tok = (eng, self.cnt[eng])
        self.ops[eng].append((fn, waits, (eng, 1)))
        self._commit(tok, reads, writes)
        self.nops += 1
        return tok

    def dma(self, q, fn, reads=(), writes=()):
        reads = self._cells(reads)
        writes = self._cells(writes)
        slot = ('dma', q, self.dma_rr[q] % self.dma_slots[q])
        self.dma_rr[q] += 1
        waits = self._deps(q, reads, writes)
        prev = self.dma_val.get(slot, 0)
        if prev and self.waited[q].get(slot, 0) < prev:
            self.waited[q][slot] = prev
            waits.append((slot, prev))
        val = prev + 16
        self.dma_val[slot] = val
        tok = (slot, val)
        self.ops[q].append((fn, waits, (slot, 16)))
        self._commit(tok, reads, writes)
        self.nops += 1
        return tok

    def wait_all(self, q='sp'):
        waits = []
        for e in self.COMPUTE:
            if self.cnt[e] and self.waited[q].get(e, 0) < self.cnt[e]:
                waits.append((e, self.cnt[e]))
        for slot, v in self.dma_val.items():
            if self.waited[q].get(slot, 0) < v:
                waits.append((slot, v))
        self.ops[q].append((None, waits, None))

    def emit(self, block):
        sems = self.sems

        def run(engname):
            def body(engine):
                for fn, waits, inc in self.ops[engname]:
                    for k, v in waits:
                        engine.wait_ge(sems[k], v)
                    if fn is None:
                        continue
                    ins = fn(engine)
                    ins.then_inc(sems[inc[0]], inc[1])
            return body

        block.tensor(run('pe'))
        block.scalar(run('act'))
        block.vector(run('dve'))
        block.gpsimd(run('pool'))
        block.sync(run('sp'))


class Arena:
    def __init__(self, space, base_ap, nbytes):
        self.space = space
        self.base = base_ap
        self.nbytes = nbytes
        self.top = 0
        self.marks = []

    def alloc(self, shape, dtype, align=CELL):
        esz = 2 if dtype == BF16 else 4
        n = 1
        for s in shape:
            n *= s
        nb = n * esz
        off = (self.top + align - 1) // align * align
        assert off + nb <= self.nbytes, f"{self.space} arena overflow: need {off + nb} > {self.nbytes}"
        self.top = off + nb
        ap = self.base[:, off // 4:(off + nb + 3) // 4]
        if dtype != F32:
            ap = ap.bitcast(dtype)
            ap = ap[:, 0:n]
        if len(shape) > 1:
            names = ' '.join(f'a{i}' for i in range(len(shape)))
            kw = {f'a{i}': s for i, s in enumerate(shape[1:], start=1)}
            ap = ap.rearrange(f'p ({names}) -> p {names}', **kw)
        return Buf(self.space, off, nb, ap)

    def mark(self):
        self.marks.append(self.top)

    def release(self):
        self.top = self.marks.pop()


def _esz(dt):
    return 2 if dt == BF16 else 4


def bsub(b, i):
    shp = b.ap.shape
    n = 1
    for s in shp[2:]:
        n *= s
    rb = n * _esz(b.ap.dtype)
    return Buf(b.space, b.off + i * rb, rb, b.ap[:, i])


def bcols(b, lo, hi):
    shp = b.ap.shape
    n = 1
    for s in shp[2:]:
        n *= s
    rb = n * _esz(b.ap.dtype)
    return Buf(b.space, b.off + lo * rb, (hi - lo) * rb, b.ap[:, lo:hi])


D = 1024
SEQ = 2048
NT = 16
HD = 64
DFF = 2816
NEXP = 8
DFE = 1408
NEG = -30000.0


class K:
    def __init__(self, nc, S, A, P, dr, nseq):
        self.nc, self.S, self.A, self.P, self.dr, self.nseq = nc, S, A, P, dr, nseq

    def mm(self, out, lhsT, rhs, start, stop, R, W):
        self.S.op('pe', lambda e: e.matmul(out, lhsT=lhsT, rhs=rhs, start=start, stop=stop,
                                           skip_group_check=True), R, W)

    def tr(self, out, in_, ident, R, W):
        self.S.op('pe', lambda e: e.transpose(out=out, in_=in_, identity=ident), R, W)

    def act(self, out, in_, func, R, W, bias=None, scale=None, accum=None):
        kw = {}
        if bias is not None:
            kw['bias'] = bias
        if scale is not None:
            kw['scale'] = scale
        if accum is not None:
            kw['accum_out'] = accum
        self.S.op('act', lambda e: e.activation(out=out, in_=in_, func=func, **kw), R, W)

    def ts(self, eng, out, in0, s1, s2, op0, op1, R, W):
        if op1 is None:
            self.S.op(eng, lambda e: e.tensor_scalar(out=out, in0=in0, scalar1=s1, scalar2=None, op0=op0), R, W)
        else:
            self.S.op(eng, lambda e: e.tensor_scalar(out=out, in0=in0, scalar1=s1, scalar2=s2, op0=op0, op1=op1), R, W)

    def tt(self, eng, out, in0, in1, op, R, W):
        self.S.op(eng, lambda e: e.tensor_tensor(out=out, in0=in0, in1=in1, op=op), R, W)

    def stt(self, out, in0, scalar, in1, op0, op1, R, W):
        self.S.op('dve', lambda e: e.scalar_tensor_tensor(out=out, in0=in0, scalar=scalar, in1=in1, op0=op0, op1=op1), R, W)

    def cp(self, eng, out, in_, R, W):
        if eng == 'act':
            self.S.op('act', lambda e: e.copy(out=out, in_=in_), R, W)
        else:
            self.S.op(eng, lambda e: e.tensor_copy(out=out, in_=in_), R, W)

    def memset(self, eng, ap, val, W):
        self.S.op(eng, lambda e: e.memset(ap, val), (), W)

    def dma(self, q, out, in_, R, W):
        self.S.dma(q, lambda e: e.dma_start(out=out, in_=in_), R, W)

    def setup(self):
        A, dr = self.A, self.dr
        self.X = A.alloc([NT, D], F32)
        self.identb = A.alloc([128], BF16)
        self.constf = A.alloc([4, 128], F32)
        self.constb = A.alloc([3, 128], BF16)
        self.small = A.alloc([64], F32)
        self.normw = A.alloc([5, 8], F32)
        self.adab = A.alloc([2, 48], F32)
        self.bc8 = A.alloc([3, 8], F32)
        self.dma('sp', self.constf.ap, dr['constf'], (), [self.constf])
        self.dma('sp', self.small.ap, dr['small'], (), [self.small])
        self.dma('sp', self.normw.ap, dr['normw'], (), [self.normw])
        self.dma('sp', self.adab.ap, dr['adab'], (), [self.adab])
        self.dma('sp', self.bc8.ap, dr['bc8'].partition_broadcast(128), (), [self.bc8])
        self.dma('pool', self.constb.ap, dr['constb'], (), [self.constb])
        self.cp('dve', self.identb.ap, self.constf.ap[:, 0], [self.constf], [self.identb])
        self.esink = A.alloc([8], F32)
        self.negone = A.alloc([8], F32, align=64)
        self.memset('pool', self.negone.ap, -1.0, [self.negone])
        self.act(self.esink.ap, self.bc8.ap[:, 1], AF.Exp, [self.bc8], [self.esink])
        self.modT = A.alloc([self.nseq, 2, 6, 8], F32)
        self.gate = A.alloc([1, D], F32)
        self.cT = A.alloc([8, self.nseq], F32)
        self.scb = A.alloc([8, self.nseq], BF16)
        self.Dg = [A.alloc([128], F32) for _ in range(2)]
        self.stat = A.alloc([2, NT], F32)
        self.cosT = None

    def ada(self, l, part):
        A, dr = self.A, self.dr
        ns = self.nseq
        A.mark()
        if l == 0 and part == 0:
            self.dma('sp', self.cT.ap, dr['cT'], (), [self.cT])
            sg = A.alloc([8, ns], F32)
            self.act(sg.ap, self.cT.ap, AF.Silu, [self.cT], [sg])
            self.cp('dve', self.scb.ap, sg.ap, [sg], [self.scb])
        wst = [A.alloc([8, 8, 128], BF16) for _ in range(2)]
        ps1 = self.pj[0]
        for m in range(3 * part, 3 * part + 3):
            w = wst[m % 2]
            self.dma('pool', w.ap, dr['ada_w'][l, m], (), [w])
            for j in range(8):
                for kc in range(8):
                    self.mm(ps1.ap[:, j * ns:(j + 1) * ns], w.ap[:, j, kc], self.scb.ap[:, kc], kc == 0, kc == 7,
                            [w, self.scb], [ps1])
            pv = ps1.ap[:, 0:8 * ns].rearrange('p (j q) -> p q j', q=ns)
            for q in range(ns):
                self.tt('dve', self.modT.ap[:, q, l, m], pv[:, q], self.adab.ap[:, l, m * 8:(m + 1) * 8], ALU.add,
                        [ps1, self.adab], [self.modT])
                if m in (1, 4):
                    nw = self.normw.ap[:, 2 * l + (0 if m == 1 else 1)]
                    self.stt(self.modT.ap[:, q, l, m], self.modT.ap[:, q, l, m], 1.0, nw, ALU.add, ALU.mult,
                             [self.modT, self.normw], [self.modT])
        A.release()

    def expand_gate(self, s, l, m):
        ones = self.constf.ap[:, 2]
        identf = self.constf.ap[:, 0]
        for j in range(8):
            dg = self.Dg[j % 2]
            self.ts('dve', dg.ap, identf, self.modT.ap[:, s, l, m, j:j + 1], None, ALU.mult, None, [self.constf, self.modT], [dg])
            pb = self.pj[j // 4]
            self.mm(pb.ap[:, (j % 4) * 128:(j % 4 + 1) * 128], ones, dg.ap, True, True, [self.constf, dg], [pb])
        for hf in range(2):
            self.cp('act', self.gate.ap[:, 0, hf * 512:(hf + 1) * 512], self.pj[hf].ap, [self.pj[hf]], [self.gate])

    def rstd(self, t0, n, sq):
        X = self.X
        st = self.stat
        for t in range(t0, t0 + n):
            self.act(sq.ap, X.ap[:, t], AF.Square, [bsub(X, t)], [sq, st], accum=st.ap[:, 0, t:t + 1])
        self.ts('dve', st.ap[:, 1, t0:t0 + n], st.ap[:, 0, t0:t0 + n], 1.0 / D, 1e-6, ALU.mult, ALU.add, [st], [st])
        self.act(st.ap[:, 1, t0:t0 + n], st.ap[:, 1, t0:t0 + n], AF.Sqrt, [st], [st])
        self.S.op('dve', lambda e: e.reciprocal(out=st.ap[:, 1, t0:t0 + n], in_=st.ap[:, 1, t0:t0 + n]),
                  self.S._cells([st]), self.S._cells([st]))

    def norm_hT2(self, l, which, hT):
        A, P = self.A, self.P
        A.mark()
        sq = A.alloc([D], F32)
        xn = A.alloc([4, D], BF16)
        pt = self.ptb
        X = self.X
        msh = self.modT.ap[:, self.cur_s, l, 0 if which == 0 else 3]
        msc = self.modT.ap[:, self.cur_s, l, 1 if which == 0 else 4]
        for c in range(4):
            self.rstd(c * 4, 4, sq)
            for tl in range(4):
                t = c * 4 + tl
                self.ts('dve', xn.ap[:, tl], X.ap[:, t], self.stat.ap[:, 1, t:t + 1], None, ALU.mult, None,
                        [bsub(X, t), self.stat], [bsub(xn, tl)])
            for kc in range(8):
                pb = pt[kc % 2]
                for tl in range(4):
                    self.tr(pb.ap[:, tl * 128:(tl + 1) * 128], xn.ap[:, tl, kc * 128:(kc + 1) * 128], self.identb.ap,
                            [bsub(xn, tl), self.identb], [pb])
                if kc % 2 == 0:
                    self.act(hT.ap[:, kc, c * 512:(c + 1) * 512], pb.ap, AF.Identity, [pb, self.modT],
                             [(hT, (kc * SEQ + c * 512) * 2, (kc * SEQ + c * 512 + 512) * 2)],
                             bias=msh[:, kc:kc + 1], scale=msc[:, kc:kc + 1])
                else:
                    self.ts('dve', hT.ap[:, kc, c * 512:(c + 1) * 512], pb.ap, msc[:, kc:kc + 1], msh[:, kc:kc + 1], ALU.mult, ALU.add,
                            [pb, self.modT], [(hT, (kc * SEQ + c * 512) * 2, (kc * SEQ + c * 512 + 512) * 2)])
        A.release()

    def rope_tables(self, s, cosT, sinT):
        A = self.A
        A.mark()
        HS = SEQ // 2
        pi_ = A.alloc([HS], F32)
        ang = A.alloc([HS], F32)
        kf = A.alloc([HS], F32)
        pii = pi_.ap.bitcast(I32)
        sm = self.small
        C1 = 6.28125
        C2 = 2 * math.pi - C1

        def wrap(buf):
            self.ts('dve', kf.ap, buf.ap, math.pi, -2 * math.pi, ALU.is_gt, ALU.mult, [buf], [kf])
            self.tt('dve', buf.ap, buf.ap, kf.ap, ALU.add, [buf, kf], [buf])
            self.ts('dve', kf.ap, buf.ap, -math.pi, 2 * math.pi, ALU.is_lt, ALU.mult, [buf], [kf])
            self.tt('dve', buf.ap, buf.ap, kf.ap, ALU.add, [buf, kf], [buf])

        for hb in range(2):
            sl = slice(hb * HS, (hb + 1) * HS)
            self.dma('sp', pii, self.dr['pos'][s][:, sl].partition_broadcast(128), (), [pi_])
            self.cp('dve', ang.ap, pii, [pi_], [ang])
            self.ts('dve', ang.ap, ang.ap, sm.ap[:, 0:1], None, ALU.mult, None, [ang, sm], [ang])
            self.ts('dve', kf.ap, ang.ap, 1.0 / (2 * math.pi), None, ALU.mult, None, [ang], [kf])
            self.cp('dve', pii, kf.ap, [kf], [pi_])
            self.cp('dve', kf.ap, pii, [pi_], [kf])
            self.stt(ang.ap, kf.ap, -C1, ang.ap, ALU.mult, ALU.add, [kf, ang], [ang])
            self.stt(ang.ap, kf.ap, -C2, ang.ap, ALU.mult, ALU.add, [kf, ang], [ang])
            wrap(ang)
            self.act(sinT.ap[:, sl], ang.ap, AF.Sin, [ang, sm], [sinT], scale=sm.ap[:, 1:2])
            self.ts('dve', ang.ap, ang.ap, math.pi / 2, None, ALU.add, None, [ang], [ang])
            wrap(ang)
            self.act(cosT.ap[:, sl], ang.ap, AF.Sin, [ang], [cosT])
        A.release()

    @staticmethod
    def hT_rng(hT, t0, t1):
        return [(hT, (kc * SEQ + t0) * 2, (kc * SEQ + t1) * 2) for kc in range(8)]

    def proj_fm(self, hT, wsrc, specs, pbanks):
        A = self.A
        A.mark()
        wst = [A.alloc([2, 8, 128], BF16) for _ in range(2)]
        t1 = [A.alloc([512], F32) for _ in range(1)]
        t2 = [A.alloc([512], F32) for _ in range(1)]
        n = 0
        for si, (kind, cids, dest, extra) in enumerate(specs):
            w = wst[si % 2]
            for ci, cid in enumerate(cids):
                self.dma('pool', w.ap[:, ci], wsrc[cid], (), [bsub(w, ci)])
            for c in range(4):
                pss = []
                for ci in range(len(cids)):
                    pb = pbanks[n % len(pbanks)]
                    n += 1
                    for kc in range(8):
                        self.mm(pb.ap, w.ap[:, ci, kc], hT.ap[:, kc, c * 512:(c + 1) * 512], kc == 0, kc == 7,
                                [bsub(w, ci)] + self.hT_rng(hT, c * 512, (c + 1) * 512), [pb])
                    pss.append(pb)
                dbuf, dap = dest(c)
                if kind == 'plain':
                    self.act(dap, pss[0].ap, AF.Copy, [pss[0]], [dbuf], scale=float(extra))
                else:
                    cosT, sinT = extra
                    a, b = t1[0], t2[0]
                    self.tt('dve', a.ap, pss[0].ap, cosT.ap[:, c * 512:(c + 1) * 512], ALU.mult, [pss[0], cosT], [a])
                    self.tt('dve', b.ap, pss[1].ap, sinT.ap[:, c * 512:(c + 1) * 512], ALU.mult, [pss[1], sinT], [b])
                    if len(dap.shape) == 3:
                        self.tt('pool', dap, a.ap.rearrange('p (t q) -> p t q', q=128), b.ap.rearrange('p (t q) -> p t q', q=128),
                                ALU.add, [a, b], [dbuf])
                    else:
                        self.tt('pool', dap, a.ap, b.ap, ALU.add, [a, b], [dbuf])
        A.release()

    def proj_tm(self, hT, wsrc, ncols, pbanks, evac):
        A = self.A
        A.mark()
        w = A.alloc([8, ncols], BF16)
        self.dma('pool', w.ap, wsrc, (), [w])
        for t in range(NT):
            pb = pbanks[t % len(pbanks)]
            for kc in range(8):
                self.mm(pb.ap[:, 0:ncols], hT.ap[:, kc, t * 128:(t + 1) * 128], w.ap[:, kc], kc == 0, kc == 7,
                        [w] + self.hT_rng(hT, t * 128, (t + 1) * 128), [pb])
            evac(t, pb)
        A.release()

    def attn_group(self, i, jlist, heads, maskfn, scale, pS, pO, PT, obuf, ocol0, sink=None, osb=None, qbatch=None):
        G = len(heads)
        nJ = len(jlist)

        def scores(jn):
            j = jlist[jn]
            ps = pS[jn % len(pS)]
            m = maskfn(j)
            started = False
            if qbatch is not None:
                out3 = ps.ap[:, 0:G * 128].rearrange('p (g q) -> p g q', q=128)
                self.mm(out3, self.identb.ap, m[1].unsqueeze(1).to_broadcast([128, G, 128]), True, False,
                        [self.identb, m[0]], [ps])
                kb_, ka_ = heads[0]['k']
                self.mm(out3, ka_[:, j * 128:(j + 1) * 128], qbatch[1], False, True, [kb_, qbatch[0]], [ps])
                return
            for hh, h in enumerate(heads):
                cols = ps.ap[:, hh * 128:(hh + 1) * 128]
                if m is not None:
                    self.mm(cols, self.identb.ap, m[1], not started, False, [self.identb, m[0]], [ps])
                    started = True
                qb, qa = h['q']
                kb, ka = h['k']
                self.mm(cols, ka[:, j * 128:(j + 1) * 128], qa[:, i * 128:(i + 1) * 128], not started,
                        h['bias'] is None, [kb, qb], [ps])
                started = True
                if h['bias'] is not None:
                    fkb, fka, fqb, fqa = h['bias']
                    self.mm(cols, fka[:, j * 128:(j + 1) * 128], fqa[:, i * 128:(i + 1) * 128], False, True,
                            [fkb, fqb], [ps])

        def exp_pv(jn):
            j = jlist[jn]
            ps = pS[jn % len(pS)]
            pt = PT[jn % len(PT)]
            self.act(pt.ap[:, 0:G * 128], ps.ap[:, 0:G * 128], AF.Exp, [ps], [pt], scale=float(scale))
            for hh, h in enumerate(heads):
                vb, va = h['v'](j)
                self.mm(pO.ap[:, hh * 65:(hh + 1) * 65], pt.ap[:, hh * 128:(hh + 1) * 128], va[:, 0:65],
                        jn == 0 and hh == 0, jn == nJ - 1, [pt, vb], [pO])

        scores(0)
        for jn in range(nJ):
            if jn + 1 < nJ:
                scores(jn + 1)
            exp_pv(jn)
        if osb is not None:
            ob = osb.ap[:, 0:G * 65]
            self.cp('act', ob, pO.ap[:, 0:G * 65], [pO], [osb])
            o3 = ob.rearrange('p (g d) -> p g d', d=65)
            od = obuf.ap[:, i, ocol0:ocol0 + G * 64].rearrange('p (g d) -> p g d', d=64)
            self.tt('pool', o3[:, :, 64], o3[:, :, 64], self.negone.ap[:, 0:G], ALU.pow, [osb, self.negone], [osb])
            self.tt('pool', od, o3[:, :, 0:64], o3[:, :, 64:65].to_broadcast([128, G, 64]), ALU.mult,
                    [osb], [(obuf, (i * D + ocol0) * 2, (i * D + ocol0 + G * 64) * 2)])
            return
        A = self.A
        A.mark()
        den = A.alloc([G], F32, align=64)
        ov = pO.ap[:, 0:G * 65].rearrange('p (g d) -> p g d', d=65)
        if sink is not None:
            self.tt('dve', den.ap, ov[:, :, 64], sink, ALU.add, [pO, self.esink], [den])
        else:
            self.cp('dve', den.ap, ov[:, :, 64], [pO], [den])
        self.S.op('dve', lambda e: e.reciprocal(out=den.ap, in_=den.ap), self.S._cells([den]), self.S._cells([den]))
        od = obuf.ap[:, i, ocol0:ocol0 + G * 64].rearrange('p (g d) -> p g d', d=64)
        self.tt('dve', od, ov[:, :, 0:64], den.ap.unsqueeze(2).to_broadcast([128, G, 64]), ALU.mult,
                [pO, den], [(obuf, (i * D + ocol0) * 2, (i * D + ocol0 + G * 64) * 2)])
        A.release()

    def fox_attn(self, p, qT, kT, V, FQ, FK, obuf, PT):
        pj, pO = self.pj, self.pO
        tri = self.constb.ap[:, 1]
        A = self.A
        rn = 0
        for hh in range(2):
            qa, ka = qT.ap[64 * hh:64 * hh + 64], kT.ap[64 * hh:64 * hh + 64]
            fqa, fka = FQ.ap[32 * hh:32 * hh + 6], FK.ap[32 * hh:32 * hh + 6]
            for c in range(4):
                po = pO[(2 * hh + c) % 2]
                nJ = 4 * c + 4

                def geom(j):
                    q0 = max(j, 4 * c)
                    off = (q0 - 4 * c) * 128
                    return q0, off, (4 * c + 4 - q0) * 128

                def scores(j, rn):
                    ps = pj[rn % 4]
                    q0, off, ncol = geom(j)
                    cols = ps.ap[:, off:off + ncol]
                    qs = slice(q0 * 128, (4 * c + 4) * 128)
                    self.mm(cols, ka[:, j * 128:(j + 1) * 128], qa[:, qs], True, False, [kT, qT], [ps])
                    diag = j >= 4 * c
                    self.mm(cols, fka[:, j * 128:(j + 1) * 128], fqa[:, qs], False, not diag, [FK, FQ], [ps])
                    if diag:
                        self.mm(ps.ap[:, off:off + 128], self.identb.ap, tri, False, True, [self.identb, self.constb], [ps])

                def exp_pv(j, rn):
                    ps = pj[rn % 4]
                    pt = PT[rn % 2]
                    q0, off, ncol = geom(j)
                    self.act(pt.ap[:, off:off + ncol], ps.ap[:, off:off + ncol], AF.Exp, [ps], [pt])
                    for t in range(q0, 4 * c + 4):
                        tl = t - 4 * c
                        self.mm(po.ap[:, tl * 65:(tl + 1) * 65], pt.ap[:, tl * 128:(tl + 1) * 128], V.ap[:, j, hh, 0:65],
                                j == 0 and tl == 0, j == t, [pt, bsub(V, j)], [po])

                scores(0, rn)
                for j in range(nJ):
                    if j + 1 < nJ:
                        scores(j + 1, rn + j + 1)
                    exp_pv(j, rn + j)
                rn += nJ
                A.mark()
                den = A.alloc([4], F32, align=64)
                ov = po.ap[:, 0:260].rearrange('p (g d) -> p g d', d=65)
                self.cp('dve', den.ap, ov[:, :, 64], [po], [den])
                self.S.op('dve', lambda e, den=den: e.reciprocal(out=den.ap, in_=den.ap), self.S._cells([den]), self.S._cells([den]))
                col0 = 128 * p + 64 * hh
                od = obuf.ap[:, 4 * c:4 * c + 4, col0:col0 + 64]
                self.tt('dve', od, ov[:, :, 0:64], den.ap.unsqueeze(2).to_broadcast([128, 4, 64]), ALU.mult,
                        [po, den], [(obuf, 4 * c * D * 2, (4 * c + 4) * D * 2)])
                A.release()

    def out_proj(self, l, obuf, pbanks, ptb):
        A = self.A
        A.mark()
        w = A.alloc([8, D], BF16)
        self.dma('pool', w.ap, self.dr['wout'][l].rearrange('(kc p) n -> p kc n', p=128), (), [w])
        oT = [A.alloc([8, 128], BF16) for _ in range(2)]
        tmp = [A.alloc([512], F32) for _ in range(2)]
        n = 0
        for t in range(NT):
            o_t = oT[t % 2]
            for k4 in range(2):
                pb = ptb[k4]
                for kk in range(4):
                    kc = 4 * k4 + kk
                    self.tr(pb.ap[:, kk * 128:(kk + 1) * 128], obuf.ap[:, t, kc * 128:(kc + 1) * 128], self.identb.ap,
                            [(obuf, t * D * 2, (t + 1) * D * 2), self.identb], [pb])
                self.cp('act', o_t.ap[:, 4 * k4:4 * k4 + 4].rearrange('p a b -> p (a b)'), pb.ap, [pb],
                        [(o_t, 4 * k4 * 256, (4 * k4 + 4) * 256)])
            for hf in range(2):
                pb = pbanks[n % len(pbanks)]
                tb = tmp[n % 2]
                n += 1
                for kc in range(8):
                    self.mm(pb.ap, o_t.ap[:, kc], w.ap[:, kc, hf * 512:(hf + 1) * 512], kc == 0, kc == 7, [o_t, w], [pb])
                self.tt('dve', tb.ap, pb.ap, self.gate.ap[:, 0, hf * 512:(hf + 1) * 512], ALU.mult, [pb, bsub(self.gate, 0)], [tb])
                xs = (self.X, (t * D + hf * 512) * 4, (t * D + hf * 512 + 512) * 4)
                xa = self.X.ap[:, t, hf * 512:(hf + 1) * 512]
                self.tt('pool', xa, xa, tb.ap, ALU.add, [xs, tb], [xs])
        A.release()

    def ffn_load(self, gsrc, usrc, dsrc, nf, wbuf):
        wg, wu, wd = wbuf
        self.dma('pool', wg.ap[:, 0:nf], gsrc.rearrange('f p k n -> p f k n'), (), [wg])
        self.dma('pool', wu.ap[:, 0:nf], usrc.rearrange('f p k n -> p f k n'), (), [wu])
        self.dma('pool', wd.ap[:, 0:nf], dsrc.rearrange('f p n -> p f n'), (), [wd])

    def ffn_gu(self, hT, nf, wbuf, gT, pbanks, tmp, c):
        wg, wu, wd = wbuf
        for f in range(nf):
            pg = pbanks[self.fn % len(pbanks)]
            pu = pbanks[(self.fn + 1) % len(pbanks)]
            self.fn += 2
            for kc in range(8):
                self.mm(pg.ap, wg.ap[:, f, kc], hT.ap[:, kc, c * 512:(c + 1) * 512], kc == 0, kc == 7,
                        [wg] + self.hT_rng(hT, c * 512, (c + 1) * 512), [pg])
            for kc in range(8):
                self.mm(pu.ap, wu.ap[:, f, kc], hT.ap[:, kc, c * 512:(c + 1) * 512], kc == 0, kc == 7,
                        [wu] + self.hT_rng(hT, c * 512, (c + 1) * 512), [pu])
            sg = tmp[2 + f % 2]
            sgb = sg.ap.bitcast(BF16)[:, 0:512]
            self.act(sgb, pg.ap, AF.Silu, [pg], [sg])
            self.tt('dve', gT.ap[:, f], pu.ap, sgb, ALU.mult, [pu, sg], [bsub(gT, f)])

    def ffn_down(self, nf, wbuf, gT, pbanks, tmp, c, comb):
        wg, wu, wd = wbuf
        for tl in range(4):
            t = c * 4 + tl
            for hf in range(2):
                pb = pbanks[self.fn % len(pbanks)]
                tb = tmp[self.fn % 2]
                self.fn += 1
                for f in range(nf):
                    self.mm(pb.ap, gT.ap[:, f, tl * 128:(tl + 1) * 128], wd.ap[:, f, hf * 512:(hf + 1) * 512],
                            f == 0, f == nf - 1, [gT, wd], [pb])
                gap = self.gate.ap[:, 0, hf * 512:(hf + 1) * 512]
                if comb is None:
                    self.tt('dve', tb.ap, pb.ap, gap, ALU.mult, [pb, bsub(self.gate, 0)], [tb])
                else:
                    cb, cap = comb(t)
                    self.stt(tb.ap, pb.ap, cap, gap, ALU.mult, ALU.mult, [pb, bsub(self.gate, 0), cb], [tb])
                xs = (self.X, (t * D + hf * 512) * 4, (t * D + hf * 512 + 512) * 4)
                xa = self.X.ap[:, t, hf * 512:(hf + 1) * 512]
                self.tt('pool', xa, xa, tb.ap, ALU.add, [xs, tb], [xs])

    def layer0_mixer(self, s):
        A, dr = self.A, self.dr
        pj, pO, ptb = self.pj, self.pO, self.ptb
        sm = self.small
        A.mark()
        hT = A.alloc([8, SEQ], BF16)
        obuf = A.alloc([NT, D], BF16)
        self.norm_hT2(0, 0, hT)
        if self.lvl < 0.45:
            return
        PT = [A.alloc([512], BF16) for _ in range(2)]
        tri = (self.constb, self.constb.ap[:, 1])
        prevb = (self.constb, self.constb.ap[:, 2])
        for p in range(4):
            A.mark()
            qT = A.alloc([SEQ], BF16)
            kT = A.alloc([SEQ], BF16)
            V = A.alloc([NT, 2, 66], BF16)
            FQ = A.alloc([SEQ], BF16)
            FK = A.alloc([SEQ], BF16)
            z = A.alloc([NT, 2], F32)
            self.memset('pool', V.ap, 1.0, [V])
            if self.lvl < 0.455:
                return
            self.proj_fm(hT, dr['wfm0'], [('plain', (p,), lambda c, b=qT: (b, b.ap[:, c * 512:(c + 1) * 512]), 0.125),
                                          ('plain', (4 + p,), lambda c, b=kT: (b, b.ap[:, c * 512:(c + 1) * 512]), 1.0)], pj)

            if self.lvl < 0.47:
                return

            def evac(t, pb, V=V, z=z, p=p):
                if self.lvl < 0.49:
                    return
                if self.lvl != 0.494:
                    self.cp('act', V.ap[:, t, :, 0:64], pb.ap[:, 0:128].rearrange('p (h d) -> p h d', d=64), [pb], [bsub(V, t)])
                if self.lvl != 0.492:
                    self.tt('dve', z.ap[:, t], pb.ap[:, 128:130], self.bc8.ap[:, 0, 2 * p:2 * p + 2], ALU.add, [pb, self.bc8], [z])
            self.proj_tm(hT, dr['wtm0f'][p], 130, pj, evac)
            if self.lvl < 0.55:
                return
            A.mark()
            sp = A.alloc([NT, 2], F32)
            Lrep = A.alloc([NT, 64], F32)
            C = A.alloc([SEQ], F32)
            Hb = A.alloc([SEQ], BF16)
            Mb = A.alloc([SEQ], BF16)
            Lb = A.alloc([SEQ], BF16)
            self.act(sp.ap, z.ap, AF.Exp, [z], [sp], scale=-1.0)
            self.act(sp.ap, sp.ap, AF.Ln, [sp], [sp], bias=1.0)
            self.memset('pool', Lrep.ap, 0.0, [Lrep])
            for t in range(NT):
                for sl in range(2):
                    self.ts('dve', Lrep.ap[:, t, 32 * sl:32 * sl + 6], sp.ap[:, t, sl:sl + 1].to_broadcast([128, 6]), -1.0, None,
                            ALU.mult, None, [sp], [bsub(Lrep, t)])
            U = self.constf.ap[:, 1]
            for t in range(NT):
                pb = pj[t % 4]
                self.mm(pb.ap[0:64, 0:128], Lrep.ap[:, t], U, True, True, [bsub(Lrep, t), self.constf], [pb])
                cs = (C, t * 512, (t + 1) * 512)
                if t == 0:
                    self.cp('dve', C.ap[0:64, 0:128], pb.ap[0:64, 0:128], [pb], [cs])
                else:
                    self.ts('dve', C.ap[0:64, t * 128:(t + 1) * 128], pb.ap[0:64, 0:128], C.ap[0:64, t * 128 - 1:t * 128], None,
                            ALU.add, None, [pb, (C, (t - 1) * 512, t * 512)], [cs])
            c64, h64, m64, l64 = C.ap[0:64], Hb.ap[0:64], Mb.ap[0:64], Lb.ap[0:64]
            self.cp('dve', h64, c64, [C], [Hb])
            self.tt('dve', c64, c64, h64, ALU.subtract, [C, Hb], [C])
            self.cp('dve', m64, c64, [C], [Mb])
            self.tt('dve', c64, c64, m64, ALU.subtract, [C, Mb], [C])
            self.cp('dve', l64, c64, [C], [Lb])
            for (dst, c0) in ((FQ, 2), (FK, 6)):
                s64 = sm.ap[0:64]
                f64 = dst.ap[0:64]
                self.ts('dve', f64, h64, s64[:, c0:c0 + 1], s64[:, c0 + 3:c0 + 4], ALU.mult, ALU.add, [Hb, sm], [dst])
                self.stt(f64, m64, s64[:, c0 + 1:c0 + 2], f64, ALU.mult, ALU.add, [Mb, sm, dst], [dst])
                self.stt(f64, l64, s64[:, c0 + 2:c0 + 3], f64, ALU.mult, ALU.add, [Lb, sm, dst], [dst])
            A.release()
            if self.lvl < 0.65:
                return
            self.fox_attn(p, qT, kT, V, FQ, FK, obuf, PT)
            A.release()
        if self.lvl < 0.75:
            return
        A.mark()
        cosT = A.alloc([SEQ], F32)
        sinT = A.alloc([SEQ], F32)
        self.rope_tables(s, cosT, sinT)
        sqT = A.alloc([NT, 4, 128], BF16)
        skT = A.alloc([SEQ], BF16)
        Vs = A.alloc([NT, 2, 66], BF16)
        self.memset('pool', Vs.ap, 1.0, [Vs])
        specs = [('rope', (8 + c, 12 + c), (lambda cc, c=c: (sqT, sqT.ap[:, 4 * cc:4 * cc + 4, c, :])), (cosT, sinT))
                 for c in range(4)]
        specs.append(('rope', (16, 17), (lambda cc: (skT, skT.ap[:, cc * 512:(cc + 1) * 512])), (cosT, sinT)))
        self.proj_fm(hT, dr['wfm0'], specs, pj)

        def evac_s(t, pb):
            self.cp('act', Vs.ap[:, t, :, 0:64], pb.ap[:, 0:128].rearrange('p (h d) -> p h d', d=64), [pb], [bsub(Vs, t)])
        self.proj_tm(hT, dr['wtm0s'], 128, pj, evac_s)
        for g in range(2):
            heads = []
            for c in range(4):
                heads.append(dict(q=None, k=(skT, skT.ap[64 * g:64 * g + 64]),
                                  v=(lambda j, g=g: (bsub(Vs, j), Vs.ap[:, j, g])), bias=None))
            for i in range(NT):
                jl = [i - 1, i] if i > 0 else [i]
                self.attn_group(i, jl, heads, (lambda j, i=i: tri if j == i else prevb), 0.125,
                                pj, pO[i % 2], PT, obuf, 512 + 256 * g, sink=self.esink.ap[:, 4 * g:4 * g + 4],
                                qbatch=(bsub(sqT, i), sqT.ap[64 * g:64 * g + 64, i]))
        A.release()
        if self.lvl < 0.85:
            return
        self.out_proj(0, obuf, pj, ptb)
        A.release()

    def run_units(self, hT, units, wb, gTs, tmp):
        self.fn = 0
        g, u_, d, nf, cb = units[0]
        self.ffn_load(g, u_, d, nf, wb[0])
        steps = [(ui, c) for ui in range(len(units)) for c in range(4)]
        for k, (ui, c) in enumerate(steps):
            if c == 1 and ui + 1 < len(units):
                g2, u2, d2, nf2, _ = units[ui + 1]
                self.ffn_load(g2, u2, d2, nf2, wb[(ui + 1) % 2])
            self.ffn_gu(hT, units[ui][3], wb[ui % 2], gTs[k % 2], self.pj, tmp, c)
            if k >= 1:
                pu_, pc_ = steps[k - 1]
                self.ffn_down(units[pu_][3], wb[pu_ % 2], gTs[(k - 1) % 2], self.pj, tmp, pc_, units[pu_][4])
        pu_, pc_ = steps[-1]
        self.ffn_down(units[pu_][3], wb[pu_ % 2], gTs[(len(steps) - 1) % 2], self.pj, tmp, pc_, units[pu_][4])

    def alloc_ffn(self, l):
        A = self.A
        hT = A.alloc([8, SEQ], BF16)
        self.norm_hT2(l, 1, hT)
        wb = [(A.alloc([6, 8, 128], BF16), A.alloc([6, 8, 128], BF16), A.alloc([6, D], BF16)) for _ in range(2)]
        gT = [A.alloc([6, 512], BF16) for _ in range(2)]
        tmp = [A.alloc([512], F32) for _ in range(4)]
        return hT, wb, gT, tmp

    def layer0_ffn(self, s):
        A, dr = self.A, self.dr
        A.mark()
        hT, wb, gT, tmp = self.alloc_ffn(0)
        units = []
        f0 = 0
        for nf in (6, 6, 5, 5):
            units.append((dr['ffn_g'][f0:f0 + nf], dr['ffn_u'][f0:f0 + nf], dr['ffn_d'][f0:f0 + nf], nf, None))
            f0 += nf
        self.run_units(hT, units, wb, gT, tmp)
        A.release()

    def layer1_mixer(self, s):
        A, dr = self.A, self.dr
        pj, pO, ptb = self.pj, self.pO, self.ptb
        A.mark()
        hT = A.alloc([8, SEQ], BF16)
        obuf = Buf(hT.space, hT.off, hT.nbytes,
                   hT.ap.rearrange('p a b -> p (a b)').rearrange('p (t d) -> p t d', d=D))
        A.mark()
        qT = A.alloc([2, NT, 4, 128], BF16)
        kT = A.alloc([2, SEQ], BF16)
        V = A.alloc([NT, 4, 66], BF16)
        iqT = A.alloc([4, SEQ], BF16)
        ikT = A.alloc([SEQ], BF16)
        iw = A.alloc([NT, 8], F32)
        A.mark()
        cosT = A.alloc([SEQ], BF16)
        sinT = A.alloc([SEQ], BF16)
        self.rope_tables(s, cosT, sinT)
        self.norm_hT2(1, 0, hT)
        self.memset('pool', V.ap, 1.0, [V])
        specs = []
        for c in range(8):
            specs.append(('rope', (c, 8 + c), (lambda cc, c=c: (bsub(qT, c // 4), qT.ap[:, c // 4, 4 * cc:4 * cc + 4, c % 4, :])), (cosT, sinT)))
        for c in range(2):
            specs.append(('rope', (16 + c, 18 + c), (lambda cc, c=c: (bsub(kT, c), kT.ap[:, c, cc * 512:(cc + 1) * 512])), (cosT, sinT)))
        for c in range(4):
            specs.append(('rope', (20 + c, 24 + c), (lambda cc, c=c: (bsub(iqT, c), iqT.ap[:, c, cc * 512:(cc + 1) * 512])), (cosT, sinT)))
        specs.append(('rope', (28, 29), (lambda cc: (ikT, ikT.ap[:, cc * 512:(cc + 1) * 512])), (cosT, sinT)))
        self.proj_fm(hT, dr['wfm1'], specs, pj)

        def evac(t, pb):
            self.cp('act', V.ap[:, t, :, 0:64], pb.ap[:, 0:256].rearrange('p (h d) -> p h d', d=64), [pb], [bsub(V, t)])
            self.cp('dve', iw.ap[:, t], pb.ap[:, 256:264], [pb], [iw])
        self.proj_tm(hT, dr['wtm1'], 264, pj, evac)
        A.release()
        sc = A.alloc([SEQ], F32)
        mb = A.alloc([SEQ], BF16)
        mbT = A.alloc([SEQ], BF16)
        bs = A.alloc([8], F32, align=64)
        TB = 25
        steps = A.alloc([TB + 1], F32, align=64)
        mids = A.alloc([TB + 1], F32, align=64)
        G = A.alloc([TB], F32, align=64)
        cand = A.alloc([TB], F32, align=64)
        rr = [A.alloc([512], F32) for _ in range(2)]
        Dm = A.alloc([8, 128], F32)
        PT = [A.alloc([512], BF16) for _ in range(2)]
        trineg = self.constf.ap[:, 3]
        osb = [A.alloc([260], F32, align=64) for _ in range(1)] * 2
        cnt = [0]

        def sc_of(t):
            if t % 2 == 1 and t <= 11:
                nb = (t + 1) * 512
                off = obuf.nbytes - nb
                ap = obuf.ap.rearrange('p t d -> p (t d)')[:, off // 2:obuf.nbytes // 2].bitcast(F32)
                return Buf(obuf.space, obuf.off + off, nb, ap)
            return sc

        ptbf = Buf('psum', ptb[1].off, 2048, self.P.base[:, ptb[1].off // 4:ptb[1].off // 4 + 512])
        pS_att = [pj[3], ptbf]
        identf = self.constf.ap[:, 0]
        lb = [pj[0], pj[1]]
        accb = pj[2]

        def indexer(i):
            L = (i + 1) * 128
            scb_ = sc_of(i)
            for h in range(8):
                self.ts('pool', Dm.ap[:, h], identf, iw.ap[:, i, h:h + 1], None, ALU.mult, None, [self.constf, iw], [bsub(Dm, h)])
            for c4 in range((L + 511) // 512):
                nc_ = min(512, L - 512 * c4)
                scs = (scb_, c4 * 2048, c4 * 2048 + nc_ * 4)
                sca = scb_.ap[:, c4 * 512:c4 * 512 + nc_]
                base = cnt[0]

                def logits(hi):
                    ps = lb[(base + hi) % 2]
                    hf = hi % 2
                    self.mm(ps.ap[:, 0:nc_], iqT.ap[64 * hf:64 * hf + 64, hi // 2, i * 128:(i + 1) * 128],
                            ikT.ap[64 * hf:64 * hf + 64, c4 * 512:c4 * 512 + nc_], True, True, [bsub(iqT, hi // 2), ikT], [ps])

                logits(0)
                for hi in range(8):
                    if hi + 1 < 8:
                        logits(hi + 1)
                    ps = lb[(base + hi) % 2]
                    r = rr[(base + hi) % 2]
                    self.act(r.ap[:, 0:nc_], ps.ap[:, 0:nc_], AF.Relu, [ps], [r])
                    self.mm(accb.ap[:, 0:nc_], Dm.ap[:, hi], r.ap[:, 0:nc_], hi == 0, hi == 7, [bsub(Dm, hi), r], [accb])
                cnt[0] += 8
                self.cp('act', sca, accb.ap[:, 0:nc_], [accb], [scs])

        def topk(i):
            L = (i + 1) * 128
            scb_ = sc_of(i)
            dg = (scb_, i * 512, (i + 1) * 512)
            scl = (scb_, 0, L * 4)
            mbl = (mb, 0, L * 2)
            sca_ = scb_.ap[:, 0:L]
            if i >= 2:
                self.S.op('dve', lambda e: e.tensor_reduce(out=bs.ap[:, 0:1], in_=sca_, axis=AX.X, op=ALU.min),
                          self.S._cells([scl]), self.S._cells([bs]))
            self.tt('pool', scb_.ap[:, i * 128:(i + 1) * 128], scb_.ap[:, i * 128:(i + 1) * 128], trineg, ALU.add, [dg, self.constf], [dg])
            if i >= 2:
                self.S.op('dve', lambda e: e.tensor_reduce(out=bs.ap[:, 1:2], in_=sca_, axis=AX.X, op=ALU.max),
                          self.S._cells([scl]), self.S._cells([bs]))
                self.tt('dve', bs.ap[:, 2:3], bs.ap[:, 1:2], bs.ap[:, 0:1], ALU.subtract, [bs], [bs])
                self.ts('dve', steps.ap, self.small.ap[:, 16:16 + TB + 1], bs.ap[:, 2:3], None, ALU.mult, None, [bs, self.small], [steps])
                self.tt('dve', mids.ap[:, 0:1], bs.ap[:, 0:1], steps.ap[:, 0:1], ALU.add, [bs, steps], [mids])
                for t in range(TB):
                    self.S.op('dve', lambda e, t=t: e.tensor_scalar(out=mb.ap[:, 0:L], in0=sca_, scalar1=mids.ap[:, t:t + 1], scalar2=0.0,
                                                                    op0=ALU.is_ge, op1=ALU.add, accum_out=bs.ap[:, 4:5]),
                              self.S._cells([scl, mids]), self.S._cells([mbl, bs]))
                    self.stt(G.ap[:, t:t + 1], bs.ap[:, 4:5], 255.5, steps.ap[:, t:t + 1], ALU.is_ge, ALU.mult, [bs, steps], [G])
                    self.stt(mids.ap[:, t + 1:t + 2], G.ap[:, t:t + 1], steps.ap[:, t + 1:t + 2], mids.ap[:, t:t + 1],
                             ALU.subtract, ALU.add, [G, steps, mids], [mids])
                self.ts('dve', G.ap, G.ap, 0.0, None, ALU.is_gt, None, [G], [G])
                self.tt('dve', cand.ap, mids.ap[:, 0:TB], G.ap, ALU.mult, [mids, G], [cand])
                self.ts('dve', G.ap, G.ap, -1.0, 1.0e30, ALU.add, ALU.mult, [G], [G])
                self.tt('dve', cand.ap, cand.ap, G.ap, ALU.add, [cand, G], [cand])
                self.S.op('dve', lambda e: e.tensor_reduce(out=bs.ap[:, 5:6], in_=cand.ap, axis=AX.X, op=ALU.max),
                          self.S._cells([cand]), self.S._cells([bs]))
                self.tt('dve', bs.ap[:, 0:1], bs.ap[:, 0:1], bs.ap[:, 5:6], ALU.max, [bs], [bs])
                self.ts('dve', mb.ap[:, 0:L], sca_, bs.ap[:, 0:1], NEG, ALU.is_lt, ALU.mult, [scl, bs], [mbl])
            else:
                self.ts('dve', mb.ap[:, 0:L], sca_, -1.0e29, NEG, ALU.is_le, ALU.mult, [scl], [mbl])

        def mask_T(i):
            for j0 in range(0, i + 1, 4):
                pb = ptb[0]
                nj = min(4, i + 1 - j0)
                for j in range(j0, j0 + nj):
                    self.tr(pb.ap[:, (j - j0) * 128:(j - j0 + 1) * 128], mb.ap[:, j * 128:(j + 1) * 128], self.identb.ap,
                            [(mb, j * 256, (j + 1) * 256), self.identb], [pb])
                self.cp('act', mbT.ap[:, j0 * 128:(j0 + nj) * 128], pb.ap[:, 0:nj * 128], [pb], [(mbT, j0 * 256, (j0 + nj) * 256)])

        def attend(i):
            for g in range(4):
                heads = []
                hf = g % 2
                for m in range(4):
                    heads.append(dict(q=None, k=(bsub(kT, g // 2), kT.ap[64 * hf:64 * hf + 64, g // 2]),
                                      v=(lambda j, g=g: (bsub(V, j), V.ap[:, j, g])), bias=None))
                self.attn_group(i, list(range(i + 1)), heads,
                                (lambda j: ((mbT, j * 256, (j + 1) * 256), mbT.ap[:, j * 128:(j + 1) * 128])), 0.125,
                                pS_att, pO[g % 2], PT, obuf, 256 * g, osb=osb[g % 2],
                                qbatch=(bsub(qT, g // 2), qT.ap[64 * hf:64 * hf + 64, g // 2, i]))

        indexer(0)
        topk(0)
        mask_T(0)
        indexer(1)
        for i in range(NT):
            early = (i + 2 < NT) and (sc_of(i + 2) is not sc or sc_of(i + 1) is not sc)
            if early:
                indexer(i + 2)
            if i + 1 < NT:
                topk(i + 1)
            if (not early) and i + 2 < NT:
                indexer(i + 2)
            attend(i)
            if i + 1 < NT:
                mask_T(i + 1)
        A.release()
        self.out_proj(1, obuf, pj, ptb)
        A.release()

    def layer1_moe(self, s):
        A, dr = self.A, self.dr
        A.mark()
        hT, wb, gT, tmp = self.alloc_ffn(1)
        lg = A.alloc([NT, 8], F32)
        M8 = A.alloc([NT, 8], F32)
        comb = A.alloc([NT, 8], F32)
        sm4 = A.alloc([4, NT], F32)
        cm2 = A.alloc([NT, 8], F32)

        def evac(t, pb):
            self.tt('dve', lg.ap[:, t], pb.ap[:, 0:8], self.bc8.ap[:, 2], ALU.add, [pb, self.bc8], [lg])
            self.S.op('dve', lambda e, t=t: e.max(out=M8.ap[:, t], in_=lg.ap[:, t]), self.S._cells([lg]), self.S._cells([M8]))
        self.proj_tm(hT, dr['wrt'], 8, self.pj, evac)
        m1, m2 = M8.ap[:, :, 0], M8.ap[:, :, 1]
        d_, e2, g1, g2 = sm4.ap[:, 0], sm4.ap[:, 1], sm4.ap[:, 2], sm4.ap[:, 3]
        self.tt('dve', d_, m2, m1, ALU.subtract, [M8], [sm4])
        self.act(e2, d_, AF.Exp, [sm4], [sm4])
        self.ts('dve', d_, e2, 1.0, None, ALU.add, None, [sm4], [sm4])
        self.S.op('dve', lambda e: e.reciprocal(out=g1, in_=d_), self.S._cells([sm4]), self.S._cells([sm4]))
        self.tt('dve', g2, e2, g1, ALU.mult, [sm4], [sm4])
        self.tt('dve', d_, g1, g2, ALU.subtract, [sm4], [sm4])
        bc = lambda a: a.unsqueeze(2).to_broadcast([128, NT, 8])
        self.tt('dve', comb.ap, lg.ap, bc(m1), ALU.is_ge, [lg, M8], [comb])
        self.tt('dve', comb.ap, comb.ap, bc(d_), ALU.mult, [comb, sm4], [comb])
        self.tt('dve', cm2.ap, lg.ap, bc(m2), ALU.is_ge, [lg, M8], [cm2])
        self.tt('dve', cm2.ap, cm2.ap, bc(g2), ALU.mult, [cm2, sm4], [cm2])
        self.tt('dve', comb.ap, comb.ap, cm2.ap, ALU.add, [comb, cm2], [comb])
        units = []
        for e in range(NEXP):
            for (f0, nf) in ((0, 6), (6, 5)):
                units.append((dr['exp_g'][e, f0:f0 + nf], dr['exp_u'][e, f0:f0 + nf], dr['exp_d'][e, f0:f0 + nf], nf,
                              (lambda t, e=e: (comb, comb.ap[:, t, e:e + 1]))))
        self.run_units(hT, units, wb, gT, tmp)
        A.release()

    def final(self, s):
        A = self.A
        A.mark()
        sq = A.alloc([D], F32)
        yb = [A.alloc([D], F32) for _ in range(2)]
        self.fnw = A.alloc([D], F32)
        self.dma('sp', self.fnw.ap, self.dr['fnw'].partition_broadcast(128), (), [self.fnw])
        X = self.X
        for t in range(NT):
            y = yb[t % 2]
            if t % 4 == 0:
                self.rstd(t, 4, sq)
            self.stt(y.ap, X.ap[:, t], self.stat.ap[:, 1, t:t + 1], self.fnw.ap, ALU.mult, ALU.mult, [bsub(X, t), self.stat, self.fnw], [y])
            self.dma('sp', self.dr['out'][s, t * 128:(t + 1) * 128, :], y.ap, [y], ())
        A.release()

    def run_seq(self, s, stages=99):
        for t in range(NT):
            self.dma('sp', self.X.ap[:, t], self.dr['x'][s, t * 128:(t + 1) * 128, :], (), [bsub(self.X, t)])
        self.lvl = stages
        self.cur_s = s
        if stages >= 0.2:
            if s == 0:
                self.ada(0, 0)
            self.expand_gate(s, 0, 2)
        if stages >= 0.4:
            self.layer0_mixer(s)
        if stages >= 2:
            if s == 0:
                self.ada(0, 1)
            self.expand_gate(s, 0, 5)
            self.layer0_ffn(s)
        if stages >= 3:
            if s == 0:
                self.ada(1, 0)
            self.expand_gate(s, 1, 2)
            self.layer1_mixer(s)
        if stages >= 4:
            if s == 0:
                self.ada(1, 1)
            self.expand_gate(s, 1, 5)
            self.layer1_moe(s)
        if stages >= 5:
            self.final(s)
        else:
            for t in range(NT):
                self.dma('sp', self.dr['out'][s, t * 128:(t + 1) * 128, :], self.X.ap[:, t], [bsub(self.X, t)], ())


SB_BYTES = 207 * 1024


def build_program(nseq, shapes, stages=99):
    nc = bass.Bass("TRN2", target_bir_lowering=False)
    dr = {}
    for name, (shp, dt, kind) in shapes.items():
        dr[name] = nc.dram_tensor(name, list(shp), dt, kind=kind).ap()
    S = Sched(nc)
    with ExitStack() as es:
        sb = es.enter_context(nc.sbuf_tensor("arena", [128, SB_BYTES // 4], F32))
        ps = es.enter_context(nc.psum_tensor("psum", [128, 4096], F32))
        for k in S.sem_keys():
            nm = "s_" + "_".join(str(x) for x in (k if isinstance(k, tuple) else (k,)))
            S.sems[k] = es.enter_context(nc.semaphore(nm))
        A = Arena('sb', sb[:], SB_BYTES)
        P = Arena('psum', ps[:], 16384)
        kb = K(nc, S, A, P, dr, nseq)
        kb.pj = [P.alloc([512], F32, align=2048) for _ in range(4)]
        kb.pO = [P.alloc([512], F32, align=2048) for _ in range(2)]
        kb.ptb = [P.alloc([512], BF16, align=2048) for _ in range(2)]
        kb.setup()
        for s in range(nseq):
            kb.run_seq(s, stages)
        S.wait_all('sp')
        with nc.Block() as block:
            S.emit(block)
    return nc, S


def _chunks(W, col_lists):
    cols = np.concatenate(col_lists)
    g = W[:, cols]
    nch = len(col_lists)
    return np.ascontiguousarray(g.reshape(8, 128, nch, 128).transpose(2, 1, 0, 3))


def _tm(W, cols):
    g = W[:, cols]
    return np.ascontiguousarray(g.reshape(8, 128, len(cols)).transpose(1, 0, 2))


def _rot(cols):
    cols = np.asarray(cols).reshape(-1, 64)
    return np.concatenate([cols[:, 32:], cols[:, :32]], axis=1).reshape(-1)


def host_prepare(inp):
    f = lambda a: np.asarray(a, dtype=np.float32)
    ar = np.arange
    w0 = f(inp['e_w_in'])[0]
    o_fq, o_fk, o_fv, o_fg, o_sq, o_sk, o_sv = 0, 512, 1024, 1536, 1544, 2056, 2184
    cl = []
    for p in range(4):
        cl.append(o_fq + 128 * p + ar(128))
    for p in range(4):
        cl.append(o_fk + 128 * p + ar(128))
    sqc = [np.concatenate([o_sq + 64 * c + ar(64), o_sq + 64 * (4 + c) + ar(64)]) for c in range(4)]
    cl += sqc
    cl += [_rot(c) for c in sqc]
    skc = o_sk + ar(128)
    cl += [skc, _rot(skc)]
    wfm0 = _chunks(w0, cl)
    wtm0f = np.stack([_tm(w0, np.concatenate([o_fv + 128 * p + ar(128), o_fg + 2 * p + ar(2)])) for p in range(4)])
    wtm0s = _tm(w0, o_sv + ar(128))
    w1 = f(inp['o_w_in'])[0]
    o_q, o_k, o_v, o_iq, o_ik, o_iw = 0, 1024, 1280, 1536, 2048, 2112
    LH = [0, 1, 2, 3, 8, 9, 10, 11]
    UH = [4, 5, 6, 7, 12, 13, 14, 15]
    qc = [np.concatenate([o_q + 64 * LH[c] + ar(64), o_q + 64 * UH[c] + ar(64)]) for c in range(8)]
    kc_ = [o_k + 128 * c + ar(128) for c in range(2)]
    iqc = [o_iq + 128 * c + ar(128) for c in range(4)]
    ikc = np.concatenate([o_ik + ar(64), o_ik + ar(64)])
    cl1 = qc + [_rot(c) for c in qc] + kc_ + [_rot(c) for c in kc_] + iqc + [_rot(c) for c in iqc] + [ikc, _rot(ikc)]
    wfm1 = _chunks(w1, cl1)
    wtm1 = _tm(w1, np.concatenate([o_v + ar(256), o_iw + ar(8)]))
    ada = np.stack([f(inp['e_ada_w'])[0], f(inp['o_ada_w'])[0]])
    ada_w = np.ascontiguousarray(ada.reshape(2, 8, 128, 6, 8, 128).transpose(0, 3, 2, 4, 1, 5))
    ada_b_flat = np.stack([f(inp['e_ada_b'])[0], f(inp['o_ada_b'])[0]])
    adab = np.ascontiguousarray(ada_b_flat.reshape(2, 48, 128).transpose(2, 0, 1))
    nws = [f(inp['e_norm_mix'])[0], f(inp['e_norm_ffn'])[0], f(inp['o_norm_mix'])[0], f(inp['o_norm_ffn'])[0],
           f(inp['final_norm'])]
    normw = np.ascontiguousarray(np.stack(nws).reshape(5, 8, 128).transpose(2, 0, 1))
    bc8 = np.concatenate([f(inp['e_forget_b'])[0], f(inp['e_sinks'])[0], f(inp['o_router_b'])[0]])[None]
    wout = np.stack([f(inp['e_w_out'])[0], f(inp['o_w_out'])[0]])
    fg, fu, fd = f(inp['e_ffn_gate'])[0], f(inp['e_ffn_up'])[0], f(inp['e_ffn_down'])[0]
    ffc = [128 * c + ar(128) for c in range(22)]
    ffn_g = _chunks(fg, ffc)
    ffn_u = _chunks(fu, ffc)
    ffn_d = np.ascontiguousarray(fd.reshape(22, 128, 1024))
    xg, xu, xd = f(inp['o_exp_gate'])[0], f(inp['o_exp_up'])[0], f(inp['o_exp_down'])[0]
    exc = [128 * c + ar(128) for c in range(11)]
    exp_g = np.stack([_chunks(xg[e], exc) for e in range(NEXP)])
    exp_u = np.stack([_chunks(xu[e], exc) for e in range(NEXP)])
    exp_d = np.ascontiguousarray(xd.reshape(NEXP, 11, 128, 1024))
    wrt = _tm(f(inp['o_router_w'])[0], ar(8))
    p = ar(128)
    constf = np.zeros((128, 4, 128), np.float32)
    constf[:, 0] = np.eye(128)
    constf[:, 1] = (p[:, None] <= p[None, :])
    constf[:, 2] = 1.0
    constf[:, 3] = np.where(p[None, :] > p[:, None], -1.0e30, 0.0)
    constb = np.zeros((128, 3, 128), np.float32)
    constb[:, 0] = np.eye(128)
    constb[:, 1] = np.where(p[:, None] > p[None, :], NEG, 0.0)
    constb[:, 2] = np.where(p[:, None] > p[None, :], 0.0, NEG)
    small = np.zeros((128, 64), np.float32)
    half = 32
    inv_freq = (10000.0 ** (-np.arange(half, dtype=np.float32) / half)).astype(np.float32)
    small[:, 0] = inv_freq[p % 32]
    small[:, 1] = np.where((p % 64) < 32, -1.0, 1.0)
    r = p % 32
    small[:, 2] = (r == 0)
    small[:, 3] = (r == 1)
    small[:, 4] = (r == 2)
    small[:, 5] = (r >= 3) & (r < 6)
    small[:, 6] = -1.0 * (r == 3)
    small[:, 7] = -1.0 * (r == 4)
    small[:, 8] = -1.0 * (r == 5)
    small[:, 9] = (r < 3)
    for t in range(40):
        small[:, 16 + t] = 2.0 ** -(t + 1)
    shared = dict(wfm0=wfm0, wtm0f=wtm0f, wtm0s=wtm0s, wfm1=wfm1, wtm1=wtm1, ada_w=ada_w, ada_b_flat=ada_b_flat,
                  adab=adab, normw=normw, bc8=bc8, wout=wout, ffn_g=ffn_g, ffn_u=ffn_u, ffn_d=ffn_d,
                  exp_g=exp_g, exp_u=exp_u, exp_d=exp_d, wrt=wrt, constf=constf, constb=constb, small=small,
                  fnw=f(inp['final_norm'])[None])
    return shared


def per_core_inputs(inp, b0, nseq):
    x = np.ascontiguousarray(np.asarray(inp['x'], np.float32)[b0:b0 + nseq])
    pos = np.ascontiguousarray(np.asarray(inp['positions'], np.int32)[b0:b0 + nseq, None, :])
    c = np.asarray(inp['c'], np.float32)[b0:b0 + nseq]
    cT = np.ascontiguousarray(c.reshape(nseq, 8, 128).transpose(2, 1, 0))
    return dict(x=x, pos=pos, cT=cT)


def make_shapes(shared, pc, nseq):
    shapes = {}
    for k, v in list(shared.items()) + list(pc.items()):
        shapes[k] = (v.shape, I32 if v.dtype == np.int32 else F32, "ExternalInput")
    shapes['out'] = ((nseq, SEQ, D), F32, "ExternalOutput")
    return shapes


_CACHE = {}


def kernel(**inputs):
    ncores, nseq = 8, 2
    shared = host_prepare(inputs)
    maps = []
    for cix in range(ncores):
        pc = per_core_inputs(inputs, cix * nseq, nseq)
        m = dict(shared)
        m.update(pc)
        maps.append(m)
    if 'nc' not in _CACHE:
        _CACHE['nc'] = build_program(nseq, make_shapes(shared, maps[0], nseq))[0]
    res = run_bass_kernel_spmd(_CACHE['nc'], maps, core_ids=list(range(ncores)))
    out = np.concatenate([np.asarray(r['out'], np.float32) for r in res.results], axis=0)
    return out
```

```python
import math
from contextlib import ExitStack

import numpy as np
import concourse.bass as bass
import concourse.mybir as mybir
from concourse.bass_utils import run_bass_kernel_spmd

F32 = mybir.dt.float32
BF16 = mybir.dt.bfloat16
I32 = mybir.dt.int32
ALU = mybir.AluOpType
AF = mybir.ActivationFunctionType
AX = mybir.AxisListType

CELL = 512
PSUM_BANK = 2048


class Buf:
    def __init__(self, space, off, nbytes, ap):
        self.space = space
        self.off = off
        self.nbytes = nbytes
        self.ap = ap

    def cells(self, lo=None, hi=None):
        lo = self.off if lo is None else self.off + lo
        hi = self.off + self.nbytes if hi is None else self.off + hi
        g = CELL if self.space != 'psum' else PSUM_BANK
        return [(self.space, c) for c in range(lo // g, (hi - 1) // g + 1)]


class Sched:
    COMPUTE = ('pe', 'act', 'dve', 'pool')
    QUEUES = ('pe', 'act', 'dve', 'pool', 'sp')

    def __init__(self, nc):
        self.nc = nc
        self.ops = {e: [] for e in self.QUEUES}
        self.cnt = {e: 0 for e in self.COMPUTE}
        self.waited = {e: {} for e in self.QUEUES}
        self.cell = {}
        self.dma_slots = {'sp': 8, 'pool': 2, 'act': 4}
        self.dma_rr = {q: 0 for q in self.dma_slots}
        self.dma_val = {}
        self.sems = {}
        self.nops = 0

    def sem_keys(self):
        keys = list(self.COMPUTE)
        for q, n in self.dma_slots.items():
            keys += [('dma', q, i) for i in range(n)]
        return keys

    def _deps(self, eng, reads, writes):
        deps = {}

        def add(tok):
            if tok is None:
                return
            k, v = tok
            if eng == 'pe' and k == 'pe':
                return
            if deps.get(k, 0) < v:
                deps[k] = v

        for c in reads:
            st = self.cell.get(c)
            if st:
                add(st[0])
        for c in writes:
            st = self.cell.get(c)
            if st:
                add(st[0])
                for k, v in st[1].items():
                    add((k, v))
        out = []
        w = self.waited[eng]
        for k, v in deps.items():
            if w.get(k, 0) < v:
                w[k] = v
                out.append((k, v))
        return out

    def _commit(self, tok, reads, writes):
        k, v = tok
        for c in reads:
            st = self.cell.setdefault(c, [None, {}])
            if st[1].get(k, 0) < v:
                st[1][k] = v
        for c in writes:
            self.cell[c] = [tok, {}]

    @staticmethod
    def _cells(lst):
        out = []
        for b in lst:
            if isinstance(b, Buf):
                out += b.cells()
            elif isinstance(b, tuple) and isinstance(b[0], Buf):
                out += b[0].cells(b[1], b[2])
            else:
                out.append(b)
        return out

    def op(self, eng, fn, reads=(), writes=()):
        reads = self._cells(reads)
        writes = self._cells(writes)
        writes = writes + [c for c in reads if c[0] == 'psum' and c not in writes]
        waits = self._deps(eng, reads, writes)
        self.cnt[eng] += 1
        tok = (eng, self.cnt[eng])
        self.ops[eng].append((fn, waits, (eng, 1)))
        self._commit(tok, reads, writes)
        self.nops += 1
        return tok

    def dma(self, q, fn, reads=(), writes=()):
        reads = self._cells(reads)
        writes = self._cells(writes)
        slot = ('dma', q, self.dma_rr[q] % self.dma_slots[q])
        self.dma_rr[q] += 1
        waits = self._deps(q, reads, writes)
        prev = self.dma_val.get(slot, 0)
        if prev and self.waited[q].get(slot, 0) < prev:
            self.waited[q][slot] = prev
            waits.append((slot, prev))
        val = prev + 16
        self.dma_val[slot] = val
        tok = (slot, val)
        self.ops[q].append((fn, waits, (slot, 16)))
        self._commit(tok, reads, writes)
        self.nops += 1
        return tok

    def wait_all(self, q='sp'):
        waits = []
        for e in self.COMPUTE:
            if self.cnt[e] and self.waited[q].get(e, 0) < self.cnt[e]:
                waits.append((e, self.cnt[e]))
        for slot, v in self.dma_val.items():
            if self.waited[q].get(slot, 0) < v:
                waits.append((slot, v))
        self.ops[q].append((None, waits, None))

    def emit(self, block):
        sems = self.sems

        def run(engname):
            def body(engine):
                for fn, waits, inc in self.ops[engname]:
                    for k, v in waits:
                        engine.wait_ge(sems[k], v)
                    if fn is None:
                        continue
                    ins = fn(engine)
                    ins.then_inc(sems[inc[0]], inc[1])
            return body

        block.tensor(run('pe'))
        block.scalar(run('act'))
        block.vector(run('dve'))
        block.gpsimd(run('pool'))
        block.sync(run('sp'))


class Arena:
    def __init__(self, space, base_ap, nbytes):
        self.space = space
        self.base = base_ap
        self.nbytes = nbytes
        self.top = 0
        self.marks = []

    def alloc(self, shape, dtype, align=CELL):
        esz = 2 if dtype == BF16 else 4
        n = 1
        for s in shape:
            n *= s
        nb = n * esz
        off = (self.top + align - 1) // align * align
        assert off + nb <= self.nbytes, f"{self.space} arena overflow: need {off + nb} > {self.nbytes}"
        self.top = off + nb
        ap = self.base[:, off // 4:(off + nb + 3) // 4]
        if dtype != F32:
            ap = ap.bitcast(dtype)
            ap = ap[:, 0:n]
        if len(shape) > 1:
            names = ' '.join(f'a{i}' for i in range(len(shape)))
            kw = {f'a{i}': s for i, s in enumerate(shape[1:], start=1)}
            ap = ap.rearrange(f'p ({names}) -> p {names}', **kw)
        return Buf(self.space, off, nb, ap)

    def mark(self):
        self.marks.append(self.top)

    def release(self):
        self.top = self.marks.pop()


def _esz(dt):
    return 2 if dt == BF16 else 4


def bsub(b, i):
    shp = b.ap.shape
    n = 1
    for s in shp[2:]:
        n *= s
    rb = n * _esz(b.ap.dtype)
    return Buf(b.space, b.off + i * rb, rb, b.ap[:, i])


def bcols(b, lo, hi):
    shp = b.ap.shape
    n = 1
    for s in shp[2:]:
        n *= s
    rb = n * _esz(b.ap.dtype)
    return Buf(b.space, b.off + lo * rb, (hi - lo) * rb, b.ap[:, lo:hi])


D = 1024
SEQ = 2048
NT = 16
HD = 64
DFF = 2816
NEXP = 8
DFE = 1408
NEG = -30000.0


class K:
    def __init__(self, nc, S, A, P, dr, nseq):
        self.nc, self.S, self.A, self.P, self.dr, self.nseq = nc, S, A, P, dr, nseq

    def mm(self, out, lhsT, rhs, start, stop, R, W):
        self.S.op('pe', lambda e: e.matmul(out, lhsT=lhsT, rhs=rhs, start=start, stop=stop,
                                           skip_group_check=True), R, W)

    def tr(self, out, in_, ident, R, W):
        self.S.op('pe', lambda e: e.transpose(out=out, in_=in_, identity=ident), R, W)

    def act(self, out, in_, func, R, W, bias=None, scale=None, accum=None):
        kw = {}
        if bias is not None:
            kw['bias'] = bias
        if scale is not None:
            kw['scale'] = scale
        if accum is not None:
            kw['accum_out'] = accum
        self.S.op('act', lambda e: e.activation(out=out, in_=in_, func=func, **kw), R, W)

    def ts(self, eng, out, in0, s1, s2, op0, op1, R, W):
        if op1 is None:
            self.S.op(eng, lambda e: e.tensor_scalar(out=out, in0=in0, scalar1=s1, scalar2=None, op0=op0), R, W)
        else:
            self.S.op(eng, lambda e: e.tensor_scalar(out=out, in0=in0, scalar1=s1, scalar2=s2, op0=op0, op1=op1), R, W)

    def tt(self, eng, out, in0, in1, op, R, W):
        self.S.op(eng, lambda e: e.tensor_tensor(out=out, in0=in0, in1=in1, op=op), R, W)

    def stt(self, out, in0, scalar, in1, op0, op1, R, W):
        self.S.op('dve', lambda e: e.scalar_tensor_tensor(out=out, in0=in0, scalar=scalar, in1=in1, op0=op0, op1=op1), R, W)

    def cp(self, eng, out, in_, R, W):
        if eng == 'act':
            self.S.op('act', lambda e: e.copy(out=out, in_=in_), R, W)
        else:
            self.S.op(eng, lambda e: e.tensor_copy(out=out, in_=in_), R, W)

    def memset(self, eng, ap, val, W):
        self.S.op(eng, lambda e: e.memset(ap, val), (), W)

    def dma(self, q, out, in_, R, W):
        self.S.dma(q, lambda e: e.dma_start(out=out, in_=in_), R, W)

    def setup(self):
        A, dr = self.A, self.dr
        self.X = A.alloc([NT, D], F32)
        self.identb = A.alloc([128], BF16)
        self.constf = A.alloc([4, 128], F32)
        self.constb = A.alloc([3, 128], BF16)
        self.small = A.alloc([64], F32)
        self.normw = A.alloc([5, 8], F32)
        self.adab = A.alloc([2, 48], F32)
        self.bc8 = A.alloc([3, 8], F32)
        self.dma('sp', self.constf.ap, dr['constf'], (), [self.constf])
        self.dma('sp', self.small.ap, dr['small'], (), [self.small])
        self.dma('sp', self.normw.ap, dr['normw'], (), [self.normw])
        self.dma('sp', self.adab.ap, dr['adab'], (), [self.adab])
        self.dma('sp', self.bc8.ap, dr['bc8'].partition_broadcast(128), (), [self.bc8])
        self.dma('pool', self.constb.ap, dr['constb'], (), [self.constb])
        self.cp('dve', self.identb.ap, self.constf.ap[:, 0], [self.constf], [self.identb])
        self.esink = A.alloc([8], F32)
        self.negone = A.alloc([8], F32, align=64)
        self.memset('pool', self.negone.ap, -1.0, [self.negone])
        self.act(self.esink.ap, self.bc8.ap[:, 1], AF.Exp, [self.bc8], [self.esink])
        self.modT = A.alloc([self.nseq, 2, 6, 8], F32)
        self.gate = A.alloc([1, D], F32)
        self.cT = A.alloc([8, self.nseq], F32)
        self.scb = A.alloc([8, self.nseq], BF16)
        self.Dg = [A.alloc([128], F32) for _ in range(2)]
        self.stat = A.alloc([2, NT], F32)
        self.cosT = None

    def ada(self, l, part):
        A, dr = self.A, self.dr
        ns = self.nseq
        A.mark()
        if l == 0 and part == 0:
            self.dma('sp', self.cT.ap, dr['cT'], (), [self.cT])
            sg = A.alloc([8, ns], F32)
            self.act(sg.ap, self.cT.ap, AF.Silu, [self.cT], [sg])
            self.cp('dve', self.scb.ap, sg.ap, [sg], [self.scb])
        wst = [A.alloc([8, 8, 128], BF16) for _ in range(2)]
        ps1 = self.pj[0]
        for m in range(3 * part, 3 * part + 3):
            w = wst[m % 2]
            self.dma('pool', w.ap, dr['ada_w'][l, m], (), [w])
            for j in range(8):
                for kc in range(8):
                    self.mm(ps1.ap[:, j * ns:(j + 1) * ns], w.ap[:, j, kc], self.scb.ap[:, kc], kc == 0, kc == 7,
                            [w, self.scb], [ps1])
            pv = ps1.ap[:, 0:8 * ns].rearrange('p (j q) -> p q j', q=ns)
            for q in range(ns):
                self.tt('dve', self.modT.ap[:, q, l, m], pv[:, q], self.adab.ap[:, l, m * 8:(m + 1) * 8], ALU.add,
                        [ps1, self.adab], [self.modT])
                if m in (1, 4):
                    nw = self.normw.ap[:, 2 * l + (0 if m == 1 else 1)]
                    self.stt(self.modT.ap[:, q, l, m], self.modT.ap[:, q, l, m], 1.0, nw, ALU.add, ALU.mult,
                             [self.modT, self.normw], [self.modT])
        A.release()

    def expand_gate(self, s, l, m):
        ones = self.constf.ap[:, 2]
        identf = self.constf.ap[:, 0]
        for j in range(8):
            dg = self.Dg[j % 2]
            self.ts('dve', dg.ap, identf, self.modT.ap[:, s, l, m, j:j + 1], None, ALU.mult, None, [self.constf, self.modT], [dg])
            pb = self.pj[j // 4]
            self.mm(pb.ap[:, (j % 4) * 128:(j % 4 + 1) * 128], ones, dg.ap, True, True, [self.constf, dg], [pb])
        for hf in range(2):
            self.cp('act', self.gate.ap[:, 0, hf * 512:(hf + 1) * 512], self.pj[hf].ap, [self.pj[hf]], [self.gate])

    def rstd(self, t0, n, sq):
        X = self.X
        st = self.stat
        for t in range(t0, t0 + n):
            self.act(sq.ap, X.ap[:, t], AF.Square, [bsub(X, t)], [sq, st], accum=st.ap[:, 0, t:t + 1])
        self.ts('dve', st.ap[:, 1, t0:t0 + n], st.ap[:, 0, t0:t0 + n], 1.0 / D, 1e-6, ALU.mult, ALU.add, [st], [st])
        self.act(st.ap[:, 1, t0:t0 + n], st.ap[:, 1, t0:t0 + n], AF.Sqrt, [st], [st])
        self.S.op('dve', lambda e: e.reciprocal(out=st.ap[:, 1, t0:t0 + n], in_=st.ap[:, 1, t0:t0 + n]),
                  self.S._cells([st]), self.S._cells([st]))

    def norm_hT2(self, l, which, hT):
        A, P = self.A, self.P
        A.mark()
        sq = A.alloc([D], F32)
        xn = A.alloc([4, D], BF16)
        pt = self.ptb
        X = self.X
        msh = self.modT.ap[:, self.cur_s, l, 0 if which == 0 else 3]
        msc = self.modT.ap[:, self.cur_s, l, 1 if which == 0 else 4]
        for c in range(4):
            self.rstd(c * 4, 4, sq)
            for tl in range(4):
                t = c * 4 + tl
                self.ts('dve', xn.ap[:, tl], X.ap[:, t], self.stat.ap[:, 1, t:t + 1], None, ALU.mult, None,
                        [bsub(X, t), self.stat], [bsub(xn, tl)])
            for kc in range(8):
                pb = pt[kc % 2]
                for tl in range(4):
                    self.tr(pb.ap[:, tl * 128:(tl + 1) * 128], xn.ap[:, tl, kc * 128:(kc + 1) * 128], self.identb.ap,
                            [bsub(xn, tl), self.identb], [pb])
                if kc % 2 == 0:
                    self.act(hT.ap[:, kc, c * 512:(c + 1) * 512], pb.ap, AF.Identity, [pb, self.modT],
                             [(hT, (kc * SEQ + c * 512) * 2, (kc * SEQ + c * 512 + 512) * 2)],
                             bias=msh[:, kc:kc + 1], scale=msc[:, kc:kc + 1])
                else:
                    self.ts('dve', hT.ap[:, kc, c * 512:(c + 1) * 512], pb.ap, msc[:, kc:kc + 1], msh[:, kc:kc + 1], ALU.mult, ALU.add,
                            [pb, self.modT], [(hT, (kc * SEQ + c * 512) * 2, (kc * SEQ + c * 512 + 512) * 2)])
        A.release()

    def rope_tables(self, s, cosT, sinT):
        A = self.A
        A.mark()
        HS = SEQ // 2
        pi_ = A.alloc([HS], F32)
        ang = A.alloc([HS], F32)
        kf = A.alloc([HS], F32)
        pii = pi_.ap.bitcast(I32)
        sm = self.small
        C1 = 6.28125
        C2 = 2 * math.pi - C1

        def wrap(buf):
            self.ts('dve', kf.ap, buf.ap, math.pi, -2 * math.pi, ALU.is_gt, ALU.mult, [buf], [kf])
            self.tt('dve', buf.ap, buf.ap, kf.ap, ALU.add, [buf, kf], [buf])
            self.ts('dve', kf.ap, buf.ap, -math.pi, 2 * math.pi, ALU.is_lt, ALU.mult, [buf], [kf])
            self.tt('dve', buf.ap, buf.ap, kf.ap, ALU.add, [buf, kf], [buf])

        for hb in range(2):
            sl = slice(hb * HS, (hb + 1) * HS)
            self.dma('sp', pii, self.dr['pos'][s][:, sl].partition_broadcast(128), (), [pi_])
            self.cp('dve', ang.ap, pii, [pi_], [ang])
            self.ts('dve', ang.ap, ang.ap, sm.ap[:, 0:1], None, ALU.mult, None, [ang, sm], [ang])
            self.ts('dve', kf.ap, ang.ap, 1.0 / (2 * math.pi), None, ALU.mult, None, [ang], [kf])
            self.cp('dve', pii, kf.ap, [kf], [pi_])
            self.cp('dve', kf.ap, pii, [pi_], [kf])
            self.stt(ang.ap, kf.ap, -C1, ang.ap, ALU.mult, ALU.add, [kf, ang], [ang])
            self.stt(ang.ap, kf.ap, -C2, ang.ap, ALU.mult, ALU.add, [kf, ang], [ang])
            wrap(ang)
            self.act(sinT.ap[:, sl], ang.ap, AF.Sin, [ang, sm], [sinT], scale=sm.ap[:, 1:2])
            self.ts('dve', ang.ap, ang.ap, math.pi / 2, None, ALU.add, None, [ang], [ang])
            wrap(ang)
            self.act(cosT.ap[:, sl], ang.ap, AF.Sin, [ang], [cosT])
        A.release()

    @staticmethod
    def hT_rng(hT, t0, t1):
        return [(hT, (kc * SEQ + t0) * 2, (kc * SEQ + t1) * 2) for kc in range(8)]

    def proj_fm(self, hT, wsrc, specs, pbanks):
        A = self.A
        A.mark()
        wst = [A.alloc([2, 8, 128], BF16) for _ in range(2)]
        t1 = [A.alloc([512], F32) for _ in range(1)]
        t2 = [A.alloc([512], F32) for _ in range(1)]
        n = 0
        for si, (kind, cids, dest, extra) in enumerate(specs):
            w = wst[si % 2]
            for ci, cid in enumerate(cids):
                self.dma('pool', w.ap[:, ci], wsrc[cid], (), [bsub(w, ci)])
            for c in range(4):
                pss = []
                for ci in range(len(cids)):
                    pb = pbanks[n % len(pbanks)]
                    n += 1
                    for kc in range(8):
                        self.mm(pb.ap, w.ap[:, ci, kc], hT.ap[:, kc, c * 512:(c + 1) * 512], kc == 0, kc == 7,
                                [bsub(w, ci)] + self.hT_rng(hT, c * 512, (c + 1) * 512), [pb])
                    pss.append(pb)
                dbuf, dap = dest(c)
                if kind == 'plain':
                    self.act(dap, pss[0].ap, AF.Copy, [pss[0]], [dbuf], scale=float(extra))
                else:
                    cosT, sinT = extra
                    a, b = t1[0], t2[0]
                    self.tt('dve', a.ap, pss[0].ap, cosT.ap[:, c * 512:(c + 1) * 512], ALU.mult, [pss[0], cosT], [a])
                    self.tt('dve', b.ap, pss[1].ap, sinT.ap[:, c * 512:(c + 1) * 512], ALU.mult, [pss[1], sinT], [b])
                    if len(dap.shape) == 3:
                        self.tt('pool', dap, a.ap.rearrange('p (t q) -> p t q', q=128), b.ap.rearrange('p (t q) -> p t q', q=128),
                                ALU.add, [a, b], [dbuf])
                    else:
                        self.tt('pool', dap, a.ap, b.ap, ALU.add, [a, b], [dbuf])
        A.release()

    def proj_tm(self, hT, wsrc, ncols, pbanks, evac):
        A = self.A
        A.mark()
        w = A.alloc([8, ncols], BF16)
        self.dma('pool', w.ap, wsrc, (), [w])
        for t in range(NT):
            pb = pbanks[t % len(pbanks)]
            for kc in range(8):
                self.mm(pb.ap[:, 0:ncols], hT.ap[:, kc, t * 128:(t + 1) * 128], w.ap[:, kc], kc == 0, kc == 7,
                        [w] + self.hT_rng(hT, t * 128, (t + 1) * 128), [pb])
            evac(t, pb)
        A.release()

    def attn_group(self, i, jlist, heads, maskfn, scale, pS, pO, PT, obuf, ocol0, sink=None, osb=None, qbatch=None):
        G = len(heads)
        nJ = len(jlist)

        def scores(jn):
            j = jlist[jn]
            ps = pS[jn % len(pS)]
            m = maskfn(j)
            started = False
            if qbatch is not None:
                out3 = ps.ap[:, 0:G * 128].rearrange('p (g q) -> p g q', q=128)
                self.mm(out3, self.identb.ap, m[1].unsqueeze(1).to_broadcast([128, G, 128]), True, False,
                        [self.identb, m[0]], [ps])
                kb_, ka_ = heads[0]['k']
                self.mm(out3, ka_[:, j * 128:(j + 1) * 128], qbatch[1], False, True, [kb_, qbatch[0]], [ps])
                return
            for hh, h in enumerate(heads):
                cols = ps.ap[:, hh * 128:(hh + 1) * 128]
                if m is not None:
                    self.mm(cols, self.identb.ap, m[1], not started, False, [self.identb, m[0]], [ps])
                    started = True
                qb, qa = h['q']
                kb, ka = h['k']
                self.mm(cols, ka[:, j * 128:(j + 1) * 128], qa[:, i * 128:(i + 1) * 128], not started,
                        h['bias'] is None, [kb, qb], [ps])
                started = True
                if h['bias'] is not None:
                    fkb, fka, fqb, fqa = h['bias']
                    self.mm(cols, fka[:, j * 128:(j + 1) * 128], fqa[:, i * 128:(i + 1) * 128], False, True,
                            [fkb, fqb], [ps])

        def exp_pv(jn):
            j = jlist[jn]
            ps = pS[jn % len(pS)]
            pt = PT[jn % len(PT)]
            self.act(pt.ap[:, 0:G * 128], ps.ap[:, 0:G * 128], AF.Exp, [ps], [pt], scale=float(scale))
            for hh, h in enumerate(heads):
                vb, va = h['v'](j)
                self.mm(pO.ap[:, hh * 65:(hh + 1) * 65], pt.ap[:, hh * 128:(hh + 1) * 128], va[:, 0:65],
                        jn == 0 and hh == 0, jn == nJ - 1, [pt, vb], [pO])

        scores(0)
        for jn in range(nJ):
            if jn + 1 < nJ:
                scores(jn + 1)
            exp_pv(jn)
        if osb is not None:
            ob = osb.ap[:, 0:G * 65]
            self.cp('act', ob, pO.ap[:, 0:G * 65], [pO], [osb])
            o3 = ob.rearrange('p (g d) -> p g d', d=65)
            od = obuf.ap[:, i, ocol0:ocol0 + G * 64].rearrange('p (g d) -> p g d', d=64)
            self.tt('pool', o3[:, :, 64], o3[:, :, 64], self.negone.ap[:, 0:G], ALU.pow, [osb, self.negone], [osb])
            self.tt('pool', od, o3[:, :, 0:64], o3[:, :, 64:65].to_broadcast([128, G, 64]), ALU.mult,
                    [osb], [(obuf, (i * D + ocol0) * 2, (i * D + ocol0 + G * 64) * 2)])
            return
        A = self.A
        A.mark()
        den = A.alloc([G], F32, align=64)
        ov = pO.ap[:, 0:G * 65].rearrange('p (g d) -> p g d', d=65)
        if sink is not None:
            self.tt('dve', den.ap, ov[:, :, 64], sink, ALU.add, [pO, self.esink], [den])
        else:
            self.cp('dve', den.ap, ov[:, :, 64], [pO], [den])
        self.S.op('dve', lambda e: e.reciprocal(out=den.ap, in_=den.ap), self.S._cells([den]), self.S._cells([den]))
        od = obuf.ap[:, i, ocol0:ocol0 + G * 64].rearrange('p (g d) -> p g d', d=64)
        self.tt('dve', od, ov[:, :, 0:64], den.ap.unsqueeze(2).to_broadcast([128, G, 64]), ALU.mult,
                [pO, den], [(obuf, (i * D + ocol0) * 2, (i * D + ocol0 + G * 64) * 2)])
        A.release()

    def fox_attn(self, p, qT, kT, V, FQ, FK, obuf, PT):
        pj, pO = self.pj, self.pO
        tri = self.constb.ap[:, 1]
        A = self.A
        rn = 0
        for hh in range(2):
            qa, ka = qT.ap[64 * hh:64 * hh + 64], kT.ap[64 * hh:64 * hh + 64]
            fqa, fka = FQ.ap[32 * hh:32 * hh + 6], FK.ap[32 * hh:32 * hh + 6]
            for c in range(4):
                po = pO[(2 * hh + c) % 2]
                nJ = 4 * c + 4

                def geom(j):
                    q0 = max(j, 4 * c)
                    off = (q0 - 4 * c) * 128
                    return q0, off, (4 * c + 4 - q0) * 128

                def scores(j, rn):
                    ps = pj[rn % 4]
                    q0, off, ncol = geom(j)
                    cols = ps.ap[:, off:off + ncol]
                    qs = slice(q0 * 128, (4 * c + 4) * 128)
                    self.mm(cols, ka[:, j * 128:(j + 1) * 128], qa[:, qs], True, False, [kT, qT], [ps])
                    diag = j >= 4 * c
                    self.mm(cols, fka[:, j * 128:(j + 1) * 128], fqa[:, qs], False, not diag, [FK, FQ], [ps])
                    if diag:
                        self.mm(ps.ap[:, off:off + 128], self.identb.ap, tri, False, True, [self.identb, self.constb], [ps])

                def exp_pv(j, rn):
                    ps = pj[rn % 4]
                    pt = PT[rn % 2]
                    q0, off, ncol = geom(j)
                    self.act(pt.ap[:, off:off + ncol], ps.ap[:, off:off + ncol], AF.Exp, [ps], [pt])
                    for t in range(q0, 4 * c + 4):
                        tl = t - 4 * c
                        self.mm(po.ap[:, tl * 65:(tl + 1) * 65], pt.ap[:, tl * 128:(tl + 1) * 128], V.ap[:, j, hh, 0:65],
                                j == 0 and tl == 0, j == t, [pt, bsub(V, j)], [po])

                scores(0, rn)
                for j in range(nJ):
                    if j + 1 < nJ:
                        scores(j + 1, rn + j + 1)
                    exp_pv(j, rn + j)
                rn += nJ
                A.mark()
                den = A.alloc([4], F32, align=64)
                ov = po.ap[:, 0:260].rearrange('p (g d) -> p g d', d=65)
                self.cp('dve', den.ap, ov[:, :, 64], [po], [den])
                self.S.op('dve', lambda e, den=den: e.reciprocal(out=den.ap, in_=den.ap), self.S._cells([den]), self.S._cells([den]))
                col0 = 128 * p + 64 * hh
                od = obuf.ap[:, 4 * c:4 * c + 4, col0:col0 + 64]
                self.tt('dve', od, ov[:, :, 0:64], den.ap.unsqueeze(2).to_broadcast([128, 4, 64]), ALU.mult,
                        [po, den], [(obuf, 4 * c * D * 2, (4 * c + 4) * D * 2)])
                A.release()

    def out_proj(self, l, obuf, pbanks, ptb):
        A = self.A
        A.mark()
        w = A.alloc([8, D], BF16)
        self.dma('pool', w.ap, self.dr['wout'][l].rearrange('(kc p) n -> p kc n', p=128), (), [w])
        oT = [A.alloc([8, 128], BF16) for _ in range(2)]
        tmp = [A.alloc([512], F32) for _ in range(2)]
        n = 0
        for t in range(NT):
            o_t = oT[t % 2]
            for k4 in range(2):
                pb = ptb[k4]
                for kk in range(4):
                    kc = 4 * k4 + kk
                    self.tr(pb.ap[:, kk * 128:(kk + 1) * 128], obuf.ap[:, t, kc * 128:(kc + 1) * 128], self.identb.ap,
                            [(obuf, t * D * 2, (t + 1) * D * 2), self.identb], [pb])
                self.cp('act', o_t.ap[:, 4 * k4:4 * k4 + 4].rearrange('p a b -> p (a b)'), pb.ap, [pb],
                        [(o_t, 4 * k4 * 256, (4 * k4 + 4) * 256)])
            for hf in range(2):
                pb = pbanks[n % len(pbanks)]
                tb = tmp[n % 2]
                n += 1
                for kc in range(8):
                    self.mm(pb.ap, o_t.ap[:, kc], w.ap[:, kc, hf * 512:(hf + 1) * 512], kc == 0, kc == 7, [o_t, w], [pb])
                self.tt('dve', tb.ap, pb.ap, self.gate.ap[:, 0, hf * 512:(hf + 1) * 512], ALU.mult, [pb, bsub(self.gate, 0)], [tb])
                xs = (self.X, (t * D + hf * 512) * 4, (t * D + hf * 512 + 512) * 4)
                xa = self.X.ap[:, t, hf * 512:(hf + 1) * 512]
                self.tt('pool', xa, xa, tb.ap, ALU.add, [xs, tb], [xs])
        A.release()

    def ffn_load(self, gsrc, usrc, dsrc, nf, wbuf):
        wg, wu, wd = wbuf
        self.dma('pool', wg.ap[:, 0:nf], gsrc.rearrange('f p k n -> p f k n'), (), [wg])
        self.dma('pool', wu.ap[:, 0:nf], usrc.rearrange('f p k n -> p f k n'), (), [wu])
        self.dma('pool', wd.ap[:, 0:nf], dsrc.rearrange('f p n -> p f n'), (), [wd])

    def ffn_gu(self, hT, nf, wbuf, gT, pbanks, tmp, c):
        wg, wu, wd = wbuf
        for f in range(nf):
            pg = pbanks[self.fn % len(pbanks)]
            pu = pbanks[(self.fn + 1) % len(pbanks)]
            self.fn += 2
            for kc in range(8):
                self.mm(pg.ap, wg.ap[:, f, kc], hT.ap[:, kc, c * 512:(c + 1) * 512], kc == 0, kc == 7,
                        [wg] + self.hT_rng(hT, c * 512, (c + 1) * 512), [pg])
            for kc in range(8):
                self.mm(pu.ap, wu.ap[:, f, kc], hT.ap[:, kc, c * 512:(c + 1) * 512], kc == 0, kc == 7,
                        [wu] + self.hT_rng(hT, c * 512, (c + 1) * 512), [pu])
            sg = tmp[2 + f % 2]
            sgb = sg.ap.bitcast(BF16)[:, 0:512]
            self.act(sgb, pg.ap, AF.Silu, [pg], [sg])
            self.tt('dve', gT.ap[:, f], pu.ap, sgb, ALU.mult, [pu, sg], [bsub(gT, f)])

    def ffn_down(self, nf, wbuf, gT, pbanks, tmp, c, comb):
        wg, wu, wd = wbuf
        for tl in range(4):
            t = c * 4 + tl
            for hf in range(2):
                pb = pbanks[self.fn % len(pbanks)]
                tb = tmp[self.fn % 2]
                self.fn += 1
                for f in range(nf):
                    self.mm(pb.ap, gT.ap[:, f, tl * 128:(tl + 1) * 128], wd.ap[:, f, hf * 512:(hf + 1) * 512],
                            f == 0, f == nf - 1, [gT, wd], [pb])
                gap = self.gate.ap[:, 0, hf * 512:(hf + 1) * 512]
                if comb is None:
                    self.tt('dve', tb.ap, pb.ap, gap, ALU.mult, [pb, bsub(self.gate, 0)], [tb])
                else:
                    cb, cap = comb(t)
                    self.stt(tb.ap, pb.ap, cap, gap, ALU.mult, ALU.mult, [pb, bsub(self.gate, 0), cb], [tb])
                xs = (self.X, (t * D + hf * 512) * 4, (t * D + hf * 512 + 512) * 4)
                xa = self.X.ap[:, t, hf * 512:(hf + 1) * 512]
                self.tt('pool', xa, xa, tb.ap, ALU.add, [xs, tb], [xs])

    def layer0_mixer(self, s):
        A, dr = self.A, self.dr
        pj, pO, ptb = self.pj, self.pO, self.ptb
        sm = self.small
        A.mark()
        hT = A.alloc([8, SEQ], BF16)
        obuf = A.alloc([NT, D], BF16)
        self.norm_hT2(0, 0, hT)
        if self.lvl < 0.45:
            return
        PT = [A.alloc([512], BF16) for _ in range(2)]
        tri = (self.constb, self.constb.ap[:, 1])
        prevb = (self.constb, self.constb.ap[:, 2])
        for p in range(4):
            A.mark()
            qT = A.alloc([SEQ], BF16)
            kT = A.alloc([SEQ], BF16)
            V = A.alloc([NT, 2, 66], BF16)
            FQ = A.alloc([SEQ], BF16)
            FK = A.alloc([SEQ], BF16)
            z = A.alloc([NT, 2], F32)
            self.memset('pool', V.ap, 1.0, [V])
            if self.lvl < 0.455:
                return
            self.proj_fm(hT, dr['wfm0'], [('plain', (p,), lambda c, b=qT: (b, b.ap[:, c * 512:(c + 1) * 512]), 0.125),
                                          ('plain', (4 + p,), lambda c, b=kT: (b, b.ap[:, c * 512:(c + 1) * 512]), 1.0)], pj)

            if self.lvl < 0.47:
                return

            def evac(t, pb, V=V, z=z, p=p):
                if self.lvl < 0.49:
                    return
                if self.lvl != 0.494:
                    self.cp('act', V.ap[:, t, :, 0:64], pb.ap[:, 0:128].rearrange('p (h d) -> p h d', d=64), [pb], [bsub(V, t)])
                if self.lvl != 0.492:
                    self.tt('dve', z.ap[:, t], pb.ap[:, 128:130], self.bc8.ap[:, 0, 2 * p:2 * p + 2], ALU.add, [pb, self.bc8], [z])
            self.proj_tm(hT, dr['wtm0f'][p], 130, pj, evac)
            if self.lvl < 0.55:
                return
            A.mark()
            sp = A.alloc([NT, 2], F32)
            Lrep = A.alloc([NT, 64], F32)
            C = A.alloc([SEQ], F32)
            Hb = A.alloc([SEQ], BF16)
            Mb = A.alloc([SEQ], BF16)
            Lb = A.alloc([SEQ], BF16)
            self.act(sp.ap, z.ap, AF.Exp, [z], [sp], scale=-1.0)
            self.act(sp.ap, sp.ap, AF.Ln, [sp], [sp], bias=1.0)
            self.memset('pool', Lrep.ap, 0.0, [Lrep])
            for t in range(NT):
                for sl in range(2):
                    self.ts('dve', Lrep.ap[:, t, 32 * sl:32 * sl + 6], sp.ap[:, t, sl:sl + 1].to_broadcast([128, 6]), -1.0, None,
                            ALU.mult, None, [sp], [bsub(Lrep, t)])
            U = self.constf.ap[:, 1]
            for t in range(NT):
                pb = pj[t % 4]
                self.mm(pb.ap[0:64, 0:128], Lrep.ap[:, t], U, True, True, [bsub(Lrep, t), self.constf], [pb])
                cs = (C, t * 512, (t + 1) * 512)
                if t == 0:
                    self.cp('dve', C.ap[0:64, 0:128], pb.ap[0:64, 0:128], [pb], [cs])
                else:
                    self.ts('dve', C.ap[0:64, t * 128:(t + 1) * 128], pb.ap[0:64, 0:128], C.ap[0:64, t * 128 - 1:t * 128], None,
                            ALU.add, None, [pb, (C, (t - 1) * 512, t * 512)], [cs])
            c64, h64, m64, l64 = C.ap[0:64], Hb.ap[0:64], Mb.ap[0:64], Lb.ap[0:64]
            self.cp('dve', h64, c64, [C], [Hb])
            self.tt('dve', c64, c64, h64, ALU.subtract, [C, Hb], [C])
            self.cp('dve', m64, c64, [C], [Mb])
            self.tt('dve', c64, c64, m64, ALU.subtract, [C, Mb], [C])
            self.cp('dve', l64, c64, [C], [Lb])
            for (dst, c0) in ((FQ, 2), (FK, 6)):
                s64 = sm.ap[0:64]
                f64 = dst.ap[0:64]
                self.ts('dve', f64, h64, s64[:, c0:c0 + 1], s64[:, c0 + 3:c0 + 4], ALU.mult, ALU.add, [Hb, sm], [dst])
                self.stt(f64, m64, s64[:, c0 + 1:c0 + 2], f64, ALU.mult, ALU.add, [Mb, sm, dst], [dst])
                self.stt(f64, l64, s64[:, c0 + 2:c0 + 3], f64, ALU.mult, ALU.add, [Lb, sm, dst], [dst])
            A.release()
            if self.lvl < 0.65:
                return
            self.fox_attn(p, qT, kT, V, FQ, FK, obuf, PT)
            A.release()
        if self.lvl < 0.75:
            return
        A.mark()
        cosT = A.alloc([SEQ], F32)
        sinT = A.alloc([SEQ], F32)
        self.rope_tables(s, cosT, sinT)
        sqT = A.alloc([NT, 4, 128], BF16)
        skT = A.alloc([SEQ], BF16)
        Vs = A.alloc([NT, 2, 66], BF16)
        self.memset('pool', Vs.ap, 1.0, [Vs])
        specs = [('rope', (8 + c, 12 + c), (lambda cc, c=c: (sqT, sqT.ap[:, 4 * cc:4 * cc + 4, c, :])), (cosT, sinT))
                 for c in range(4)]
        specs.append(('rope', (16, 17), (lambda cc: (skT, skT.ap[:, cc * 512:(cc + 1) * 512])), (cosT, sinT)))
        self.proj_fm(hT, dr['wfm0'], specs, pj)

        def evac_s(t, pb):
            self.cp('act', Vs.ap[:, t, :, 0:64], pb.ap[:, 0:128].rearrange('p (h d) -> p h d', d=64), [pb], [bsub(Vs, t)])
        self.proj_tm(hT, dr['wtm0s'], 128, pj, evac_s)
        for g in range(2):
            heads = []
            for c in range(4):
                heads.append(dict(q=None, k=(skT, skT.ap[64 * g:64 * g + 64]),
                                  v=(lambda j, g=g: (bsub(Vs, j), Vs.ap[:, j, g])), bias=None))
            for i in range(NT):
                jl = [i - 1, i] if i > 0 else [i]
                self.attn_group(i, jl, heads, (lambda j, i=i: tri if j == i else prevb), 0.125,
                                pj, pO[i % 2], PT, obuf, 512 + 256 * g, sink=self.esink.ap[:, 4 * g:4 * g + 4],
                                qbatch=(bsub(sqT, i), sqT.ap[64 * g:64 * g + 64, i]))
        A.release()
        if self.lvl < 0.85:
            return
        self.out_proj(0, obuf, pj, ptb)
        A.release()

    def run_units(self, hT, units, wb, gTs, tmp):
        self.fn = 0
        g, u_, d, nf, cb = units[0]
        self.ffn_load(g, u_, d, nf, wb[0])
        steps = [(ui, c) for ui in range(len(units)) for c in range(4)]
        for k, (ui, c) in enumerate(steps):
            if c == 1 and ui + 1 < len(units):
                g2, u2, d2, nf2, _ = units[ui + 1]
                self.ffn_load(g2, u2, d2, nf2, wb[(ui + 1) % 2])
            self.ffn_gu(hT, units[ui][3], wb[ui % 2], gTs[k % 2], self.pj, tmp, c)
            if k >= 1:
                pu_, pc_ = steps[k - 1]
                self.ffn_down(units[pu_][3], wb[pu_ % 2], gTs[(k - 1) % 2], self.pj, tmp, pc_, units[pu_][4])
        pu_, pc_ = steps[-1]
        self.ffn_down(units[pu_][3], wb[pu_ % 2], gTs[(len(steps) - 1) % 2], self.pj, tmp, pc_, units[pu_][4])

    def alloc_ffn(self, l):
        A = self.A
        hT = A.alloc([8, SEQ], BF16)
        self.norm_hT2(l, 1, hT)
        wb = [(A.alloc([6, 8, 128], BF16), A.alloc([6, 8, 128], BF16), A.alloc([6, D], BF16)) for _ in range(2)]
        gT = [A.alloc([6, 512], BF16) for _ in range(2)]
        tmp = [A.alloc([512], F32) for _ in range(4)]
        return hT, wb, gT, tmp

    def layer0_ffn(self, s):
        A, dr = self.A, self.dr
        A.mark()
        hT, wb, gT, tmp = self.alloc_ffn(0)
        units = []
        f0 = 0
        for nf in (6, 6, 5, 5):
            units.append((dr['ffn_g'][f0:f0 + nf], dr['ffn_u'][f0:f0 + nf], dr['ffn_d'][f0:f0 + nf], nf, None))
            f0 += nf
        self.run_units(hT, units, wb, gT, tmp)
        A.release()

    def layer1_mixer(self, s):
        A, dr = self.A, self.dr
        pj, pO, ptb = self.pj, self.pO, self.ptb
        A.mark()
        hT = A.alloc([8, SEQ], BF16)
        obuf = Buf(hT.space, hT.off, hT.nbytes,
                   hT.ap.rearrange('p a b -> p (a b)').rearrange('p (t d) -> p t d', d=D))
        A.mark()
        qT = A.alloc([2, NT, 4, 128], BF16)
        kT = A.alloc([2, SEQ], BF16)
        V = A.alloc([NT, 4, 66], BF16)
        iqT = A.alloc([4, SEQ], BF16)
        ikT = A.alloc([SEQ], BF16)
        iw = A.alloc([NT, 8], F32)
        A.mark()
        cosT = A.alloc([SEQ], BF16)
        sinT = A.alloc([SEQ], BF16)
        self.rope_tables(s, cosT, sinT)
        self.norm_hT2(1, 0, hT)
        self.memset('pool', V.ap, 1.0, [V])
        specs = []
        for c in range(8):
            specs.append(('rope', (c, 8 + c), (lambda cc, c=c: (bsub(qT, c // 4), qT.ap[:, c // 4, 4 * cc:4 * cc + 4, c % 4, :])), (cosT, sinT)))
        for c in range(2):
            specs.append(('rope', (16 + c, 18 + c), (lambda cc, c=c: (bsub(kT, c), kT.ap[:, c, cc * 512:(cc + 1) * 512])), (cosT, sinT)))
        for c in range(4):
            specs.append(('rope', (20 + c, 24 + c), (lambda cc, c=c: (bsub(iqT, c), iqT.ap[:, c, cc * 512:(cc + 1) * 512])), (cosT, sinT)))
        specs.append(('rope', (28, 29), (lambda cc: (ikT, ikT.ap[:, cc * 512:(cc + 1) * 512])), (cosT, sinT)))
        self.proj_fm(hT, dr['wfm1'], specs, pj)

        def evac(t, pb):
            self.cp('act', V.ap[:, t, :, 0:64], pb.ap[:, 0:256].rearrange('p (h d) -> p h d', d=64), [pb], [bsub(V, t)])
            self.cp('dve', iw.ap[:, t], pb.ap[:, 256:264], [pb], [iw])
        self.proj_tm(hT, dr['wtm1'], 264, pj, evac)
        A.release()
        sc = A.alloc([SEQ], F32)
        mb = A.alloc([SEQ], BF16)
        mbT = A.alloc([SEQ], BF16)
        bs = A.alloc([8], F32, align=64)
        TB = 25
        steps = A.alloc([TB + 1], F32, align=64)
        mids = A.alloc([TB + 1], F32, align=64)
        G = A.alloc([TB], F32, align=64)
        cand = A.alloc([TB], F32, align=64)
        rr = [A.alloc([512], F32) for _ in range(2)]
        Dm = A.alloc([8, 128], F32)
        PT = [A.alloc([512], BF16) for _ in range(2)]
        trineg = self.constf.ap[:, 3]
        osb = [A.alloc([260], F32, align=64) for _ in range(1)] * 2
        cnt = [0]

        def sc_of(t):
            if t % 2 == 1:
                nb = (t + 1) * 512
                ap = obuf.ap.rearrange('p t d -> p (t d)')[:, 0:nb // 2].bitcast(F32)
                return Buf(obuf.space, obuf.off, nb, ap)
            return sc

        ptbf = Buf('psum', ptb[1].off, 2048, self.P.base[:, ptb[1].off // 4:ptb[1].off // 4 + 512])
        pS_att = [pj[3], ptbf]
        identf = self.constf.ap[:, 0]
        lb = [pj[0], pj[1]]
        accb = pj[2]

        def indexer(i):
            L = (i + 1) * 128
            scb_ = sc_of(i)
            for h in range(8):
                self.ts('pool', Dm.ap[:, h], identf, iw.ap[:, i, h:h + 1], None, ALU.mult, None, [self.constf, iw], [bsub(Dm, h)])
            for c4 in range((L + 511) // 512):
                nc_ = min(512, L - 512 * c4)
                scs = (scb_, c4 * 2048, c4 * 2048 + nc_ * 4)
                sca = scb_.ap[:, c4 * 512:c4 * 512 + nc_]
                base = cnt[0]

                def logits(hi):
                    ps = lb[(base + hi) % 2]
                    hf = hi % 2
                    self.mm(ps.ap[:, 0:nc_], iqT.ap[64 * hf:64 * hf + 64, hi // 2, i * 128:(i + 1) * 128],
                            ikT.ap[64 * hf:64 * hf + 64, c4 * 512:c4 * 512 + nc_], True, True, [bsub(iqT, hi // 2), ikT], [ps])

                logits(0)
                for hi in range(8):
                    if hi + 1 < 8:
                        logits(hi + 1)
                    ps = lb[(base + hi) % 2]
                    r = rr[(base + hi) % 2]
                    self.act(r.ap[:, 0:nc_], ps.ap[:, 0:nc_], AF.Relu, [ps], [r])
                    self.mm(accb.ap[:, 0:nc_], Dm.ap[:, hi], r.ap[:, 0:nc_], hi == 0, hi == 7, [bsub(Dm, hi), r], [accb])
                cnt[0] += 8
                self.cp('act', sca, accb.ap[:, 0:nc_], [accb], [scs])

        def topk(i):
            L = (i + 1) * 128
            scb_ = sc_of(i)
            dg = (scb_, i * 512, (i + 1) * 512)
            scl = (scb_, 0, L * 4)
            mbl = (mb, 0, L * 2)
            sca_ = scb_.ap[:, 0:L]
            if i >= 2:
                self.S.op('dve', lambda e: e.tensor_reduce(out=bs.ap[:, 0:1], in_=sca_, axis=AX.X, op=ALU.min),
                          self.S._cells([scl]), self.S._cells([bs]))
            self.tt('pool', scb_.ap[:, i * 128:(i + 1) * 128], scb_.ap[:, i * 128:(i + 1) * 128], trineg, ALU.add, [dg, self.constf], [dg])
            if i >= 2:
                self.S.op('dve', lambda e: e.tensor_reduce(out=bs.ap[:, 1:2], in_=sca_, axis=AX.X, op=ALU.max),
                          self.S._cells([scl]), self.S._cells([bs]))
                self.tt('dve', bs.ap[:, 2:3], bs.ap[:, 1:2], bs.ap[:, 0:1], ALU.subtract, [bs], [bs])
                self.ts('dve', steps.ap, self.small.ap[:, 16:16 + TB + 1], bs.ap[:, 2:3], None, ALU.mult, None, [bs, self.small], [steps])
                self.tt('dve', mids.ap[:, 0:1], bs.ap[:, 0:1], steps.ap[:, 0:1], ALU.add, [bs, steps], [mids])
                for t in range(TB):
                    self.S.op('dve', lambda e, t=t: e.tensor_scalar(out=mb.ap[:, 0:L], in0=sca_, scalar1=mids.ap[:, t:t + 1], scalar2=0.0,
                                                                    op0=ALU.is_ge, op1=ALU.add, accum_out=bs.ap[:, 4:5]),
                              self.S._cells([scl, mids]), self.S._cells([mbl, bs]))
                    self.stt(G.ap[:, t:t + 1], bs.ap[:, 4:5], 255.5, steps.ap[:, t:t + 1], ALU.is_ge, ALU.mult, [bs, steps], [G])
                    self.stt(mids.ap[:, t + 1:t + 2], G.ap[:, t:t + 1], steps.ap[:, t + 1:t + 2], mids.ap[:, t:t + 1],
                             ALU.subtract, ALU.add, [G, steps, mids], [mids])
                self.ts('dve', G.ap, G.ap, 0.0, None, ALU.is_gt, None, [G], [G])
                self.tt('dve', cand.ap, mids.ap[:, 0:TB], G.ap, ALU.mult, [mids, G], [cand])
                self.ts('dve', G.ap, G.ap, -1.0, 1.0e30, ALU.add, ALU.mult, [G], [G])
                self.tt('dve', cand.ap, cand.ap, G.ap, ALU.add, [cand, G], [cand])
                self.S.op('dve', lambda e: e.tensor_reduce(out=bs.ap[:, 5:6], in_=cand.ap, axis=AX.X, op=ALU.max),
                          self.S._cells([cand]), self.S._cells([bs]))
                self.tt('dve', bs.ap[:, 0:1], bs.ap[:, 0:1], bs.ap[:, 5:6], ALU.max, [bs], [bs])
                self.ts('dve', mb.ap[:, 0:L], sca_, bs.ap[:, 0:1], NEG, ALU.is_lt, ALU.mult, [scl, bs], [mbl])
            else:
                self.ts('dve', mb.ap[:, 0:L], sca_, -1.0e29, NEG, ALU.is_le, ALU.mult, [scl], [mbl])

        def mask_T(i):
            for j0 in range(0, i + 1, 4):
                pb = ptb[0]
                nj = min(4, i + 1 - j0)
                for j in range(j0, j0 + nj):
                    self.tr(pb.ap[:, (j - j0) * 128:(j - j0 + 1) * 128], mb.ap[:, j * 128:(j + 1) * 128], self.identb.ap,
                            [(mb, j * 256, (j + 1) * 256), self.identb], [pb])
                self.cp('act', mbT.ap[:, j0 * 128:(j0 + nj) * 128], pb.ap[:, 0:nj * 128], [pb], [(mbT, j0 * 256, (j0 + nj) * 256)])

        def attend(i):
            for g in range(4):
                heads = []
                hf = g % 2
                for m in range(4):
                    heads.append(dict(q=None, k=(bsub(kT, g // 2), kT.ap[64 * hf:64 * hf + 64, g // 2]),
                                      v=(lambda j, g=g: (bsub(V, j), V.ap[:, j, g])), bias=None))
                self.attn_group(i, list(range(i + 1)), heads,
                                (lambda j: ((mbT, j * 256, (j + 1) * 256), mbT.ap[:, j * 128:(j + 1) * 128])), 0.125,
                                pS_att, pO[g % 2], PT, obuf, 256 * g, osb=osb[g % 2],
                                qbatch=(bsub(qT, g // 2), qT.ap[64 * hf:64 * hf + 64, g // 2, i]))

        order = list(range(NT - 1, -1, -1))
        indexer(order[0])
        topk(order[0])
        mask_T(order[0])
        indexer(order[1])
        for k in range(NT):
            cur = order[k]
            nxt = order[k + 1] if k + 1 < NT else None
            nn = order[k + 2] if k + 2 < NT else None
            if nn is not None:
                assert sc_of(nn).off != sc_of(nxt).off
                indexer(nn)
            if nxt is not None:
                topk(nxt)
            attend(cur)
            if nxt is not None:
                mask_T(nxt)
        A.release()
        self.out_proj(1, obuf, pj, ptb)
        A.release()

    def layer1_moe(self, s):
        A, dr = self.A, self.dr
        A.mark()
        hT, wb, gT, tmp = self.alloc_ffn(1)
        lg = A.alloc([NT, 8], F32)
        M8 = A.alloc([NT, 8], F32)
        comb = A.alloc([NT, 8], F32)
        sm4 = A.alloc([4, NT], F32)
        cm2 = A.alloc([NT, 8], F32)

        def evac(t, pb):
            self.tt('dve', lg.ap[:, t], pb.ap[:, 0:8], self.bc8.ap[:, 2], ALU.add, [pb, self.bc8], [lg])
            self.S.op('dve', lambda e, t=t: e.max(out=M8.ap[:, t], in_=lg.ap[:, t]), self.S._cells([lg]), self.S._cells([M8]))
        self.proj_tm(hT, dr['wrt'], 8, self.pj, evac)
        m1, m2 = M8.ap[:, :, 0], M8.ap[:, :, 1]
        d_, e2, g1, g2 = sm4.ap[:, 0], sm4.ap[:, 1], sm4.ap[:, 2], sm4.ap[:, 3]
        self.tt('dve', d_, m2, m1, ALU.subtract, [M8], [sm4])
        self.act(e2, d_, AF.Exp, [sm4], [sm4])
        self.ts('dve', d_, e2, 1.0, None, ALU.add, None, [sm4], [sm4])
        self.S.op('dve', lambda e: e.reciprocal(out=g1, in_=d_), self.S._cells([sm4]), self.S._cells([sm4]))
        self.tt('dve', g2, e2, g1, ALU.mult, [sm4], [sm4])
        self.tt('dve', d_, g1, g2, ALU.subtract, [sm4], [sm4])
        bc = lambda a: a.unsqueeze(2).to_broadcast([128, NT, 8])
        self.tt('dve', comb.ap, lg.ap, bc(m1), ALU.is_ge, [lg, M8], [comb])
        self.tt('dve', comb.ap, comb.ap, bc(d_), ALU.mult, [comb, sm4], [comb])
        self.tt('dve', cm2.ap, lg.ap, bc(m2), ALU.is_ge, [lg, M8], [cm2])
        self.tt('dve', cm2.ap, cm2.ap, bc(g2), ALU.mult, [cm2, sm4], [cm2])
        self.tt('dve', comb.ap, comb.ap, cm2.ap, ALU.add, [comb, cm2], [comb])
        units = []
        for e in range(NEXP):
            for (f0, nf) in ((0, 6), (6, 5)):
                units.append((dr['exp_g'][e, f0:f0 + nf], dr['exp_u'][e, f0:f0 + nf], dr['exp_d'][e, f0:f0 + nf], nf,
                              (lambda t, e=e: (comb, comb.ap[:, t, e:e + 1]))))
        self.run_units(hT, units, wb, gT, tmp)
        A.release()

    def final(self, s):
        A = self.A
        A.mark()
        sq = A.alloc([D], F32)
        yb = [A.alloc([D], F32) for _ in range(2)]
        self.fnw = A.alloc([D], F32)
        self.dma('sp', self.fnw.ap, self.dr['fnw'].partition_broadcast(128), (), [self.fnw])
        X = self.X
        for t in range(NT):
            y = yb[t % 2]
            if t % 4 == 0:
                self.rstd(t, 4, sq)
            self.stt(y.ap, X.ap[:, t], self.stat.ap[:, 1, t:t + 1], self.fnw.ap, ALU.mult, ALU.mult, [bsub(X, t), self.stat, self.fnw], [y])
            self.dma('sp', self.dr['out'][s, t * 128:(t + 1) * 128, :], y.ap, [y], ())
        A.release()

    def run_seq(self, s, stages=99):
        for t in range(NT):
            self.dma('sp', self.X.ap[:, t], self.dr['x'][s, t * 128:(t + 1) * 128, :], (), [bsub(self.X, t)])
        self.lvl = stages
        self.cur_s = s
        if stages >= 0.2:
            if s == 0:
                self.ada(0, 0)
            self.expand_gate(s, 0, 2)
        if stages >= 0.4:
            self.layer0_mixer(s)
        if stages >= 2:
            if s == 0:
                self.ada(0, 1)
            self.expand_gate(s, 0, 5)
            self.layer0_ffn(s)
        if stages >= 3:
            if s == 0:
                self.ada(1, 0)
            self.expand_gate(s, 1, 2)
            self.layer1_mixer(s)
        if stages >= 4:
            if s == 0:
                self.ada(1, 1)
            self.expand_gate(s, 1, 5)
            self.layer1_moe(s)
        if stages >= 5:
            self.final(s)
        else:
            for t in range(NT):
                self.dma('sp', self.dr['out'][s, t * 128:(t + 1) * 128, :], self.X.ap[:, t], [bsub(self.X, t)], ())


SB_BYTES = 207 * 1024


def build_program(nseq, shapes, stages=99):
    nc = bass.Bass("TRN2", target_bir_lowering=False)
    dr = {}
    for name, (shp, dt, kind) in shapes.items():
        dr[name] = nc.dram_tensor(name, list(shp), dt, kind=kind).ap()
    S = Sched(nc)
    with ExitStack() as es:
        sb = es.enter_context(nc.sbuf_tensor("arena", [128, SB_BYTES // 4], F32))
        ps = es.enter_context(nc.psum_tensor("psum", [128, 4096], F32))
        for k in S.sem_keys():
            nm = "s_" + "_".join(str(x) for x in (k if isinstance(k, tuple) else (k,)))
            S.sems[k] = es.enter_context(nc.semaphore(nm))
        A = Arena('sb', sb[:], SB_BYTES)
        P = Arena('psum', ps[:], 16384)
        kb = K(nc, S, A, P, dr, nseq)
        kb.pj = [P.alloc([512], F32, align=2048) for _ in range(4)]
        kb.pO = [P.alloc([512], F32, align=2048) for _ in range(2)]
        kb.ptb = [P.alloc([512], BF16, align=2048) for _ in range(2)]
        kb.setup()
        for s in range(nseq):
            kb.run_seq(s, stages)
        S.wait_all('sp')
        with nc.Block() as block:
            S.emit(block)
    return nc, S


def _chunks(W, col_lists):
    cols = np.concatenate(col_lists)
    g = W[:, cols]
    nch = len(col_lists)
    return np.ascontiguousarray(g.reshape(8, 128, nch, 128).transpose(2, 1, 0, 3))


def _tm(W, cols):
    g = W[:, cols]
    return np.ascontiguousarray(g.reshape(8, 128, len(cols)).transpose(1, 0, 2))


def _rot(cols):
    cols = np.asarray(cols).reshape(-1, 64)
    return np.concatenate([cols[:, 32:], cols[:, :32]], axis=1).reshape(-1)


def host_prepare(inp):
    f = lambda a: np.asarray(a, dtype=np.float32)
    ar = np.arange
    w0 = f(inp['e_w_in'])[0]
    o_fq, o_fk, o_fv, o_fg, o_sq, o_sk, o_sv = 0, 512, 1024, 1536, 1544, 2056, 2184
    cl = []
    for p in range(4):
        cl.append(o_fq + 128 * p + ar(128))
    for p in range(4):
        cl.append(o_fk + 128 * p + ar(128))
    sqc = [np.concatenate([o_sq + 64 * c + ar(64), o_sq + 64 * (4 + c) + ar(64)]) for c in range(4)]
    cl += sqc
    cl += [_rot(c) for c in sqc]
    skc = o_sk + ar(128)
    cl += [skc, _rot(skc)]
    wfm0 = _chunks(w0, cl)
    wtm0f = np.stack([_tm(w0, np.concatenate([o_fv + 128 * p + ar(128), o_fg + 2 * p + ar(2)])) for p in range(4)])
    wtm0s = _tm(w0, o_sv + ar(128))
    w1 = f(inp['o_w_in'])[0]
    o_q, o_k, o_v, o_iq, o_ik, o_iw = 0, 1024, 1280, 1536, 2048, 2112
    LH = [0, 1, 2, 3, 8, 9, 10, 11]
    UH = [4, 5, 6, 7, 12, 13, 14, 15]
    qc = [np.concatenate([o_q + 64 * LH[c] + ar(64), o_q + 64 * UH[c] + ar(64)]) for c in range(8)]
    kc_ = [o_k + 128 * c + ar(128) for c in range(2)]
    iqc = [o_iq + 128 * c + ar(128) for c in range(4)]
    ikc = np.concatenate([o_ik + ar(64), o_ik + ar(64)])
    cl1 = qc + [_rot(c) for c in qc] + kc_ + [_rot(c) for c in kc_] + iqc + [_rot(c) for c in iqc] + [ikc, _rot(ikc)]
    wfm1 = _chunks(w1, cl1)
    wtm1 = _tm(w1, np.concatenate([o_v + ar(256), o_iw + ar(8)]))
    ada = np.stack([f(inp['e_ada_w'])[0], f(inp['o_ada_w'])[0]])
    ada_w = np.ascontiguousarray(ada.reshape(2, 8, 128, 6, 8, 128).transpose(0, 3, 2, 4, 1, 5))
    ada_b_flat = np.stack([f(inp['e_ada_b'])[0], f(inp['o_ada_b'])[0]])
    adab = np.ascontiguousarray(ada_b_flat.reshape(2, 48, 128).transpose(2, 0, 1))
    nws = [f(inp['e_norm_mix'])[0], f(inp['e_norm_ffn'])[0], f(inp['o_norm_mix'])[0], f(inp['o_norm_ffn'])[0],
           f(inp['final_norm'])]
    normw = np.ascontiguousarray(np.stack(nws).reshape(5, 8, 128).transpose(2, 0, 1))
    bc8 = np.concatenate([f(inp['e_forget_b'])[0], f(inp['e_sinks'])[0], f(inp['o_router_b'])[0]])[None]
    wout = np.stack([f(inp['e_w_out'])[0], f(inp['o_w_out'])[0]])
    fg, fu, fd = f(inp['e_ffn_gate'])[0], f(inp['e_ffn_up'])[0], f(inp['e_ffn_down'])[0]
    ffc = [128 * c + ar(128) for c in range(22)]
    ffn_g = _chunks(fg, ffc)
    ffn_u = _chunks(fu, ffc)
    ffn_d = np.ascontiguousarray(fd.reshape(22, 128, 1024))
    xg, xu, xd = f(inp['o_exp_gate'])[0], f(inp['o_exp_up'])[0], f(inp['o_exp_down'])[0]
    exc = [128 * c + ar(128) for c in range(11)]
    exp_g = np.stack([_chunks(xg[e], exc) for e in range(NEXP)])
    exp_u = np.stack([_chunks(xu[e], exc) for e in range(NEXP)])
    exp_d = np.ascontiguousarray(xd.reshape(NEXP, 11, 128, 1024))
    wrt = _tm(f(inp['o_router_w'])[0], ar(8))
    p = ar(128)
    constf = np.zeros((128, 4, 128), np.float32)
    constf[:, 0] = np.eye(128)
    constf[:, 1] = (p[:, None] <= p[None, :])
    constf[:, 2] = 1.0
    constf[:, 3] = np.where(p[None, :] > p[:, None], -1.0e30, 0.0)
    constb = np.zeros((128, 3, 128), np.float32)
    constb[:, 0] = np.eye(128)
    constb[:, 1] = np.where(p[:, None] > p[None, :], NEG, 0.0)
    constb[:, 2] = np.where(p[:, None] > p[None, :], 0.0, NEG)
    small = np.zeros((128, 64), np.float32)
    half = 32
    inv_freq = (10000.0 ** (-np.arange(half, dtype=np.float32) / half)).astype(np.float32)
    small[:, 0] = inv_freq[p % 32]
    small[:, 1] = np.where((p % 64) < 32, -1.0, 1.0)
    r = p % 32
    small[:, 2] = (r == 0)
    small[:, 3] = (r == 1)
    small[:, 4] = (r == 2)
    small[:, 5] = (r >= 3) & (r < 6)
    small[:, 6] = -1.0 * (r == 3)
    small[:, 7] = -1.0 * (r == 4)
    small[:, 8] = -1.0 * (r == 5)
    small[:, 9] = (r < 3)
    for t in range(40):
        small[:, 16 + t] = 2.0 ** -(t + 1)
    shared = dict(wfm0=wfm0, wtm0f=wtm0f, wtm0s=wtm0s, wfm1=wfm1, wtm1=wtm1, ada_w=ada_w, ada_b_flat=ada_b_flat,
                  adab=adab, normw=normw, bc8=bc8, wout=wout, ffn_g=ffn_g, ffn_u=ffn_u, ffn_d=ffn_d,
                  exp_g=exp_g, exp_u=exp_u, exp_d=exp_d, wrt=wrt, constf=constf, constb=constb, small=small,
                  fnw=f(inp['final_norm'])[None])
    return shared


def per_core_inputs(inp, b0, nseq):
    x = np.ascontiguousarray(np.asarray(inp['x'], np.float32)[b0:b0 + nseq])
    pos = np.ascontiguousarray(np.asarray(inp['positions'], np.int32)[b0:b0 + nseq, None, :])
    c = np.asarray(inp['c'], np.float32)[b0:b0 + nseq]
    cT = np.ascontiguousarray(c.reshape(nseq, 8, 128).transpose(2, 1, 0))
    return dict(x=x, pos=pos, cT=cT)


def make_shapes(shared, pc, nseq):
    shapes = {}
    for k, v in list(shared.items()) + list(pc.items()):
        shapes[k] = (v.shape, I32 if v.dtype == np.int32 else F32, "ExternalInput")
    shapes['out'] = ((nseq, SEQ, D), F32, "ExternalOutput")
    return shapes


_CACHE = {}


def kernel(**inputs):
    ncores, nseq = 8, 2
    shared = host_prepare(inputs)
    maps = []
    for cix in range(ncores):
        pc = per_core_inputs(inputs, cix * nseq, nseq)
        m = dict(shared)
        m.update(pc)
        maps.append(m)
    if 'nc' not in _CACHE:
        _CACHE['nc'] = build_program(nseq, make_shapes(shared, maps[0], nseq))[0]
    res = run_bass_kernel_spmd(_CACHE['nc'], maps, core_ids=list(range(ncores)))
    out = np.concatenate([np.asarray(r['out'], np.float32) for r in res.results], axis=0)
    return out
```

```python
import math
from contextlib import ExitStack

import numpy as np
import concourse.bass as bass
import concourse.mybir as mybir
from concourse.bass_utils import run_bass_kernel_spmd

F32 = mybir.dt.float32
BF16 = mybir.dt.bfloat16
I32 = mybir.dt.int32
ALU = mybir.AluOpType
AF = mybir.ActivationFunctionType
AX = mybir.AxisListType

CELL = 512
PSUM_BANK = 2048


class Buf:
    def __init__(self, space, off, nbytes, ap):
        self.space = space
        self.off = off
        self.nbytes = nbytes
        self.ap = ap

    def cells(self, lo=None, hi=None):
        lo = self.off if lo is None else self.off + lo
        hi = self.off + self.nbytes if hi is None else self.off + hi
        g = CELL if self.space != 'psum' else PSUM_BANK
        return [(self.space, c) for c in range(lo // g, (hi - 1) // g + 1)]


class Sched:
    COMPUTE = ('pe', 'act', 'dve', 'pool')
    QUEUES = ('pe', 'act', 'dve', 'pool', 'sp')

    def __init__(self, nc):
        self.nc = nc
        self.ops = {e: [] for e in self.QUEUES}
        self.cnt = {e: 0 for e in self.COMPUTE}
        self.waited = {e: {} for e in self.QUEUES}
        self.cell = {}
        self.dma_slots = {'sp': 8, 'pool': 2, 'act': 4}
        self.dma_rr = {q: 0 for q in self.dma_slots}
        self.dma_val = {}
        self.sems = {}
        self.nops = 0

    def sem_keys(self):
        keys = list(self.COMPUTE)
        for q, n in self.dma_slots.items():
            keys += [('dma', q, i) for i in range(n)]
        return keys

    def _deps(self, eng, reads, writes):
        deps = {}

        def add(tok):
            if tok is None:
                return
            k, v = tok
            if eng == 'pe' and k == 'pe':
                return
            if deps.get(k, 0) < v:
                deps[k] = v

        for c in reads:
            st = self.cell.get(c)
            if st:
                add(st[0])
        for c in writes:
            st = self.cell.get(c)
            if st:
                add(st[0])
                for k, v in st[1].items():
                    add((k, v))
        out = []
        w = self.waited[eng]
        for k, v in deps.items():
            if w.get(k, 0) < v:
                w[k] = v
                out.append((k, v))
        return out

    def _commit(self, tok, reads, writes):
        k, v = tok
        for c in reads:
            st = self.cell.setdefault(c, [None, {}])
            if st[1].get(k, 0) < v:
                st[1][k] = v
        for c in writes:
            self.cell[c] = [tok, {}]

    @staticmethod
    def _cells(lst):
        out = []
        for b in lst:
            if isinstance(b, Buf):
                out += b.cells()
            elif isinstance(b, tuple) and isinstance(b[0], Buf):
                out += b[0].cells(b[1], b[2])
            else:
                out.append(b)
        return out

    def op(self, eng, fn, reads=(), writes=()):
        reads = self._cells(reads)
        writes = self._cells(writes)
        writes = writes + [c for c in reads if c[0] == 'psum' and c not in writes]
        waits = self._deps(eng, reads, writes)
        self.cnt[eng] += 1
        tok = (eng, self.cnt[eng])
        self.ops[eng].append((fn, waits, (eng, 1)))
        self._commit(tok, reads, writes)
        self.nops += 1
        return tok

    def dma(self, q, fn, reads=(), writes=()):
        reads = self._cells(reads)
        writes = self._cells(writes)
        slot = ('dma', q, self.dma_rr[q] % self.dma_slots[q])
        self.dma_rr[q] += 1
        waits = self._deps(q, reads, writes)
        prev = self.dma_val.get(slot, 0)
        if prev and self.waited[q].get(slot, 0) < prev:
            self.waited[q][slot] = prev
            waits.append((slot, prev))
        val = prev + 16
        self.dma_val[slot] = val
        tok = (slot, val)
        self.ops[q].append((fn, waits, (slot, 16)))
        self._commit(tok, reads, writes)
        self.nops += 1
        return tok

    def wait_all(self, q='sp'):
        waits = []
        for e in self.COMPUTE:
            if self.cnt[e] and self.waited[q].get(e, 0) < self.cnt[e]:
                waits.append((e, self.cnt[e]))
        for slot, v in self.dma_val.items():
            if self.waited[q].get(slot, 0) < v:
                waits.append((slot, v))
        self.ops[q].append((None, waits, None))

    def emit(self, block):
        sems = self.sems

        def run(engname):
            def body(engine):
                for fn, waits, inc in self.ops[engname]:
                    for k, v in waits:
                        engine.wait_ge(sems[k], v)
                    if fn is None:
                        continue
                    ins = fn(engine)
                    ins.then_inc(sems[inc[0]], inc[1])
            return body

        block.tensor(run('pe'))
        block.scalar(run('act'))
        block.vector(run('dve'))
        block.gpsimd(run('pool'))
        block.sync(run('sp'))


class Arena:
    def __init__(self, space, base_ap, nbytes):
        self.space = space
        self.base = base_ap
        self.nbytes = nbytes
        self.top = 0
        self.marks = []

    def alloc(self, shape, dtype, align=CELL):
        esz = 2 if dtype == BF16 else 4
        n = 1
        for s in shape:
            n *= s
        nb = n * esz
        off = (self.top + align - 1) // align * align
        assert off + nb <= self.nbytes, f"{self.space} arena overflow: need {off + nb} > {self.nbytes}"
        self.top = off + nb
        ap = self.base[:, off // 4:(off + nb + 3) // 4]
        if dtype != F32:
            ap = ap.bitcast(dtype)
            ap = ap[:, 0:n]
        if len(shape) > 1:
            names = ' '.join(f'a{i}' for i in range(len(shape)))
            kw = {f'a{i}': s for i, s in enumerate(shape[1:], start=1)}
            ap = ap.rearrange(f'p ({names}) -> p {names}', **kw)
        return Buf(self.space, off, nb, ap)

    def mark(self):
        self.marks.append(self.top)

    def release(self):
        self.top = self.marks.pop()


def _esz(dt):
    return 2 if dt == BF16 else 4


def bsub(b, i):
    shp = b.ap.shape
    n = 1
    for s in shp[2:]:
        n *= s
    rb = n * _esz(b.ap.dtype)
    return Buf(b.space, b.off + i * rb, rb, b.ap[:, i])


def bcols(b, lo, hi):
    shp = b.ap.shape
    n = 1
    for s in shp[2:]:
        n *= s
    rb = n * _esz(b.ap.dtype)
    return Buf(b.space, b.off + lo * rb, (hi - lo) * rb, b.ap[:, lo:hi])


D = 1024
SEQ = 2048
NT = 16
HD = 64
DFF = 2816
NEXP = 8
DFE = 1408
NEG = -30000.0


class K:
    def __init__(self, nc, S, A, P, dr, nseq):
        self.nc, self.S, self.A, self.P, self.dr, self.nseq = nc, S, A, P, dr, nseq

    def mm(self, out, lhsT, rhs, start, stop, R, W):
        self.S.op('pe', lambda e: e.matmul(out, lhsT=lhsT, rhs=rhs, start=start, stop=stop,
                                           skip_group_check=True), R, W)

    def tr(self, out, in_, ident, R, W):
        self.S.op('pe', lambda e: e.transpose(out=out, in_=in_, identity=ident), R, W)

    def act(self, out, in_, func, R, W, bias=None, scale=None, accum=None):
        kw = {}
        if bias is not None:
            kw['bias'] = bias
        if scale is not None:
            kw['scale'] = scale
        if accum is not None:
            kw['accum_out'] = accum
        self.S.op('act', lambda e: e.activation(out=out, in_=in_, func=func, **kw), R, W)

    def ts(self, eng, out, in0, s1, s2, op0, op1, R, W):
        if op1 is None:
            self.S.op(eng, lambda e: e.tensor_scalar(out=out, in0=in0, scalar1=s1, scalar2=None, op0=op0), R, W)
        else:
            self.S.op(eng, lambda e: e.tensor_scalar(out=out, in0=in0, scalar1=s1, scalar2=s2, op0=op0, op1=op1), R, W)

    def tt(self, eng, out, in0, in1, op, R, W):
        self.S.op(eng, lambda e: e.tensor_tensor(out=out, in0=in0, in1=in1, op=op), R, W)

    def stt(self, out, in0, scalar, in1, op0, op1, R, W):
        self.S.op('dve', lambda e: e.scalar_tensor_tensor(out=out, in0=in0, scalar=scalar, in1=in1, op0=op0, op1=op1), R, W)

    def cp(self, eng, out, in_, R, W):
        if eng == 'act':
            self.S.op('act', lambda e: e.copy(out=out, in_=in_), R, W)
        else:
            self.S.op(eng, lambda e: e.tensor_copy(out=out, in_=in_), R, W)

    def memset(self, eng, ap, val, W):
        self.S.op(eng, lambda e: e.memset(ap, val), (), W)

    def dma(self, q, out, in_, R, W):
        self.S.dma(q, lambda e: e.dma_start(out=out, in_=in_), R, W)

    def setup(self):
        A, dr = self.A, self.dr
        self.X = A.alloc([NT, D], F32)
        self.identb = A.alloc([128], BF16)
        self.constf = A.alloc([4, 128], F32)
        self.constb = A.alloc([3, 128], BF16)
        self.small = A.alloc([64], F32)
        self.normw = A.alloc([5, 8], F32)
        self.adab = A.alloc([2, 48], F32)
        self.bc8 = A.alloc([3, 8], F32)
        self.dma('sp', self.constf.ap, dr['constf'], (), [self.constf])
        self.dma('sp', self.small.ap, dr['small'], (), [self.small])
        self.dma('sp', self.normw.ap, dr['normw'], (), [self.normw])
        self.dma('sp', self.adab.ap, dr['adab'], (), [self.adab])
        self.dma('sp', self.bc8.ap, dr['bc8'].partition_broadcast(128), (), [self.bc8])
        self.dma('pool', self.constb.ap, dr['constb'], (), [self.constb])
        self.cp('dve', self.identb.ap, self.constf.ap[:, 0], [self.constf], [self.identb])
        self.esink = A.alloc([8], F32)
        self.negone = A.alloc([8], F32, align=64)
        self.memset('pool', self.negone.ap, -1.0, [self.negone])
        self.act(self.esink.ap, self.bc8.ap[:, 1], AF.Exp, [self.bc8], [self.esink])
        self.modT = A.alloc([self.nseq, 2, 6, 8], F32)
        self.gate = A.alloc([1, D], F32)
        self.cT = A.alloc([8, self.nseq], F32)
        self.scb = A.alloc([8, self.nseq], BF16)
        self.Dg = [A.alloc([128], F32) for _ in range(2)]
        self.stat = A.alloc([2, NT], F32)
        self.cosT = None

    def ada(self, l, part):
        A, dr = self.A, self.dr
        ns = self.nseq
        A.mark()
        if l == 0 and part == 0:
            self.dma('sp', self.cT.ap, dr['cT'], (), [self.cT])
            sg = A.alloc([8, ns], F32)
            self.act(sg.ap, self.cT.ap, AF.Silu, [self.cT], [sg])
            self.cp('dve', self.scb.ap, sg.ap, [sg], [self.scb])
        wst = [A.alloc([8, 8, 128], BF16) for _ in range(2)]
        ps1 = self.pj[0]
        for m in range(3 * part, 3 * part + 3):
            w = wst[m % 2]
            self.dma('pool', w.ap, dr['ada_w'][l, m], (), [w])
            for j in range(8):
                for kc in range(8):
                    self.mm(ps1.ap[:, j * ns:(j + 1) * ns], w.ap[:, j, kc], self.scb.ap[:, kc], kc == 0, kc == 7,
                            [w, self.scb], [ps1])
            pv = ps1.ap[:, 0:8 * ns].rearrange('p (j q) -> p q j', q=ns)
            for q in range(ns):
                self.tt('dve', self.modT.ap[:, q, l, m], pv[:, q], self.adab.ap[:, l, m * 8:(m + 1) * 8], ALU.add,
                        [ps1, self.adab], [self.modT])
                if m in (1, 4):
                    nw = self.normw.ap[:, 2 * l + (0 if m == 1 else 1)]
                    self.stt(self.modT.ap[:, q, l, m], self.modT.ap[:, q, l, m], 1.0, nw, ALU.add, ALU.mult,
                             [self.modT, self.normw], [self.modT])
        A.release()

    def expand_gate(self, s, l, m):
        ones = self.constf.ap[:, 2]
        identf = self.constf.ap[:, 0]
        for j in range(8):
            dg = self.Dg[j % 2]
            self.ts('dve', dg.ap, identf, self.modT.ap[:, s, l, m, j:j + 1], None, ALU.mult, None, [self.constf, self.modT], [dg])
            pb = self.pj[j // 4]
            self.mm(pb.ap[:, (j % 4) * 128:(j % 4 + 1) * 128], ones, dg.ap, True, True, [self.constf, dg], [pb])
        for hf in range(2):
            self.cp('act', self.gate.ap[:, 0, hf * 512:(hf + 1) * 512], self.pj[hf].ap, [self.pj[hf]], [self.gate])

    def rstd(self, t0, n, sq):
        X = self.X
        st = self.stat
        for t in range(t0, t0 + n):
            self.act(sq.ap, X.ap[:, t], AF.Square, [bsub(X, t)], [sq, st], accum=st.ap[:, 0, t:t + 1])
        self.ts('dve', st.ap[:, 1, t0:t0 + n], st.ap[:, 0, t0:t0 + n], 1.0 / D, 1e-6, ALU.mult, ALU.add, [st], [st])
        self.act(st.ap[:, 1, t0:t0 + n], st.ap[:, 1, t0:t0 + n], AF.Sqrt, [st], [st])
        self.S.op('dve', lambda e: e.reciprocal(out=st.ap[:, 1, t0:t0 + n], in_=st.ap[:, 1, t0:t0 + n]),
                  self.S._cells([st]), self.S._cells([st]))

    def norm_hT2(self, l, which, hT):
        A, P = self.A, self.P
        A.mark()
        sq = A.alloc([D], F32)
        xn = A.alloc([4, D], BF16)
        pt = self.ptb
        X = self.X
        msh = self.modT.ap[:, self.cur_s, l, 0 if which == 0 else 3]
        msc = self.modT.ap[:, self.cur_s, l, 1 if which == 0 else 4]
        for c in range(4):
            self.rstd(c * 4, 4, sq)
            for tl in range(4):
                t = c * 4 + tl
                self.ts('dve', xn.ap[:, tl], X.ap[:, t], self.stat.ap[:, 1, t:t + 1], None, ALU.mult, None,
                        [bsub(X, t), self.stat], [bsub(xn, tl)])
            for kc in range(8):
                pb = pt[kc % 2]
                for tl in range(4):
                    self.tr(pb.ap[:, tl * 128:(tl + 1) * 128], xn.ap[:, tl, kc * 128:(kc + 1) * 128], self.identb.ap,
                            [bsub(xn, tl), self.identb], [pb])
                if kc % 2 == 0:
                    self.act(hT.ap[:, kc, c * 512:(c + 1) * 512], pb.ap, AF.Identity, [pb, self.modT],
                             [(hT, (kc * SEQ + c * 512) * 2, (kc * SEQ + c * 512 + 512) * 2)],
                             bias=msh[:, kc:kc + 1], scale=msc[:, kc:kc + 1])
                else:
                    self.ts('dve', hT.ap[:, kc, c * 512:(c + 1) * 512], pb.ap, msc[:, kc:kc + 1], msh[:, kc:kc + 1], ALU.mult, ALU.add,
                            [pb, self.modT], [(hT, (kc * SEQ + c * 512) * 2, (kc * SEQ + c * 512 + 512) * 2)])
        A.release()

    def rope_tables(self, s, cosT, sinT):
        A = self.A
        A.mark()
        HS = SEQ // 2
        pi_ = A.alloc([HS], F32)
        ang = A.alloc([HS], F32)
        kf = A.alloc([HS], F32)
        pii = pi_.ap.bitcast(I32)
        sm = self.small
        C1 = 6.28125
        C2 = 2 * math.pi - C1

        def wrap(buf):
            self.ts('dve', kf.ap, buf.ap, math.pi, -2 * math.pi, ALU.is_gt, ALU.mult, [buf], [kf])
            self.tt('dve', buf.ap, buf.ap, kf.ap, ALU.add, [buf, kf], [buf])
            self.ts('dve', kf.ap, buf.ap, -math.pi, 2 * math.pi, ALU.is_lt, ALU.mult, [buf], [kf])
            self.tt('dve', buf.ap, buf.ap, kf.ap, ALU.add, [buf, kf], [buf])

        for hb in range(2):
            sl = slice(hb * HS, (hb + 1) * HS)
            self.dma('sp', pii, self.dr['pos'][s][:, sl].partition_broadcast(128), (), [pi_])
            self.cp('dve', ang.ap, pii, [pi_], [ang])
            self.ts('dve', ang.ap, ang.ap, sm.ap[:, 0:1], None, ALU.mult, None, [ang, sm], [ang])
            self.ts('dve', kf.ap, ang.ap, 1.0 / (2 * math.pi), None, ALU.mult, None, [ang], [kf])
            self.cp('dve', pii, kf.ap, [kf], [pi_])
            self.cp('dve', kf.ap, pii, [pi_], [kf])
            self.stt(ang.ap, kf.ap, -C1, ang.ap, ALU.mult, ALU.add, [kf, ang], [ang])
            self.stt(ang.ap, kf.ap, -C2, ang.ap, ALU.mult, ALU.add, [kf, ang], [ang])
            wrap(ang)
            self.act(sinT.ap[:, sl], ang.ap, AF.Sin, [ang, sm], [sinT], scale=sm.ap[:, 1:2])
            self.ts('dve', ang.ap, ang.ap, math.pi / 2, None, ALU.add, None, [ang], [ang])
            wrap(ang)
            self.act(cosT.ap[:, sl], ang.ap, AF.Sin, [ang], [cosT])
        A.release()

    @staticmethod
    def hT_rng(hT, t0, t1):
        return [(hT, (kc * SEQ + t0) * 2, (kc * SEQ + t1) * 2) for kc in range(8)]

    def proj_fm(self, hT, wsrc, specs, pbanks):
        A = self.A
        A.mark()
        nw = 3 if (A.nbytes - A.top) >= 3 * 4096 + 4 * 2048 + 1024 else 2
        wst = [A.alloc([2, 8, 128], BF16) for _ in range(nw)]
        nb = 2 if (A.nbytes - A.top) >= 4 * 2048 + 1024 else 1
        t1 = [A.alloc([512], F32) for _ in range(nb)]
        t2 = [A.alloc([512], F32) for _ in range(nb)]
        rn = 0
        n = 0

        def issue(si):
            cids = specs[si][1]
            w = wst[si % nw]
            if len(cids) == 2 and cids[1] == cids[0] + 1:
                self.dma('pool', w.ap, wsrc[cids[0]:cids[0] + 2].rearrange('c p k n -> p c k n'), (), [w])
            else:
                for ci, cid in enumerate(cids):
                    self.dma('pool', w.ap[:, ci], wsrc[cid], (), [bsub(w, ci)])

        for k in range(min(nw - 1, len(specs))):
            issue(k)
        for si, (kind, cids, dest, extra) in enumerate(specs):
            w = wst[si % nw]
            if si + nw - 1 < len(specs):
                issue(si + nw - 1)
            for c in range(4):
                pss = []
                for ci in range(len(cids)):
                    pb = pbanks[n % len(pbanks)]
                    n += 1
                    for kc in range(8):
                        self.mm(pb.ap, w.ap[:, ci, kc], hT.ap[:, kc, c * 512:(c + 1) * 512], kc == 0, kc == 7,
                                [bsub(w, ci)] + self.hT_rng(hT, c * 512, (c + 1) * 512), [pb])
                    pss.append(pb)
                dbuf, dap = dest(c)
                if kind == 'plain':
                    self.act(dap, pss[0].ap, AF.Copy, [pss[0]], [dbuf], scale=float(extra))
                else:
                    cosT, sinT = extra
                    a, b = t1[rn % nb], t2[rn % nb]
                    rn += 1
                    self.tt('dve', a.ap, pss[0].ap, cosT.ap[:, c * 512:(c + 1) * 512], ALU.mult, [pss[0], cosT], [a])
                    self.tt('dve', b.ap, pss[1].ap, sinT.ap[:, c * 512:(c + 1) * 512], ALU.mult, [pss[1], sinT], [b])
                    if len(dap.shape) == 3:
                        self.tt('pool', dap, a.ap.rearrange('p (t q) -> p t q', q=128), b.ap.rearrange('p (t q) -> p t q', q=128),
                                ALU.add, [a, b], [dbuf])
                    else:
                        self.tt('pool', dap, a.ap, b.ap, ALU.add, [a, b], [dbuf])
        A.release()

    def proj_tm(self, hT, wsrc, ncols, pbanks, evac):
        A = self.A
        A.mark()
        w = A.alloc([8, ncols], BF16)
        self.dma('pool', w.ap, wsrc, (), [w])
        for t in range(NT):
            pb = pbanks[t % len(pbanks)]
            for kc in range(8):
                self.mm(pb.ap[:, 0:ncols], hT.ap[:, kc, t * 128:(t + 1) * 128], w.ap[:, kc], kc == 0, kc == 7,
                        [w] + self.hT_rng(hT, t * 128, (t + 1) * 128), [pb])
            evac(t, pb)
        A.release()

    def attn_group(self, i, jlist, heads, maskfn, scale, pS, pO, PT, obuf, ocol0, sink=None, osb=None, qbatch=None):
        G = len(heads)
        nJ = len(jlist)

        def scores(jn):
            j = jlist[jn]
            ps = pS[jn % len(pS)]
            m = maskfn(j)
            started = False
            if qbatch is not None:
                out3 = ps.ap[:, 0:G * 128].rearrange('p (g q) -> p g q', q=128)
                self.mm(out3, self.identb.ap, m[1].unsqueeze(1).to_broadcast([128, G, 128]), True, False,
                        [self.identb, m[0]], [ps])
                kb_, ka_ = heads[0]['k']
                self.mm(out3, ka_[:, j * 128:(j + 1) * 128], qbatch[1], False, True, [kb_, qbatch[0]], [ps])
                return
            for hh, h in enumerate(heads):
                cols = ps.ap[:, hh * 128:(hh + 1) * 128]
                if m is not None:
                    self.mm(cols, self.identb.ap, m[1], not started, False, [self.identb, m[0]], [ps])
                    started = True
                qb, qa = h['q']
                kb, ka = h['k']
                self.mm(cols, ka[:, j * 128:(j + 1) * 128], qa[:, i * 128:(i + 1) * 128], not started,
                        h['bias'] is None, [kb, qb], [ps])
                started = True
                if h['bias'] is not None:
                    fkb, fka, fqb, fqa = h['bias']
                    self.mm(cols, fka[:, j * 128:(j + 1) * 128], fqa[:, i * 128:(i + 1) * 128], False, True,
                            [fkb, fqb], [ps])

        def exp_pv(jn):
            j = jlist[jn]
            ps = pS[jn % len(pS)]
            pt = PT[jn % len(PT)]
            self.act(pt.ap[:, 0:G * 128], ps.ap[:, 0:G * 128], AF.Exp, [ps], [pt], scale=float(scale))
            for hh, h in enumerate(heads):
                vb, va = h['v'](j)
                self.mm(pO.ap[:, hh * 65:(hh + 1) * 65], pt.ap[:, hh * 128:(hh + 1) * 128], va[:, 0:65],
                        jn == 0 and hh == 0, jn == nJ - 1, [pt, vb], [pO])

        scores(0)
        for jn in range(nJ):
            if jn + 1 < nJ:
                scores(jn + 1)
            exp_pv(jn)
        if osb is not None:
            ob = osb.ap[:, 0:G * 65]
            self.cp('act', ob, pO.ap[:, 0:G * 65], [pO], [osb])
            o3 = ob.rearrange('p (g d) -> p g d', d=65)
            od = obuf.ap[:, i, ocol0:ocol0 + G * 64].rearrange('p (g d) -> p g d', d=64)
            self.tt('pool', o3[:, :, 64], o3[:, :, 64], self.negone.ap[:, 0:G], ALU.pow, [osb, self.negone], [osb])
            self.tt('pool', od, o3[:, :, 0:64], o3[:, :, 64:65].to_broadcast([128, G, 64]), ALU.mult,
                    [osb], [(obuf, (i * D + ocol0) * 2, (i * D + ocol0 + G * 64) * 2)])
            return
        A = self.A
        A.mark()
        den = A.alloc([G], F32, align=64)
        ov = pO.ap[:, 0:G * 65].rearrange('p (g d) -> p g d', d=65)
        if sink is not None:
            self.tt('dve', den.ap, ov[:, :, 64], sink, ALU.add, [pO, self.esink], [den])
        else:
            self.cp('dve', den.ap, ov[:, :, 64], [pO], [den])
        self.S.op('dve', lambda e: e.reciprocal(out=den.ap, in_=den.ap), self.S._cells([den]), self.S._cells([den]))
        od = obuf.ap[:, i, ocol0:ocol0 + G * 64].rearrange('p (g d) -> p g d', d=64)
        self.tt('dve', od, ov[:, :, 0:64], den.ap.unsqueeze(2).to_broadcast([128, G, 64]), ALU.mult,
                [pO, den], [(obuf, (i * D + ocol0) * 2, (i * D + ocol0 + G * 64) * 2)])
        A.release()

    def fox_attn(self, p, qT, kT, V, FQ, FK, obuf, PT):
        pj, pO = self.pj, self.pO
        tri = self.constb.ap[:, 1]
        A = self.A
        rn = 0
        for hh in range(2):
            qa, ka = qT.ap[64 * hh:64 * hh + 64], kT.ap[64 * hh:64 * hh + 64]
            fqa, fka = FQ.ap[32 * hh:32 * hh + 6], FK.ap[32 * hh:32 * hh + 6]
            for c in range(4):
                po = pO[(2 * hh + c) % 2]
                nJ = 4 * c + 4

                def geom(j):
                    q0 = max(j, 4 * c)
                    off = (q0 - 4 * c) * 128
                    return q0, off, (4 * c + 4 - q0) * 128

                def scores(j, rn):
                    ps = pj[rn % 4]
                    q0, off, ncol = geom(j)
                    cols = ps.ap[:, off:off + ncol]
                    qs = slice(q0 * 128, (4 * c + 4) * 128)
                    self.mm(cols, ka[:, j * 128:(j + 1) * 128], qa[:, qs], True, False, [kT, qT], [ps])
                    diag = j >= 4 * c
                    self.mm(cols, fka[:, j * 128:(j + 1) * 128], fqa[:, qs], False, not diag, [FK, FQ], [ps])
                    if diag:
                        self.mm(ps.ap[:, off:off + 128], self.identb.ap, tri, False, True, [self.identb, self.constb], [ps])

                def exp_pv(j, rn):
                    ps = pj[rn % 4]
                    pt = PT[rn % 2]
                    q0, off, ncol = geom(j)
                    self.act(pt.ap[:, off:off + ncol], ps.ap[:, off:off + ncol], AF.Exp, [ps], [pt])
                    for t in range(q0, 4 * c + 4):
                        tl = t - 4 * c
                        self.mm(po.ap[:, tl * 65:(tl + 1) * 65], pt.ap[:, tl * 128:(tl + 1) * 128], V.ap[:, j, hh, 0:65],
                                j == 0 and tl == 0, j == t, [pt, bsub(V, j)], [po])

                scores(0, rn)
                for j in range(nJ):
                    if j + 1 < nJ:
                        scores(j + 1, rn + j + 1)
                    exp_pv(j, rn + j)
                rn += nJ
                A.mark()
                den = A.alloc([4], F32, align=64)
                ov = po.ap[:, 0:260].rearrange('p (g d) -> p g d', d=65)
                self.cp('dve', den.ap, ov[:, :, 64], [po], [den])
                self.S.op('dve', lambda e, den=den: e.reciprocal(out=den.ap, in_=den.ap), self.S._cells([den]), self.S._cells([den]))
                col0 = 128 * p + 64 * hh
                od = obuf.ap[:, 4 * c:4 * c + 4, col0:col0 + 64]
                self.tt('dve', od, ov[:, :, 0:64], den.ap.unsqueeze(2).to_broadcast([128, 4, 64]), ALU.mult,
                        [po, den], [(obuf, 4 * c * D * 2, (4 * c + 4) * D * 2)])
                A.release()

    def out_proj(self, l, obuf, pbanks, ptb):
        A = self.A
        A.mark()
        w = A.alloc([8, D], BF16)
        self.dma('pool', w.ap, self.dr['wout'][l].rearrange('(kc p) n -> p kc n', p=128), (), [w])
        oT = [A.alloc([8, 128], BF16) for _ in range(2)]
        tmp = [A.alloc([512], F32) for _ in range(2)]
        n = 0
        for t in range(NT):
            o_t = oT[t % 2]
            for k4 in range(2):
                pb = ptb[k4]
                for kk in range(4):
                    kc = 4 * k4 + kk
                    self.tr(pb.ap[:, kk * 128:(kk + 1) * 128], obuf.ap[:, t, kc * 128:(kc + 1) * 128], self.identb.ap,
                            [(obuf, t * D * 2, (t + 1) * D * 2), self.identb], [pb])
                self.cp('act', o_t.ap[:, 4 * k4:4 * k4 + 4].rearrange('p a b -> p (a b)'), pb.ap, [pb],
                        [(o_t, 4 * k4 * 256, (4 * k4 + 4) * 256)])
            for hf in range(2):
                pb = pbanks[n % len(pbanks)]
                tb = tmp[n % 2]
                n += 1
                for kc in range(8):
                    self.mm(pb.ap, o_t.ap[:, kc], w.ap[:, kc, hf * 512:(hf + 1) * 512], kc == 0, kc == 7, [o_t, w], [pb])
                self.tt('dve', tb.ap, pb.ap, self.gate.ap[:, 0, hf * 512:(hf + 1) * 512], ALU.mult, [pb, bsub(self.gate, 0)], [tb])
                xs = (self.X, (t * D + hf * 512) * 4, (t * D + hf * 512 + 512) * 4)
                xa = self.X.ap[:, t, hf * 512:(hf + 1) * 512]
                self.tt('pool', xa, xa, tb.ap, ALU.add, [xs, tb], [xs])
        A.release()

    def ffn_load(self, gsrc, usrc, dsrc, nf, wbuf):
        wg, wu, wd = wbuf
        self.dma('pool', wg.ap[:, 0:nf], gsrc.rearrange('f p k n -> p f k n'), (), [wg])
        self.dma('pool', wu.ap[:, 0:nf], usrc.rearrange('f p k n -> p f k n'), (), [wu])
        self.dma('pool', wd.ap[:, 0:nf], dsrc.rearrange('f p n -> p f n'), (), [wd])

    def ffn_gu(self, hT, nf, wbuf, gT, pbanks, tmp, c):
        wg, wu, wd = wbuf
        for f in range(nf):
            pg = pbanks[self.fn % len(pbanks)]
            pu = pbanks[(self.fn + 1) % len(pbanks)]
            self.fn += 2
            for kc in range(8):
                self.mm(pg.ap, wg.ap[:, f, kc], hT.ap[:, kc, c * 512:(c + 1) * 512], kc == 0, kc == 7,
                        [wg] + self.hT_rng(hT, c * 512, (c + 1) * 512), [pg])
            for kc in range(8):
                self.mm(pu.ap, wu.ap[:, f, kc], hT.ap[:, kc, c * 512:(c + 1) * 512], kc == 0, kc == 7,
                        [wu] + self.hT_rng(hT, c * 512, (c + 1) * 512), [pu])
            sg = tmp[2 + f % 2]
            sgb = sg.ap.bitcast(BF16)[:, 0:512]
            self.act(sgb, pg.ap, AF.Silu, [pg], [sg])
            self.tt('dve', gT.ap[:, f], pu.ap, sgb, ALU.mult, [pu, sg], [bsub(gT, f)])

    def ffn_down(self, nf, wbuf, gT, pbanks, tmp, c, comb):
        wg, wu, wd = wbuf
        for tl in range(4):
            t = c * 4 + tl
            for hf in range(2):
                pb = pbanks[self.fn % len(pbanks)]
                tb = tmp[self.fn % 2]
                self.fn += 1
                for f in range(nf):
                    self.mm(pb.ap, gT.ap[:, f, tl * 128:(tl + 1) * 128], wd.ap[:, f, hf * 512:(hf + 1) * 512],
                            f == 0, f == nf - 1, [gT, wd], [pb])
                gap = self.gate.ap[:, 0, hf * 512:(hf + 1) * 512]
                if comb is None:
                    self.tt('dve', tb.ap, pb.ap, gap, ALU.mult, [pb, bsub(self.gate, 0)], [tb])
                else:
                    cb, cap = comb(t)
                    self.stt(tb.ap, pb.ap, cap, gap, ALU.mult, ALU.mult, [pb, bsub(self.gate, 0), cb], [tb])
                xs = (self.X, (t * D + hf * 512) * 4, (t * D + hf * 512 + 512) * 4)
                xa = self.X.ap[:, t, hf * 512:(hf + 1) * 512]
                self.tt('pool', xa, xa, tb.ap, ALU.add, [xs, tb], [xs])

    def layer0_mixer(self, s):
        A, dr = self.A, self.dr
        pj, pO, ptb = self.pj, self.pO, self.ptb
        sm = self.small
        A.mark()
        hT = A.alloc([8, SEQ], BF16)
        obuf = A.alloc([NT, D], BF16)
        self.norm_hT2(0, 0, hT)
        if self.lvl < 0.45:
            return
        PT = [A.alloc([512], BF16) for _ in range(2)]
        tri = (self.constb, self.constb.ap[:, 1])
        prevb = (self.constb, self.constb.ap[:, 2])
        for p in range(4):
            A.mark()
            qT = A.alloc([SEQ], BF16)
            kT = A.alloc([SEQ], BF16)
            V = A.alloc([NT, 2, 66], BF16)
            FQ = A.alloc([SEQ], BF16)
            FK = A.alloc([SEQ], BF16)
            z = A.alloc([NT, 2], F32)
            self.memset('pool', V.ap, 1.0, [V])
            if self.lvl < 0.455:
                return
            self.proj_fm(hT, dr['wfm0'], [('plain', (p,), lambda c, b=qT: (b, b.ap[:, c * 512:(c + 1) * 512]), 0.125),
                                          ('plain', (4 + p,), lambda c, b=kT: (b, b.ap[:, c * 512:(c + 1) * 512]), 1.0)], pj)

            if self.lvl < 0.47:
                return

            def evac(t, pb, V=V, z=z, p=p):
                if self.lvl < 0.49:
                    return
                if self.lvl != 0.494:
                    self.cp('act', V.ap[:, t, :, 0:64], pb.ap[:, 0:128].rearrange('p (h d) -> p h d', d=64), [pb], [bsub(V, t)])
                if self.lvl != 0.492:
                    self.tt('dve', z.ap[:, t], pb.ap[:, 128:130], self.bc8.ap[:, 0, 2 * p:2 * p + 2], ALU.add, [pb, self.bc8], [z])
            self.proj_tm(hT, dr['wtm0f'][p], 130, pj, evac)
            if self.lvl < 0.55:
                return
            A.mark()
            sp = A.alloc([NT, 2], F32)
            Lrep = A.alloc([NT, 64], F32)
            C = A.alloc([SEQ], F32)
            Hb = A.alloc([SEQ], BF16)
            Mb = A.alloc([SEQ], BF16)
            Lb = A.alloc([SEQ], BF16)
            self.act(sp.ap, z.ap, AF.Exp, [z], [sp], scale=-1.0)
            self.act(sp.ap, sp.ap, AF.Ln, [sp], [sp], bias=1.0)
            self.memset('pool', Lrep.ap, 0.0, [Lrep])
            for t in range(NT):
                for sl in range(2):
                    self.ts('dve', Lrep.ap[:, t, 32 * sl:32 * sl + 6], sp.ap[:, t, sl:sl + 1].to_broadcast([128, 6]), -1.0, None,
                            ALU.mult, None, [sp], [bsub(Lrep, t)])
            U = self.constf.ap[:, 1]
            for t in range(NT):
                pb = pj[t % 4]
                self.mm(pb.ap[0:64, 0:128], Lrep.ap[:, t], U, True, True, [bsub(Lrep, t), self.constf], [pb])
                cs = (C, t * 512, (t + 1) * 512)
                if t == 0:
                    self.cp('dve', C.ap[0:64, 0:128], pb.ap[0:64, 0:128], [pb], [cs])
                else:
                    self.ts('dve', C.ap[0:64, t * 128:(t + 1) * 128], pb.ap[0:64, 0:128], C.ap[0:64, t * 128 - 1:t * 128], None,
                            ALU.add, None, [pb, (C, (t - 1) * 512, t * 512)], [cs])
            c64, h64, m64, l64 = C.ap[0:64], Hb.ap[0:64], Mb.ap[0:64], Lb.ap[0:64]
            self.cp('dve', h64, c64, [C], [Hb])
            self.tt('dve', c64, c64, h64, ALU.subtract, [C, Hb], [C])
            self.cp('dve', m64, c64, [C], [Mb])
            self.tt('dve', c64, c64, m64, ALU.subtract, [C, Mb], [C])
            self.cp('dve', l64, c64, [C], [Lb])
            for (dst, c0) in ((FQ, 2), (FK, 6)):
                s64 = sm.ap[0:64]
                f64 = dst.ap[0:64]
                self.ts('dve', f64, h64, s64[:, c0:c0 + 1], s64[:, c0 + 3:c0 + 4], ALU.mult, ALU.add, [Hb, sm], [dst])
                self.stt(f64, m64, s64[:, c0 + 1:c0 + 2], f64, ALU.mult, ALU.add, [Mb, sm, dst], [dst])
                self.stt(f64, l64, s64[:, c0 + 2:c0 + 3], f64, ALU.mult, ALU.add, [Lb, sm, dst], [dst])
            A.release()
            if self.lvl < 0.65:
                return
            self.fox_attn(p, qT, kT, V, FQ, FK, obuf, PT)
            A.release()
        if self.lvl < 0.75:
            return
        A.mark()
        cosT = A.alloc([SEQ], F32)
        sinT = A.alloc([SEQ], F32)
        self.rope_tables(s, cosT, sinT)
        sqT = A.alloc([NT, 4, 128], BF16)
        skT = A.alloc([SEQ], BF16)
        Vs = A.alloc([NT, 2, 66], BF16)
        self.memset('pool', Vs.ap, 1.0, [Vs])
        specs = [('rope', (8 + 2 * c, 9 + 2 * c), (lambda cc, c=c: (sqT, sqT.ap[:, 4 * cc:4 * cc + 4, c, :])), (cosT, sinT))
                 for c in range(4)]
        specs.append(('rope', (16, 17), (lambda cc: (skT, skT.ap[:, cc * 512:(cc + 1) * 512])), (cosT, sinT)))
        self.proj_fm(hT, dr['wfm0'], specs, pj)

        def evac_s(t, pb):
            self.cp('act', Vs.ap[:, t, :, 0:64], pb.ap[:, 0:128].rearrange('p (h d) -> p h d', d=64), [pb], [bsub(Vs, t)])
        self.proj_tm(hT, dr['wtm0s'], 128, pj, evac_s)
        for g in range(2):
            heads = []
            for c in range(4):
                heads.append(dict(q=None, k=(skT, skT.ap[64 * g:64 * g + 64]),
                                  v=(lambda j, g=g: (bsub(Vs, j), Vs.ap[:, j, g])), bias=None))
            for i in range(NT):
                jl = [i - 1, i] if i > 0 else [i]
                self.attn_group(i, jl, heads, (lambda j, i=i: tri if j == i else prevb), 0.125,
                                pj, pO[i % 2], PT, obuf, 512 + 256 * g, sink=self.esink.ap[:, 4 * g:4 * g + 4],
                                qbatch=(bsub(sqT, i), sqT.ap[64 * g:64 * g + 64, i]))
        A.release()
        if self.lvl < 0.85:
            return
        self.out_proj(0, obuf, pj, ptb)
        A.release()

    def run_units(self, hT, units, wb, gTs, tmp):
        self.fn = 0
        g, u_, d, nf, cb = units[0]
        self.ffn_load(g, u_, d, nf, wb[0])
        steps = [(ui, c) for ui in range(len(units)) for c in range(4)]
        for k, (ui, c) in enumerate(steps):
            if c == 1 and ui + 1 < len(units):
                g2, u2, d2, nf2, _ = units[ui + 1]
                self.ffn_load(g2, u2, d2, nf2, wb[(ui + 1) % 2])
            self.ffn_gu(hT, units[ui][3], wb[ui % 2], gTs[k % 2], self.pj, tmp, c)
            if k >= 1:
                pu_, pc_ = steps[k - 1]
                self.ffn_down(units[pu_][3], wb[pu_ % 2], gTs[(k - 1) % 2], self.pj, tmp, pc_, units[pu_][4])
        pu_, pc_ = steps[-1]
        self.ffn_down(units[pu_][3], wb[pu_ % 2], gTs[(len(steps) - 1) % 2], self.pj, tmp, pc_, units[pu_][4])

    def alloc_ffn(self, l):
        A = self.A
        hT = A.alloc([8, SEQ], BF16)
        self.norm_hT2(l, 1, hT)
        wb = [(A.alloc([6, 8, 128], BF16), A.alloc([6, 8, 128], BF16), A.alloc([6, D], BF16)) for _ in range(2)]
        gT = [A.alloc([6, 512], BF16) for _ in range(2)]
        tmp = [A.alloc([512], F32) for _ in range(4)]
        return hT, wb, gT, tmp

    def layer0_ffn(self, s):
        A, dr = self.A, self.dr
        A.mark()
        hT, wb, gT, tmp = self.alloc_ffn(0)
        units = []
        f0 = 0
        for nf in (6, 6, 5, 5):
            units.append((dr['ffn_g'][f0:f0 + nf], dr['ffn_u'][f0:f0 + nf], dr['ffn_d'][f0:f0 + nf], nf, None))
            f0 += nf
        self.run_units(hT, units, wb, gT, tmp)
        A.release()

    def layer1_mixer(self, s):
        A, dr = self.A, self.dr
        pj, pO, ptb = self.pj, self.pO, self.ptb
        A.mark()
        hT = A.alloc([8, SEQ], BF16)
        obuf = Buf(hT.space, hT.off, hT.nbytes,
                   hT.ap.rearrange('p a b -> p (a b)').rearrange('p (t d) -> p t d', d=D))
        A.mark()
        qT = A.alloc([2, NT, 4, 128], BF16)
        kT = A.alloc([2, SEQ], BF16)
        V = A.alloc([NT, 4, 66], BF16)
        iqT = A.alloc([4, SEQ], BF16)
        ikT = A.alloc([SEQ], BF16)
        iw = A.alloc([NT, 8], F32)
        A.mark()
        cosT = A.alloc([SEQ], BF16)
        sinT = A.alloc([SEQ], BF16)
        self.rope_tables(s, cosT, sinT)
        self.norm_hT2(1, 0, hT)
        self.memset('pool', V.ap, 1.0, [V])
        specs = []
        for c in range(8):
            specs.append(('rope', (2 * c, 2 * c + 1), (lambda cc, c=c: (bsub(qT, c // 4), qT.ap[:, c // 4, 4 * cc:4 * cc + 4, c % 4, :])), (cosT, sinT)))
        for c in range(2):
            specs.append(('rope', (16 + 2 * c, 17 + 2 * c), (lambda cc, c=c: (bsub(kT, c), kT.ap[:, c, cc * 512:(cc + 1) * 512])), (cosT, sinT)))
        for c in range(4):
            specs.append(('rope', (20 + 2 * c, 21 + 2 * c), (lambda cc, c=c: (bsub(iqT, c), iqT.ap[:, c, cc * 512:(cc + 1) * 512])), (cosT, sinT)))
        specs.append(('rope', (28, 29), (lambda cc: (ikT, ikT.ap[:, cc * 512:(cc + 1) * 512])), (cosT, sinT)))
        self.proj_fm(hT, dr['wfm1'], specs, pj)

        def evac(t, pb):
            self.cp('act', V.ap[:, t, :, 0:64], pb.ap[:, 0:256].rearrange('p (h d) -> p h d', d=64), [pb], [bsub(V, t)])
            self.cp('dve', iw.ap[:, t], pb.ap[:, 256:264], [pb], [iw])
        self.proj_tm(hT, dr['wtm1'], 264, pj, evac)
        A.release()
        sc = A.alloc([SEQ], F32)
        mb = A.alloc([SEQ], BF16)
        mbT = A.alloc([SEQ], BF16)
        bs = A.alloc([8], F32, align=64)
        TB = 25
        steps = A.alloc([TB + 1], F32, align=64)
        mids = A.alloc([TB + 1], F32, align=64)
        G = A.alloc([TB], F32, align=64)
        cand = A.alloc([TB], F32, align=64)
        rr = [A.alloc([512], F32) for _ in range(2)]
        Dm = A.alloc([8, 128], F32)
        PT = [A.alloc([512], BF16) for _ in range(2)]
        trineg = self.constf.ap[:, 3]
        osb = [A.alloc([260], F32, align=64) for _ in range(1)] * 2
        cnt = [0]

        def sc_of(t):
            if t % 2 == 1:
                nb = (t + 1) * 512
                ap = obuf.ap.rearrange('p t d -> p (t d)')[:, 0:nb // 2].bitcast(F32)
                return Buf(obuf.space, obuf.off, nb, ap)
            return sc

        ptbf = Buf('psum', ptb[1].off, 2048, self.P.base[:, ptb[1].off // 4:ptb[1].off // 4 + 512])
        pS_att = [pj[3], ptbf]
        identf = self.constf.ap[:, 0]
        lb = [pj[0], pj[1]]
        accb = pj[2]

        def indexer(i):
            L = (i + 1) * 128
            scb_ = sc_of(i)
            for h in range(8):
                self.ts('pool', Dm.ap[:, h], identf, iw.ap[:, i, h:h + 1], None, ALU.mult, None, [self.constf, iw], [bsub(Dm, h)])
            for c4 in range((L + 511) // 512):
                nc_ = min(512, L - 512 * c4)
                scs = (scb_, c4 * 2048, c4 * 2048 + nc_ * 4)
                sca = scb_.ap[:, c4 * 512:c4 * 512 + nc_]
                base = cnt[0]

                def logits(hi):
                    ps = lb[(base + hi) % 2]
                    hf = hi % 2
                    self.mm(ps.ap[:, 0:nc_], iqT.ap[64 * hf:64 * hf + 64, hi // 2, i * 128:(i + 1) * 128],
                            ikT.ap[64 * hf:64 * hf + 64, c4 * 512:c4 * 512 + nc_], True, True, [bsub(iqT, hi // 2), ikT], [ps])

                logits(0)
                for hi in range(8):
                    if hi + 1 < 8:
                        logits(hi + 1)
                    ps = lb[(base + hi) % 2]
                    r = rr[(base + hi) % 2]
                    self.act(r.ap[:, 0:nc_], ps.ap[:, 0:nc_], AF.Relu, [ps], [r])
                    self.mm(accb.ap[:, 0:nc_], Dm.ap[:, hi], r.ap[:, 0:nc_], hi == 0, hi == 7, [bsub(Dm, hi), r], [accb])
                cnt[0] += 8
                self.cp('act', sca, accb.ap[:, 0:nc_], [accb], [scs])

        def topk(i):
            L = (i + 1) * 128
            scb_ = sc_of(i)
            dg = (scb_, i * 512, (i + 1) * 512)
            scl = (scb_, 0, L * 4)
            mbl = (mb, 0, L * 2)
            sca_ = scb_.ap[:, 0:L]
            if i >= 2:
                self.S.op('dve', lambda e: e.tensor_reduce(out=bs.ap[:, 0:1], in_=sca_, axis=AX.X, op=ALU.min),
                          self.S._cells([scl]), self.S._cells([bs]))
            self.tt('pool', scb_.ap[:, i * 128:(i + 1) * 128], scb_.ap[:, i * 128:(i + 1) * 128], trineg, ALU.add, [dg, self.constf], [dg])
            if i >= 2:
                self.S.op('dve', lambda e: e.tensor_reduce(out=bs.ap[:, 1:2], in_=sca_, axis=AX.X, op=ALU.max),
                          self.S._cells([scl]), self.S._cells([bs]))
                self.tt('dve', bs.ap[:, 2:3], bs.ap[:, 1:2], bs.ap[:, 0:1], ALU.subtract, [bs], [bs])
                self.ts('dve', steps.ap, self.small.ap[:, 16:16 + TB + 1], bs.ap[:, 2:3], None, ALU.mult, None, [bs, self.small], [steps])
                self.tt('dve', mids.ap[:, 0:1], bs.ap[:, 0:1], steps.ap[:, 0:1], ALU.add, [bs, steps], [mids])
                for t in range(TB):
                    self.S.op('dve', lambda e, t=t: e.tensor_scalar(out=mb.ap[:, 0:L], in0=sca_, scalar1=mids.ap[:, t:t + 1], scalar2=0.0,
                                                                    op0=ALU.is_ge, op1=ALU.add, accum_out=bs.ap[:, 4:5]),
                              self.S._cells([scl, mids]), self.S._cells([mbl, bs]))
                    self.stt(G.ap[:, t:t + 1], bs.ap[:, 4:5], 255.5, steps.ap[:, t:t + 1], ALU.is_ge, ALU.mult, [bs, steps], [G])
                    self.stt(mids.ap[:, t + 1:t + 2], G.ap[:, t:t + 1], steps.ap[:, t + 1:t + 2], mids.ap[:, t:t + 1],
                             ALU.subtract, ALU.add, [G, steps, mids], [mids])
                self.ts('dve', G.ap, G.ap, 0.0, None, ALU.is_gt, None, [G], [G])
                self.tt('dve', cand.ap, mids.ap[:, 0:TB], G.ap, ALU.mult, [mids, G], [cand])
                self.ts('dve', G.ap, G.ap, -1.0, 1.0e30, ALU.add, ALU.mult, [G], [G])
                self.tt('dve', cand.ap, cand.ap, G.ap, ALU.add, [cand, G], [cand])
                self.S.op('dve', lambda e: e.tensor_reduce(out=bs.ap[:, 5:6], in_=cand.ap, axis=AX.X, op=ALU.max),
                          self.S._cells([cand]), self.S._cells([bs]))
                self.tt('dve', bs.ap[:, 0:1], bs.ap[:, 0:1], bs.ap[:, 5:6], ALU.max, [bs], [bs])
                self.ts('dve', mb.ap[:, 0:L], sca_, bs.ap[:, 0:1], NEG, ALU.is_lt, ALU.mult, [scl, bs], [mbl])
            else:
                self.ts('dve', mb.ap[:, 0:L], sca_, -1.0e29, NEG, ALU.is_le, ALU.mult, [scl], [mbl])

        def mask_T(i):
            for j0 in range(0, i + 1, 4):
                pb = ptb[0]
                nj = min(4, i + 1 - j0)
                for j in range(j0, j0 + nj):
                    self.tr(pb.ap[:, (j - j0) * 128:(j - j0 + 1) * 128], mb.ap[:, j * 128:(j + 1) * 128], self.identb.ap,
                            [(mb, j * 256, (j + 1) * 256), self.identb], [pb])
                self.cp('act', mbT.ap[:, j0 * 128:(j0 + nj) * 128], pb.ap[:, 0:nj * 128], [pb], [(mbT, j0 * 256, (j0 + nj) * 256)])

        def attend(i):
            for g in range(4):
                heads = []
                hf = g % 2
                for m in range(4):
                    heads.append(dict(q=None, k=(bsub(kT, g // 2), kT.ap[64 * hf:64 * hf + 64, g // 2]),
                                      v=(lambda j, g=g: (bsub(V, j), V.ap[:, j, g])), bias=None))
                self.attn_group(i, list(range(i + 1)), heads,
                                (lambda j: ((mbT, j * 256, (j + 1) * 256), mbT.ap[:, j * 128:(j + 1) * 128])), 0.125,
                                pS_att, pO[g % 2], PT, obuf, 256 * g, osb=osb[g % 2],
                                qbatch=(bsub(qT, g // 2), qT.ap[64 * hf:64 * hf + 64, g // 2, i]))

        order = list(range(NT - 1, -1, -1))
        indexer(order[0])
        topk(order[0])
        mask_T(order[0])
        indexer(order[1])
        for k in range(NT):
            cur = order[k]
            nxt = order[k + 1] if k + 1 < NT else None
            nn = order[k + 2] if k + 2 < NT else None
            if nn is not None:
                assert sc_of(nn).off != sc_of(nxt).off
                indexer(nn)
            if nxt is not None:
                topk(nxt)
            attend(cur)
            if nxt is not None:
                mask_T(nxt)
        A.release()
        self.out_proj(1, obuf, pj, ptb)
        A.release()

    def layer1_moe(self, s):
        A, dr = self.A, self.dr
        A.mark()
        hT, wb, gT, tmp = self.alloc_ffn(1)
        lg = A.alloc([NT, 8], F32)
        M8 = A.alloc([NT, 8], F32)
        comb = A.alloc([NT, 8], F32)
        sm4 = A.alloc([4, NT], F32)
        cm2 = A.alloc([NT, 8], F32)

        def evac(t, pb):
            self.tt('dve', lg.ap[:, t], pb.ap[:, 0:8], self.bc8.ap[:, 2], ALU.add, [pb, self.bc8], [lg])
            self.S.op('dve', lambda e, t=t: e.max(out=M8.ap[:, t], in_=lg.ap[:, t]), self.S._cells([lg]), self.S._cells([M8]))
        self.proj_tm(hT, dr['wrt'], 8, self.pj, evac)
        m1, m2 = M8.ap[:, :, 0], M8.ap[:, :, 1]
        d_, e2, g1, g2 = sm4.ap[:, 0], sm4.ap[:, 1], sm4.ap[:, 2], sm4.ap[:, 3]
        self.tt('dve', d_, m2, m1, ALU.subtract, [M8], [sm4])
        self.act(e2, d_, AF.Exp, [sm4], [sm4])
        self.ts('dve', d_, e2, 1.0, None, ALU.add, None, [sm4], [sm4])
        self.S.op('dve', lambda e: e.reciprocal(out=g1, in_=d_), self.S._cells([sm4]), self.S._cells([sm4]))
        self.tt('dve', g2, e2, g1, ALU.mult, [sm4], [sm4])
        self.tt('dve', d_, g1, g2, ALU.subtract, [sm4], [sm4])
        bc = lambda a: a.unsqueeze(2).to_broadcast([128, NT, 8])
        self.tt('dve', comb.ap, lg.ap, bc(m1), ALU.is_ge, [lg, M8], [comb])
        self.tt('dve', comb.ap, comb.ap, bc(d_), ALU.mult, [comb, sm4], [comb])
        self.tt('dve', cm2.ap, lg.ap, bc(m2), ALU.is_ge, [lg, M8], [cm2])
        self.tt('dve', cm2.ap, cm2.ap, bc(g2), ALU.mult, [cm2, sm4], [cm2])
        self.tt('dve', comb.ap, comb.ap, cm2.ap, ALU.add, [comb, cm2], [comb])
        units = []
        for e in range(NEXP):
            for (f0, nf) in ((0, 6), (6, 5)):
                units.append((dr['exp_g'][e, f0:f0 + nf], dr['exp_u'][e, f0:f0 + nf], dr['exp_d'][e, f0:f0 + nf], nf,
                              (lambda t, e=e: (comb, comb.ap[:, t, e:e + 1]))))
        self.run_units(hT, units, wb, gT, tmp)
        A.release()

    def final(self, s):
        A = self.A
        A.mark()
        sq = A.alloc([D], F32)
        yb = [A.alloc([D], F32) for _ in range(2)]
        self.fnw = A.alloc([D], F32)
        self.dma('sp', self.fnw.ap, self.dr['fnw'].partition_broadcast(128), (), [self.fnw])
        X = self.X
        for t in range(NT):
            y = yb[t % 2]
            if t % 4 == 0:
                self.rstd(t, 4, sq)
            self.stt(y.ap, X.ap[:, t], self.stat.ap[:, 1, t:t + 1], self.fnw.ap, ALU.mult, ALU.mult, [bsub(X, t), self.stat, self.fnw], [y])
            self.dma('sp', self.dr['out'][s, t * 128:(t + 1) * 128, :], y.ap, [y], ())
        A.release()

    def run_seq(self, s, stages=99):
        for t in range(NT):
            self.dma('sp', self.X.ap[:, t], self.dr['x'][s, t * 128:(t + 1) * 128, :], (), [bsub(self.X, t)])
        self.lvl = stages
        self.cur_s = s
        if stages >= 0.2:
            if s == 0:
                self.ada(0, 0)
            self.expand_gate(s, 0, 2)
        if stages >= 0.4:
            self.layer0_mixer(s)
        if stages >= 2:
            if s == 0:
                self.ada(0, 1)
            self.expand_gate(s, 0, 5)
            self.layer0_ffn(s)
        if stages >= 3:
            if s == 0:
                self.ada(1, 0)
            self.expand_gate(s, 1, 2)
            self.layer1_mixer(s)
        if stages >= 4:
            if s == 0:
                self.ada(1, 1)
            self.expand_gate(s, 1, 5)
            self.layer1_moe(s)
        if stages >= 5:
            self.final(s)
        else:
            for t in range(NT):
                self.dma('sp', self.dr['out'][s, t * 128:(t + 1) * 128, :], self.X.ap[:, t], [bsub(self.X, t)], ())


SB_BYTES = 207 * 1024


def build_program(nseq, shapes, stages=99):
    nc = bass.Bass("TRN2", target_bir_lowering=False)
    dr = {}
    for name, (shp, dt, kind) in shapes.items():
        dr[name] = nc.dram_tensor(name, list(shp), dt, kind=kind).ap()
    S = Sched(nc)
    with ExitStack() as es:
        sb = es.enter_context(nc.sbuf_tensor("arena", [128, SB_BYTES // 4], F32))
        ps = es.enter_context(nc.psum_tensor("psum", [128, 4096], F32))
        for k in S.sem_keys():
            nm = "s_" + "_".join(str(x) for x in (k if isinstance(k, tuple) else (k,)))
            S.sems[k] = es.enter_context(nc.semaphore(nm))
        A = Arena('sb', sb[:], SB_BYTES)
        P = Arena('psum', ps[:], 16384)
        kb = K(nc, S, A, P, dr, nseq)
        kb.pj = [P.alloc([512], F32, align=2048) for _ in range(4)]
        kb.pO = [P.alloc([512], F32, align=2048) for _ in range(2)]
        kb.ptb = [P.alloc([512], BF16, align=2048) for _ in range(2)]
        kb.setup()
        for s in range(nseq):
            kb.run_seq(s, stages)
        S.wait_all('sp')
        with nc.Block() as block:
            S.emit(block)
    return nc, S


def _chunks(W, col_lists):
    cols = np.concatenate(col_lists)
    g = W[:, cols]
    nch = len(col_lists)
    return np.ascontiguousarray(g.reshape(8, 128, nch, 128).transpose(2, 1, 0, 3))


def _tm(W, cols):
    g = W[:, cols]
    return np.ascontiguousarray(g.reshape(8, 128, len(cols)).transpose(1, 0, 2))


def _rot(cols):
    cols = np.asarray(cols).reshape(-1, 64)
    return np.concatenate([cols[:, 32:], cols[:, :32]], axis=1).reshape(-1)


def host_prepare(inp):
    f = lambda a: np.asarray(a, dtype=np.float32)
    ar = np.arange
    w0 = f(inp['e_w_in'])[0]
    o_fq, o_fk, o_fv, o_fg, o_sq, o_sk, o_sv = 0, 512, 1024, 1536, 1544, 2056, 2184
    cl = []
    for p in range(4):
        cl.append(o_fq + 128 * p + ar(128))
    for p in range(4):
        cl.append(o_fk + 128 * p + ar(128))
    sqc = [np.concatenate([o_sq + 64 * c + ar(64), o_sq + 64 * (4 + c) + ar(64)]) for c in range(4)]
    for c in sqc:
        cl += [c, _rot(c)]
    skc = o_sk + ar(128)
    cl += [skc, _rot(skc)]
    wfm0 = _chunks(w0, cl)
    wtm0f = np.stack([_tm(w0, np.concatenate([o_fv + 128 * p + ar(128), o_fg + 2 * p + ar(2)])) for p in range(4)])
    wtm0s = _tm(w0, o_sv + ar(128))
    w1 = f(inp['o_w_in'])[0]
    o_q, o_k, o_v, o_iq, o_ik, o_iw = 0, 1024, 1280, 1536, 2048, 2112
    LH = [0, 1, 2, 3, 8, 9, 10, 11]
    UH = [4, 5, 6, 7, 12, 13, 14, 15]
    qc = [np.concatenate([o_q + 64 * LH[c] + ar(64), o_q + 64 * UH[c] + ar(64)]) for c in range(8)]
    kc_ = [o_k + 128 * c + ar(128) for c in range(2)]
    iqc = [o_iq + 128 * c + ar(128) for c in range(4)]
    ikc = np.concatenate([o_ik + ar(64), o_ik + ar(64)])
    cl1 = []
    for c in qc + kc_ + iqc + [ikc]:
        cl1 += [c, _rot(c)]
    wfm1 = _chunks(w1, cl1)
    wtm1 = _tm(w1, np.concatenate([o_v + ar(256), o_iw + ar(8)]))
    ada = np.stack([f(inp['e_ada_w'])[0], f(inp['o_ada_w'])[0]])
    ada_w = np.ascontiguousarray(ada.reshape(2, 8, 128, 6, 8, 128).transpose(0, 3, 2, 4, 1, 5))
    ada_b_flat = np.stack([f(inp['e_ada_b'])[0], f(inp['o_ada_b'])[0]])
    adab = np.ascontiguousarray(ada_b_flat.reshape(2, 48, 128).transpose(2, 0, 1))
    nws = [f(inp['e_norm_mix'])[0], f(inp['e_norm_ffn'])[0], f(inp['o_norm_mix'])[0], f(inp['o_norm_ffn'])[0],
           f(inp['final_norm'])]
    normw = np.ascontiguousarray(np.stack(nws).reshape(5, 8, 128).transpose(2, 0, 1))
    bc8 = np.concatenate([f(inp['e_forget_b'])[0], f(inp['e_sinks'])[0], f(inp['o_router_b'])[0]])[None]
    wout = np.stack([f(inp['e_w_out'])[0], f(inp['o_w_out'])[0]])
    fg, fu, fd = f(inp['e_ffn_gate'])[0], f(inp['e_ffn_up'])[0], f(inp['e_ffn_down'])[0]
    ffc = [128 * c + ar(128) for c in range(22)]
    ffn_g = _chunks(fg, ffc)
    ffn_u = _chunks(fu, ffc)
    ffn_d = np.ascontiguousarray(fd.reshape(22, 128, 1024))
    xg, xu, xd = f(inp['o_exp_gate'])[0], f(inp['o_exp_up'])[0], f(inp['o_exp_down'])[0]
    exc = [128 * c + ar(128) for c in range(11)]
    exp_g = np.stack([_chunks(xg[e], exc) for e in range(NEXP)])
    exp_u = np.stack([_chunks(xu[e], exc) for e in range(NEXP)])
    exp_d = np.ascontiguousarray(xd.reshape(NEXP, 11, 128, 1024))
    wrt = _tm(f(inp['o_router_w'])[0], ar(8))
    p = ar(128)
    constf = np.zeros((128, 4, 128), np.float32)
    constf[:, 0] = np.eye(128)
    constf[:, 1] = (p[:, None] <= p[None, :])
    constf[:, 2] = 1.0
    constf[:, 3] = np.where(p[None, :] > p[:, None], -1.0e30, 0.0)
    constb = np.zeros((128, 3, 128), np.float32)
    constb[:, 0] = np.eye(128)
    constb[:, 1] = np.where(p[:, None] > p[None, :], NEG, 0.0)
    constb[:, 2] = np.where(p[:, None] > p[None, :], 0.0, NEG)
    small = np.zeros((128, 64), np.float32)
    half = 32
    inv_freq = (10000.0 ** (-np.arange(half, dtype=np.float32) / half)).astype(np.float32)
    small[:, 0] = inv_freq[p % 32]
    small[:, 1] = np.where((p % 64) < 32, -1.0, 1.0)
    r = p % 32
    small[:, 2] = (r == 0)
    small[:, 3] = (r == 1)
    small[:, 4] = (r == 2)
    small[:, 5] = (r >= 3) & (r < 6)
    small[:, 6] = -1.0 * (r == 3)
    small[:, 7] = -1.0 * (r == 4)
    small[:, 8] = -1.0 * (r == 5)
    small[:, 9] = (r < 3)
    for t in range(40):
        small[:, 16 + t] = 2.0 ** -(t + 1)
    shared = dict(wfm0=wfm0, wtm0f=wtm0f, wtm0s=wtm0s, wfm1=wfm1, wtm1=wtm1, ada_w=ada_w, ada_b_flat=ada_b_flat,
                  adab=adab, normw=normw, bc8=bc8, wout=wout, ffn_g=ffn_g, ffn_u=ffn_u, ffn_d=ffn_d,
                  exp_g=exp_g, exp_u=exp_u, exp_d=exp_d, wrt=wrt, constf=constf, constb=constb, small=small,
                  fnw=f(inp['final_norm'])[None])
    return shared


def per_core_inputs(inp, b0, nseq):
    x = np.ascontiguousarray(np.asarray(inp['x'], np.float32)[b0:b0 + nseq])
    pos = np.ascontiguousarray(np.asarray(inp['positions'], np.int32)[b0:b0 + nseq, None, :])
    c = np.asarray(inp['c'], np.float32)[b0:b0 + nseq]
    cT = np.ascontiguousarray(c.reshape(nseq, 8, 128).transpose(2, 1, 0))
    return dict(x=x, pos=pos, cT=cT)


def make_shapes(shared, pc, nseq):
    shapes = {}
    for k, v in list(shared.items()) + list(pc.items()):
        shapes[k] = (v.shape, I32 if v.dtype == np.int32 else F32, "ExternalInput")
    shapes['out'] = ((nseq, SEQ, D), F32, "ExternalOutput")
    return shapes


_CACHE = {}


def kernel(**inputs):
    ncores, nseq = 8, 2
    shared = host_prepare(inputs)
    maps = []
    for cix in range(ncores):
        pc = per_core_inputs(inputs, cix * nseq, nseq)
        m = dict(shared)
        m.update(pc)
        maps.append(m)
    if 'nc' not in _CACHE:
        _CACHE['nc'] = build_program(nseq, make_shapes(shared, maps[0], nseq))[0]
    res = run_bass_kernel_spmd(_CACHE['nc'], maps, core_ids=list(range(ncores)))
    out = np.concatenate([np.asarray(r['out'], np.float32) for r in res.results], axis=0)
    return out
```

```python
import math
from contextlib import ExitStack

import numpy as np
import concourse.bass as bass
import concourse.mybir as mybir
from concourse.bass_utils import run_bass_kernel_spmd

F32 = mybir.dt.float32
BF16 = mybir.dt.bfloat16
I32 = mybir.dt.int32
ALU = mybir.AluOpType
AF = mybir.ActivationFunctionType
AX = mybir.AxisListType

CELL = 512
PSUM_BANK = 2048


class Buf:
    def __init__(self, space, off, nbytes, ap):
        self.space = space
        self.off = off
        self.nbytes = nbytes
        self.ap = ap

    def cells(self, lo=None, hi=None):
        lo = self.off if lo is None else self.off + lo
        hi = self.off + self.nbytes if hi is None else self.off + hi
        g = CELL if self.space != 'psum' else PSUM_BANK
        return [(self.space, c) for c in range(lo // g, (hi - 1) // g + 1)]


class Sched:
    COMPUTE = ('pe', 'act', 'dve', 'pool')
    QUEUES = ('pe', 'act', 'dve', 'pool', 'sp')

    def __init__(self, nc):
        self.nc = nc
        self.ops = {e: [] for e in self.QUEUES}
        self.cnt = {e: 0 for e in self.COMPUTE}
        self.waited = {e: {} for e in self.QUEUES}
        self.cell = {}
        self.dma_slots = {'sp': 8, 'pool': 2, 'act': 4}
        self.dma_rr = {q: 0 for q in self.dma_slots}
        self.dma_val = {}
        self.sems = {}
        self.nops = 0

    def sem_keys(self):
        keys = list(self.COMPUTE)
        for q, n in self.dma_slots.items():
            keys += [('dma', q, i) for i in range(n)]
        return keys

    def _deps(self, eng, reads, writes):
        deps = {}

        def add(tok):
            if tok is None:
                return
            k, v = tok
            if eng == 'pe' and k == 'pe':
                return
            if deps.get(k, 0) < v:
                deps[k] = v

        for c in reads:
            st = self.cell.get(c)
            if st:
                add(st[0])
        for c in writes:
            st = self.cell.get(c)
            if st:
                add(st[0])
                for k, v in st[1].items():
                    add((k, v))
        out = []
        w = self.waited[eng]
        for k, v in deps.items():
            if w.get(k, 0) < v:
                w[k] = v
                out.append((k, v))
        return out

    def _commit(self, tok, reads, writes):
        k, v = tok
        for c in reads:
            st = self.cell.setdefault(c, [None, {}])
            if st[1].get(k, 0) < v:
                st[1][k] = v
        for c in writes:
            self.cell[c] = [tok, {}]

    @staticmethod
    def _cells(lst):
        out = []
        for b in lst:
            if isinstance(b, Buf):
                out += b.cells()
            elif isinstance(b, tuple) and isinstance(b[0], Buf):
                out += b[0].cells(b[1], b[2])
            else:
                out.append(b)
        return out

    def op(self, eng, fn, reads=(), writes=()):
        reads = self._cells(reads)
        writes = self._cells(writes)
        writes = writes + [c for c in reads if c[0] == 'psum' and c not in writes]
        waits = self._deps(eng, reads, writes)
        self.cnt[eng] += 1
        tok = (eng, self.cnt[eng])
        self.ops[eng].append((fn, waits, (eng, 1)))
        self._commit(tok, reads, writes)
        self.nops += 1
        return tok

    def dma(self, q, fn, reads=(), writes=()):
        reads = self._cells(reads)
        writes = self._cells(writes)
        slot = ('dma', q, self.dma_rr[q] % self.dma_slots[q])
        self.dma_rr[q] += 1
        waits = self._deps(q, reads, writes)
        prev = self.dma_val.get(slot, 0)
        if prev and self.waited[q].get(slot, 0) < prev:
            self.waited[q][slot] = prev
            waits.append((slot, prev))
        val = prev + 16
        self.dma_val[slot] = val
        tok = (slot, val)
        self.ops[q].append((fn, waits, (slot, 16)))
        self._commit(tok, reads, writes)
        self.nops += 1
        return tok

    def wait_all(self, q='sp'):
        waits = []
        for e in self.COMPUTE:
            if self.cnt[e] and self.waited[q].get(e, 0) < self.cnt[e]:
                waits.append((e, self.cnt[e]))
        for slot, v in self.dma_val.items():
            if self.waited[q].get(slot, 0) < v:
                waits.append((slot, v))
        self.ops[q].append((None, waits, None))

    def emit(self, block):
        sems = self.sems

        def run(engname):
            def body(engine):
                for fn, waits, inc in self.ops[engname]:
                    for k, v in waits:
                        engine.wait_ge(sems[k], v)
                    if fn is None:
                        continue
                    ins = fn(engine)
                    ins.then_inc(sems[inc[0]], inc[1])
            return body

        block.tensor(run('pe'))
        block.scalar(run('act'))
        block.vector(run('dve'))
        block.gpsimd(run('pool'))
        block.sync(run('sp'))


class Arena:
    def __init__(self, space, base_ap, nbytes):
        self.space = space
        self.base = base_ap
        self.nbytes = nbytes
        self.top = 0
        self.marks = []

    def alloc(self, shape, dtype, align=CELL):
        esz = 2 if dtype == BF16 else 4
        n = 1
        for s in shape:
            n *= s
        nb = n * esz
        off = (self.top + align - 1) // align * align
        assert off + nb <= self.nbytes, f"{self.space} arena overflow: need {off + nb} > {self.nbytes}"
        self.top = off + nb
        ap = self.base[:, off // 4:(off + nb + 3) // 4]
        if dtype != F32:
            ap = ap.bitcast(dtype)
            ap = ap[:, 0:n]
        if len(shape) > 1:
            names = ' '.join(f'a{i}' for i in range(len(shape)))
            kw = {f'a{i}': s for i, s in enumerate(shape[1:], start=1)}
            ap = ap.rearrange(f'p ({names}) -> p {names}', **kw)
        return Buf(self.space, off, nb, ap)

    def mark(self):
        self.marks.append(self.top)

    def release(self):
        self.top = self.marks.pop()


def _esz(dt):
    return 2 if dt == BF16 else 4


def bsub(b, i):
    shp = b.ap.shape
    n = 1
    for s in shp[2:]:
        n *= s
    rb = n * _esz(b.ap.dtype)
    return Buf(b.space, b.off + i * rb, rb, b.ap[:, i])


def bcols(b, lo, hi):
    shp = b.ap.shape
    n = 1
    for s in shp[2:]:
        n *= s
    rb = n * _esz(b.ap.dtype)
    return Buf(b.space, b.off + lo * rb, (hi - lo) * rb, b.ap[:, lo:hi])


D = 1024
SEQ = 2048
NT = 16
HD = 64
DFF = 2816
NEXP = 8
DFE = 1408
NEG = -30000.0


class K:
    def __init__(self, nc, S, A, P, dr, nseq):
        self.nc, self.S, self.A, self.P, self.dr, self.nseq = nc, S, A, P, dr, nseq

    def mm(self, out, lhsT, rhs, start, stop, R, W):
        self.S.op('pe', lambda e: e.matmul(out, lhsT=lhsT, rhs=rhs, start=start, stop=stop,
                                           skip_group_check=True), R, W)

    def tr(self, out, in_, ident, R, W):
        self.S.op('pe', lambda e: e.transpose(out=out, in_=in_, identity=ident), R, W)

    def act(self, out, in_, func, R, W, bias=None, scale=None, accum=None):
        kw = {}
        if bias is not None:
            kw['bias'] = bias
        if scale is not None:
            kw['scale'] = scale
        if accum is not None:
            kw['accum_out'] = accum
        self.S.op('act', lambda e: e.activation(out=out, in_=in_, func=func, **kw), R, W)

    def ts(self, eng, out, in0, s1, s2, op0, op1, R, W):
        if op1 is None:
            self.S.op(eng, lambda e: e.tensor_scalar(out=out, in0=in0, scalar1=s1, scalar2=None, op0=op0), R, W)
        else:
            self.S.op(eng, lambda e: e.tensor_scalar(out=out, in0=in0, scalar1=s1, scalar2=s2, op0=op0, op1=op1), R, W)

    def tt(self, eng, out, in0, in1, op, R, W):
        self.S.op(eng, lambda e: e.tensor_tensor(out=out, in0=in0, in1=in1, op=op), R, W)

    def stt(self, out, in0, scalar, in1, op0, op1, R, W):
        self.S.op('dve', lambda e: e.scalar_tensor_tensor(out=out, in0=in0, scalar=scalar, in1=in1, op0=op0, op1=op1), R, W)

    def cp(self, eng, out, in_, R, W):
        if eng == 'act':
            self.S.op('act', lambda e: e.copy(out=out, in_=in_), R, W)
        else:
            self.S.op(eng, lambda e: e.tensor_copy(out=out, in_=in_), R, W)

    def memset(self, eng, ap, val, W):
        self.S.op(eng, lambda e: e.memset(ap, val), (), W)

    def dma(self, q, out, in_, R, W):
        self.S.dma(q, lambda e: e.dma_start(out=out, in_=in_), R, W)

    def setup(self):
        A, dr = self.A, self.dr
        self.X = A.alloc([NT, D], F32)
        self.identb = A.alloc([128], BF16)
        self.constf = A.alloc([4, 128], F32)
        self.constb = A.alloc([3, 128], BF16)
        self.small = A.alloc([64], F32)
        self.normw = A.alloc([5, 8], F32)
        self.adab = A.alloc([2, 48], F32)
        self.bc8 = A.alloc([3, 8], F32)
        self.dma('sp', self.constf.ap, dr['constf'], (), [self.constf])
        self.dma('sp', self.small.ap, dr['small'], (), [self.small])
        self.dma('sp', self.normw.ap, dr['normw'], (), [self.normw])
        self.dma('sp', self.adab.ap, dr['adab'], (), [self.adab])
        self.dma('sp', self.bc8.ap, dr['bc8'].partition_broadcast(128), (), [self.bc8])
        self.dma('pool', self.constb.ap, dr['constb'], (), [self.constb])
        self.cp('dve', self.identb.ap, self.constf.ap[:, 0], [self.constf], [self.identb])
        self.esink = A.alloc([8], F32)
        self.negone = A.alloc([8], F32, align=64)
        self.memset('pool', self.negone.ap, -1.0, [self.negone])
        self.act(self.esink.ap, self.bc8.ap[:, 1], AF.Exp, [self.bc8], [self.esink])
        self.modT = A.alloc([self.nseq, 2, 6, 8], F32)
        self.gate = A.alloc([1, D], F32)
        self.cT = A.alloc([8, self.nseq], F32)
        self.scb = A.alloc([8, self.nseq], BF16)
        self.Dg = [A.alloc([128], F32) for _ in range(2)]
        self.stat = A.alloc([2, NT], F32)
        self.cosT = None

    def ada(self, l, part):
        A, dr = self.A, self.dr
        ns = self.nseq
        A.mark()
        if l == 0 and part == 0:
            self.dma('sp', self.cT.ap, dr['cT'], (), [self.cT])
            sg = A.alloc([8, ns], F32)
            self.act(sg.ap, self.cT.ap, AF.Silu, [self.cT], [sg])
            self.cp('dve', self.scb.ap, sg.ap, [sg], [self.scb])
        wst = [A.alloc([8, 8, 128], BF16) for _ in range(2)]
        ps1 = self.pj[0]
        for m in range(3 * part, 3 * part + 3):
            w = wst[m % 2]
            self.dma('pool', w.ap, dr['ada_w'][l, m], (), [w])
            for j in range(8):
                for kc in range(8):
                    self.mm(ps1.ap[:, j * ns:(j + 1) * ns], w.ap[:, j, kc], self.scb.ap[:, kc], kc == 0, kc == 7,
                            [w, self.scb], [ps1])
            pv = ps1.ap[:, 0:8 * ns].rearrange('p (j q) -> p q j', q=ns)
            for q in range(ns):
                self.tt('dve', self.modT.ap[:, q, l, m], pv[:, q], self.adab.ap[:, l, m * 8:(m + 1) * 8], ALU.add,
                        [ps1, self.adab], [self.modT])
                if m in (1, 4):
                    nw = self.normw.ap[:, 2 * l + (0 if m == 1 else 1)]
                    self.stt(self.modT.ap[:, q, l, m], self.modT.ap[:, q, l, m], 1.0, nw, ALU.add, ALU.mult,
                             [self.modT, self.normw], [self.modT])
        A.release()

    def expand_gate(self, s, l, m):
        ones = self.constf.ap[:, 2]
        identf = self.constf.ap[:, 0]
        for j in range(8):
            dg = self.Dg[j % 2]
            self.ts('dve', dg.ap, identf, self.modT.ap[:, s, l, m, j:j + 1], None, ALU.mult, None, [self.constf, self.modT], [dg])
            pb = self.pj[j // 4]
            self.mm(pb.ap[:, (j % 4) * 128:(j % 4 + 1) * 128], ones, dg.ap, True, True, [self.constf, dg], [pb])
        for hf in range(2):
            self.cp('act', self.gate.ap[:, 0, hf * 512:(hf + 1) * 512], self.pj[hf].ap, [self.pj[hf]], [self.gate])

    def rstd(self, t0, n, sq):
        X = self.X
        st = self.stat
        for t in range(t0, t0 + n):
            self.act(sq.ap, X.ap[:, t], AF.Square, [bsub(X, t)], [sq, st], accum=st.ap[:, 0, t:t + 1])
        self.ts('dve', st.ap[:, 1, t0:t0 + n], st.ap[:, 0, t0:t0 + n], 1.0 / D, 1e-6, ALU.mult, ALU.add, [st], [st])
        self.act(st.ap[:, 1, t0:t0 + n], st.ap[:, 1, t0:t0 + n], AF.Sqrt, [st], [st])
        self.S.op('dve', lambda e: e.reciprocal(out=st.ap[:, 1, t0:t0 + n], in_=st.ap[:, 1, t0:t0 + n]),
                  self.S._cells([st]), self.S._cells([st]))

    def norm_hT2(self, l, which, hT):
        A, P = self.A, self.P
        A.mark()
        sq = A.alloc([D], F32)
        xn = A.alloc([4, D], BF16)
        pt = self.ptb
        X = self.X
        msh = self.modT.ap[:, self.cur_s, l, 0 if which == 0 else 3]
        msc = self.modT.ap[:, self.cur_s, l, 1 if which == 0 else 4]
        for c in range(4):
            self.rstd(c * 4, 4, sq)
            for tl in range(4):
                t = c * 4 + tl
                self.ts('dve', xn.ap[:, tl], X.ap[:, t], self.stat.ap[:, 1, t:t + 1], None, ALU.mult, None,
                        [bsub(X, t), self.stat], [bsub(xn, tl)])
            for kc in range(8):
                pb = pt[kc % 2]
                for tl in range(4):
                    self.tr(pb.ap[:, tl * 128:(tl + 1) * 128], xn.ap[:, tl, kc * 128:(kc + 1) * 128], self.identb.ap,
                            [bsub(xn, tl), self.identb], [pb])
                if kc % 2 == 0:
                    self.act(hT.ap[:, kc, c * 512:(c + 1) * 512], pb.ap, AF.Identity, [pb, self.modT],
                             [(hT, (kc * SEQ + c * 512) * 2, (kc * SEQ + c * 512 + 512) * 2)],
                             bias=msh[:, kc:kc + 1], scale=msc[:, kc:kc + 1])
                else:
                    self.ts('dve', hT.ap[:, kc, c * 512:(c + 1) * 512], pb.ap, msc[:, kc:kc + 1], msh[:, kc:kc + 1], ALU.mult, ALU.add,
                            [pb, self.modT], [(hT, (kc * SEQ + c * 512) * 2, (kc * SEQ + c * 512 + 512) * 2)])
        A.release()

    def rope_tables(self, s, cosT, sinT):
        A = self.A
        A.mark()
        HS = SEQ // 2
        pi_ = A.alloc([HS], F32)
        ang = A.alloc([HS], F32)
        kf = A.alloc([HS], F32)
        pii = pi_.ap.bitcast(I32)
        sm = self.small
        C1 = 6.28125
        C2 = 2 * math.pi - C1

        def wrap(buf):
            self.ts('dve', kf.ap, buf.ap, math.pi, -2 * math.pi, ALU.is_gt, ALU.mult, [buf], [kf])
            self.tt('dve', buf.ap, buf.ap, kf.ap, ALU.add, [buf, kf], [buf])
            self.ts('dve', kf.ap, buf.ap, -math.pi, 2 * math.pi, ALU.is_lt, ALU.mult, [buf], [kf])
            self.tt('dve', buf.ap, buf.ap, kf.ap, ALU.add, [buf, kf], [buf])

        for hb in range(2):
            sl = slice(hb * HS, (hb + 1) * HS)
            self.dma('sp', pii, self.dr['pos'][s][:, sl].partition_broadcast(128), (), [pi_])
            self.cp('dve', ang.ap, pii, [pi_], [ang])
            self.ts('dve', ang.ap, ang.ap, sm.ap[:, 0:1], None, ALU.mult, None, [ang, sm], [ang])
            self.ts('dve', kf.ap, ang.ap, 1.0 / (2 * math.pi), None, ALU.mult, None, [ang], [kf])
            self.cp('dve', pii, kf.ap, [kf], [pi_])
            self.cp('dve', kf.ap, pii, [pi_], [kf])
            self.stt(ang.ap, kf.ap, -C1, ang.ap, ALU.mult, ALU.add, [kf, ang], [ang])
            self.stt(ang.ap, kf.ap, -C2, ang.ap, ALU.mult, ALU.add, [kf, ang], [ang])
            wrap(ang)
            self.act(sinT.ap[:, sl], ang.ap, AF.Sin, [ang, sm], [sinT], scale=sm.ap[:, 1:2])
            self.ts('dve', ang.ap, ang.ap, math.pi / 2, None, ALU.add, None, [ang], [ang])
            wrap(ang)
            self.act(cosT.ap[:, sl], ang.ap, AF.Sin, [ang], [cosT])
        A.release()

    @staticmethod
    def hT_rng(hT, t0, t1):
        return [(hT, (kc * SEQ + t0) * 2, (kc * SEQ + t1) * 2) for kc in range(8)]

    def proj_fm(self, hT, wsrc, specs, pbanks):
        A = self.A
        A.mark()
        nw = 3 if (A.nbytes - A.top) >= 3 * 4096 + 4 * 2048 + 1024 else 2
        wst = [A.alloc([2, 8, 128], BF16) for _ in range(nw)]
        nb = 2 if (A.nbytes - A.top) >= 4 * 2048 + 1024 else 1
        t1 = [A.alloc([512], F32) for _ in range(nb)]
        t2 = [A.alloc([512], F32) for _ in range(nb)]
        rn = 0
        n = 0

        def issue(si):
            cids = specs[si][1]
            w = wst[si % nw]
            if len(cids) == 2 and cids[1] == cids[0] + 1:
                self.dma('pool', w.ap, wsrc[cids[0]:cids[0] + 2].rearrange('c p k n -> p c k n'), (), [w])
            else:
                for ci, cid in enumerate(cids):
                    self.dma('pool', w.ap[:, ci], wsrc[cid], (), [bsub(w, ci)])

        for k in range(min(nw - 1, len(specs))):
            issue(k)
        for si, (kind, cids, dest, extra) in enumerate(specs):
            w = wst[si % nw]
            if si + nw - 1 < len(specs):
                issue(si + nw - 1)
            for c in range(4):
                pss = []
                for ci in range(len(cids)):
                    pb = pbanks[n % len(pbanks)]
                    n += 1
                    for kc in range(8):
                        self.mm(pb.ap, w.ap[:, ci, kc], hT.ap[:, kc, c * 512:(c + 1) * 512], kc == 0, kc == 7,
                                [bsub(w, ci)] + self.hT_rng(hT, c * 512, (c + 1) * 512), [pb])
                    pss.append(pb)
                dbuf, dap = dest(c)
                if kind == 'plain':
                    self.act(dap, pss[0].ap, AF.Copy, [pss[0]], [dbuf], scale=float(extra))
                else:
                    cosT, sinT = extra
                    a, b = t1[rn % nb], t2[rn % nb]
                    rn += 1
                    self.tt('dve', a.ap, pss[0].ap, cosT.ap[:, c * 512:(c + 1) * 512], ALU.mult, [pss[0], cosT], [a])
                    self.tt('dve', b.ap, pss[1].ap, sinT.ap[:, c * 512:(c + 1) * 512], ALU.mult, [pss[1], sinT], [b])
                    if len(dap.shape) == 3:
                        self.tt('pool', dap, a.ap.rearrange('p (t q) -> p t q', q=128), b.ap.rearrange('p (t q) -> p t q', q=128),
                                ALU.add, [a, b], [dbuf])
                    else:
                        self.tt('pool', dap, a.ap, b.ap, ALU.add, [a, b], [dbuf])
        A.release()

    def proj_tm(self, hT, wsrc, ncols, pbanks, evac):
        A = self.A
        A.mark()
        w = A.alloc([8, ncols], BF16)
        self.dma('pool', w.ap, wsrc, (), [w])
        for t in range(NT):
            pb = pbanks[t % len(pbanks)]
            for kc in range(8):
                self.mm(pb.ap[:, 0:ncols], hT.ap[:, kc, t * 128:(t + 1) * 128], w.ap[:, kc], kc == 0, kc == 7,
                        [w] + self.hT_rng(hT, t * 128, (t + 1) * 128), [pb])
            evac(t, pb)
        A.release()

    def attn_group(self, i, jlist, heads, maskfn, scale, pS, pO, PT, obuf, ocol0, sink=None, osb=None, qbatch=None):
        G = len(heads)
        nJ = len(jlist)

        def scores(jn):
            j = jlist[jn]
            ps = pS[jn % len(pS)]
            m = maskfn(j)
            started = False
            if qbatch is not None:
                out3 = ps.ap[:, 0:G * 128].rearrange('p (g q) -> p g q', q=128)
                self.mm(out3, self.identb.ap, m[1].unsqueeze(1).to_broadcast([128, G, 128]), True, False,
                        [self.identb, m[0]], [ps])
                kb_, ka_ = heads[0]['k']
                self.mm(out3, ka_[:, j * 128:(j + 1) * 128], qbatch[1], False, True, [kb_, qbatch[0]], [ps])
                return
            for hh, h in enumerate(heads):
                cols = ps.ap[:, hh * 128:(hh + 1) * 128]
                if m is not None:
                    self.mm(cols, self.identb.ap, m[1], not started, False, [self.identb, m[0]], [ps])
                    started = True
                qb, qa = h['q']
                kb, ka = h['k']
                self.mm(cols, ka[:, j * 128:(j + 1) * 128], qa[:, i * 128:(i + 1) * 128], not started,
                        h['bias'] is None, [kb, qb], [ps])
                started = True
                if h['bias'] is not None:
                    fkb, fka, fqb, fqa = h['bias']
                    self.mm(cols, fka[:, j * 128:(j + 1) * 128], fqa[:, i * 128:(i + 1) * 128], False, True,
                            [fkb, fqb], [ps])

        def exp_pv(jn):
            j = jlist[jn]
            ps = pS[jn % len(pS)]
            pt = PT[jn % len(PT)]
            self.act(pt.ap[:, 0:G * 128], ps.ap[:, 0:G * 128], AF.Exp, [ps], [pt], scale=float(scale))
            for hh, h in enumerate(heads):
                vb, va = h['v'](j)
                self.mm(pO.ap[:, hh * 65:(hh + 1) * 65], pt.ap[:, hh * 128:(hh + 1) * 128], va[:, 0:65],
                        jn == 0 and hh == 0, jn == nJ - 1, [pt, vb], [pO])

        scores(0)
        for jn in range(nJ):
            if jn + 1 < nJ:
                scores(jn + 1)
            exp_pv(jn)
        if osb is not None:
            ob = osb.ap[:, 0:G * 65]
            self.cp('act', ob, pO.ap[:, 0:G * 65], [pO], [osb])
            o3 = ob.rearrange('p (g d) -> p g d', d=65)
            od = obuf.ap[:, i, ocol0:ocol0 + G * 64].rearrange('p (g d) -> p g d', d=64)
            self.tt('pool', o3[:, :, 64], o3[:, :, 64], self.negone.ap[:, 0:G], ALU.pow, [osb, self.negone], [osb])
            self.tt('pool', od, o3[:, :, 0:64], o3[:, :, 64:65].to_broadcast([128, G, 64]), ALU.mult,
                    [osb], [(obuf, (i * D + ocol0) * 2, (i * D + ocol0 + G * 64) * 2)])
            return
        A = self.A
        A.mark()
        den = A.alloc([G], F32, align=64)
        ov = pO.ap[:, 0:G * 65].rearrange('p (g d) -> p g d', d=65)
        if sink is not None:
            self.tt('dve', den.ap, ov[:, :, 64], sink, ALU.add, [pO, self.esink], [den])
        else:
            self.cp('dve', den.ap, ov[:, :, 64], [pO], [den])
        self.S.op('dve', lambda e: e.reciprocal(out=den.ap, in_=den.ap), self.S._cells([den]), self.S._cells([den]))
        od = obuf.ap[:, i, ocol0:ocol0 + G * 64].rearrange('p (g d) -> p g d', d=64)
        self.tt('dve', od, ov[:, :, 0:64], den.ap.unsqueeze(2).to_broadcast([128, G, 64]), ALU.mult,
                [pO, den], [(obuf, (i * D + ocol0) * 2, (i * D + ocol0 + G * 64) * 2)])
        A.release()

    def fox_attn(self, p, qT, kT, V, FQ, FK, obuf, PT):
        pj, pO = self.pj, self.pO
        tri = self.constb.ap[:, 1]
        A = self.A
        rn = 0
        for hh in range(2):
            qa, ka = qT.ap[64 * hh:64 * hh + 64], kT.ap[64 * hh:64 * hh + 64]
            fqa, fka = FQ.ap[32 * hh:32 * hh + 6], FK.ap[32 * hh:32 * hh + 6]
            for c in range(4):
                po = pO[(2 * hh + c) % 2]
                nJ = 4 * c + 4

                def geom(j):
                    q0 = max(j, 4 * c)
                    off = (q0 - 4 * c) * 128
                    return q0, off, (4 * c + 4 - q0) * 128

                def scores(j, rn):
                    ps = pj[rn % 4]
                    q0, off, ncol = geom(j)
                    cols = ps.ap[:, off:off + ncol]
                    qs = slice(q0 * 128, (4 * c + 4) * 128)
                    self.mm(cols, ka[:, j * 128:(j + 1) * 128], qa[:, qs], True, False, [kT, qT], [ps])
                    diag = j >= 4 * c
                    self.mm(cols, fka[:, j * 128:(j + 1) * 128], fqa[:, qs], False, not diag, [FK, FQ], [ps])
                    if diag:
                        self.mm(ps.ap[:, off:off + 128], self.identb.ap, tri, False, True, [self.identb, self.constb], [ps])

                def exp_pv(j, rn):
                    ps = pj[rn % 4]
                    pt = PT[rn % 2]
                    q0, off, ncol = geom(j)
                    self.act(pt.ap[:, off:off + ncol], ps.ap[:, off:off + ncol], AF.Exp, [ps], [pt])
                    for t in range(q0, 4 * c + 4):
                        tl = t - 4 * c
                        self.mm(po.ap[:, tl * 65:(tl + 1) * 65], pt.ap[:, tl * 128:(tl + 1) * 128], V.ap[:, j, hh, 0:65],
                                j == 0 and tl == 0, j == t, [pt, bsub(V, j)], [po])

                scores(0, rn)
                for j in range(nJ):
                    if j + 1 < nJ:
                        scores(j + 1, rn + j + 1)
                    exp_pv(j, rn + j)
                rn += nJ
                A.mark()
                den = A.alloc([4], F32, align=64)
                ov = po.ap[:, 0:260].rearrange('p (g d) -> p g d', d=65)
                self.cp('dve', den.ap, ov[:, :, 64], [po], [den])
                self.S.op('dve', lambda e, den=den: e.reciprocal(out=den.ap, in_=den.ap), self.S._cells([den]), self.S._cells([den]))
                col0 = 128 * p + 64 * hh
                od = obuf.ap[:, 4 * c:4 * c + 4, col0:col0 + 64]
                self.tt('dve', od, ov[:, :, 0:64], den.ap.unsqueeze(2).to_broadcast([128, 4, 64]), ALU.mult,
                        [po, den], [(obuf, 4 * c * D * 2, (4 * c + 4) * D * 2)])
                A.release()

    def out_proj(self, l, obuf, pbanks, ptb):
        A = self.A
        A.mark()
        w = A.alloc([8, D], BF16)
        self.dma('pool', w.ap, self.dr['wout'][l].rearrange('(kc p) n -> p kc n', p=128), (), [w])
        oT = [A.alloc([8, 128], BF16) for _ in range(2)]
        tmp = [A.alloc([512], F32) for _ in range(2)]
        n = 0
        for t in range(NT):
            o_t = oT[t % 2]
            for k4 in range(2):
                pb = ptb[k4]
                for kk in range(4):
                    kc = 4 * k4 + kk
                    self.tr(pb.ap[:, kk * 128:(kk + 1) * 128], obuf.ap[:, t, kc * 128:(kc + 1) * 128], self.identb.ap,
                            [(obuf, t * D * 2, (t + 1) * D * 2), self.identb], [pb])
                self.cp('act', o_t.ap[:, 4 * k4:4 * k4 + 4].rearrange('p a b -> p (a b)'), pb.ap, [pb],
                        [(o_t, 4 * k4 * 256, (4 * k4 + 4) * 256)])
            for hf in range(2):
                pb = pbanks[n % len(pbanks)]
                tb = tmp[n % 2]
                n += 1
                for kc in range(8):
                    self.mm(pb.ap, o_t.ap[:, kc], w.ap[:, kc, hf * 512:(hf + 1) * 512], kc == 0, kc == 7, [o_t, w], [pb])
                self.tt('dve', tb.ap, pb.ap, self.gate.ap[:, 0, hf * 512:(hf + 1) * 512], ALU.mult, [pb, bsub(self.gate, 0)], [tb])
                xs = (self.X, (t * D + hf * 512) * 4, (t * D + hf * 512 + 512) * 4)
                xa = self.X.ap[:, t, hf * 512:(hf + 1) * 512]
                self.tt('pool', xa, xa, tb.ap, ALU.add, [xs, tb], [xs])
        A.release()

    def ffn_load(self, gsrc, usrc, dsrc, nf, wbuf):
        wg, wu, wd = wbuf
        self.dma('pool', wg.ap[:, 0:nf], gsrc.rearrange('f p k n -> p f k n'), (), [wg])
        self.dma('pool', wu.ap[:, 0:nf], usrc.rearrange('f p k n -> p f k n'), (), [wu])
        self.dma('pool', wd.ap[:, 0:nf], dsrc.rearrange('f p n -> p f n'), (), [wd])

    def ffn_gu(self, hT, nf, wbuf, gT, pbanks, tmp, c):
        wg, wu, wd = wbuf
        for f in range(nf):
            pg = pbanks[self.fn % len(pbanks)]
            pu = pbanks[(self.fn + 1) % len(pbanks)]
            self.fn += 2
            for kc in range(8):
                self.mm(pg.ap, wg.ap[:, f, kc], hT.ap[:, kc, c * 512:(c + 1) * 512], kc == 0, kc == 7,
                        [wg] + self.hT_rng(hT, c * 512, (c + 1) * 512), [pg])
            for kc in range(8):
                self.mm(pu.ap, wu.ap[:, f, kc], hT.ap[:, kc, c * 512:(c + 1) * 512], kc == 0, kc == 7,
                        [wu] + self.hT_rng(hT, c * 512, (c + 1) * 512), [pu])
            sg = tmp[2 + f % 2]
            sgb = sg.ap.bitcast(BF16)[:, 0:512]
            self.act(sgb, pg.ap, AF.Silu, [pg], [sg])
            self.tt('dve', gT.ap[:, f], pu.ap, sgb, ALU.mult, [pu, sg], [bsub(gT, f)])

    def ffn_down(self, nf, wbuf, gT, pbanks, tmp, c, comb):
        wg, wu, wd = wbuf
        for tl in range(4):
            t = c * 4 + tl
            for hf in range(2):
                pb = pbanks[self.fn % len(pbanks)]
                tb = tmp[self.fn % 2]
                self.fn += 1
                for f in range(nf):
                    self.mm(pb.ap, gT.ap[:, f, tl * 128:(tl + 1) * 128], wd.ap[:, f, hf * 512:(hf + 1) * 512],
                            f == 0, f == nf - 1, [gT, wd], [pb])
                gap = self.gate.ap[:, 0, hf * 512:(hf + 1) * 512]
                if comb is None:
                    self.tt('dve', tb.ap, pb.ap, gap, ALU.mult, [pb, bsub(self.gate, 0)], [tb])
                else:
                    cb, cap = comb(t)
                    self.stt(tb.ap, pb.ap, cap, gap, ALU.mult, ALU.mult, [pb, bsub(self.gate, 0), cb], [tb])
                xs = (self.X, (t * D + hf * 512) * 4, (t * D + hf * 512 + 512) * 4)
                xa = self.X.ap[:, t, hf * 512:(hf + 1) * 512]
                self.tt('pool', xa, xa, tb.ap, ALU.add, [xs, tb], [xs])

    def layer0_mixer(self, s):
        A, dr = self.A, self.dr
        pj, pO, ptb = self.pj, self.pO, self.ptb
        sm = self.small
        A.mark()
        hT = A.alloc([8, SEQ], BF16)
        obuf = A.alloc([NT, D], BF16)
        self.norm_hT2(0, 0, hT)
        if self.lvl < 0.45:
            return
        PT = [A.alloc([512], BF16) for _ in range(2)]
        tri = (self.constb, self.constb.ap[:, 1])
        prevb = (self.constb, self.constb.ap[:, 2])
        for p in range(4):
            A.mark()
            qT = A.alloc([SEQ], BF16)
            kT = A.alloc([SEQ], BF16)
            V = A.alloc([NT, 2, 66], BF16)
            FQ = A.alloc([SEQ], BF16)
            FK = A.alloc([SEQ], BF16)
            z = A.alloc([NT, 2], F32)
            self.memset('pool', V.ap, 1.0, [V])
            if self.lvl < 0.455:
                return
            self.proj_fm(hT, dr['wfm0'], [('plain', (p,), lambda c, b=qT: (b, b.ap[:, c * 512:(c + 1) * 512]), 0.125),
                                          ('plain', (4 + p,), lambda c, b=kT: (b, b.ap[:, c * 512:(c + 1) * 512]), 1.0)], pj)

            if self.lvl < 0.47:
                return

            def evac(t, pb, V=V, z=z, p=p):
                if self.lvl < 0.49:
                    return
                if self.lvl != 0.494:
                    self.cp('act', V.ap[:, t, :, 0:64], pb.ap[:, 0:128].rearrange('p (h d) -> p h d', d=64), [pb], [bsub(V, t)])
                if self.lvl != 0.492:
                    self.tt('dve', z.ap[:, t], pb.ap[:, 128:130], self.bc8.ap[:, 0, 2 * p:2 * p + 2], ALU.add, [pb, self.bc8], [z])
            self.proj_tm(hT, dr['wtm0f'][p], 130, pj, evac)
            if self.lvl < 0.55:
                return
            A.mark()
            sp = A.alloc([NT, 2], F32)
            Lrep = A.alloc([NT, 64], F32)
            C = A.alloc([SEQ], F32)
            Hb = A.alloc([SEQ], BF16)
            Mb = A.alloc([SEQ], BF16)
            Lb = A.alloc([SEQ], BF16)
            self.act(sp.ap, z.ap, AF.Exp, [z], [sp], scale=-1.0)
            self.act(sp.ap, sp.ap, AF.Ln, [sp], [sp], bias=1.0)
            self.memset('pool', Lrep.ap, 0.0, [Lrep])
            for t in range(NT):
                for sl in range(2):
                    self.ts('dve', Lrep.ap[:, t, 32 * sl:32 * sl + 6], sp.ap[:, t, sl:sl + 1].to_broadcast([128, 6]), -1.0, None,
                            ALU.mult, None, [sp], [bsub(Lrep, t)])
            U = self.constf.ap[:, 1]
            for t in range(NT):
                pb = pj[t % 4]
                self.mm(pb.ap[0:64, 0:128], Lrep.ap[:, t], U, True, True, [bsub(Lrep, t), self.constf], [pb])
                cs = (C, t * 512, (t + 1) * 512)
                if t == 0:
                    self.cp('dve', C.ap[0:64, 0:128], pb.ap[0:64, 0:128], [pb], [cs])
                else:
                    self.ts('dve', C.ap[0:64, t * 128:(t + 1) * 128], pb.ap[0:64, 0:128], C.ap[0:64, t * 128 - 1:t * 128], None,
                            ALU.add, None, [pb, (C, (t - 1) * 512, t * 512)], [cs])
            c64, h64, m64, l64 = C.ap[0:64], Hb.ap[0:64], Mb.ap[0:64], Lb.ap[0:64]
            self.cp('dve', h64, c64, [C], [Hb])
            self.tt('dve', c64, c64, h64, ALU.subtract, [C, Hb], [C])
            self.cp('dve', m64, c64, [C], [Mb])
            self.tt('dve', c64, c64, m64, ALU.subtract, [C, Mb], [C])
            self.cp('dve', l64, c64, [C], [Lb])
            for (dst, c0) in ((FQ, 2), (FK, 6)):
                s64 = sm.ap[0:64]
                f64 = dst.ap[0:64]
                self.ts('dve', f64, h64, s64[:, c0:c0 + 1], s64[:, c0 + 3:c0 + 4], ALU.mult, ALU.add, [Hb, sm], [dst])
                self.stt(f64, m64, s64[:, c0 + 1:c0 + 2], f64, ALU.mult, ALU.add, [Mb, sm, dst], [dst])
                self.stt(f64, l64, s64[:, c0 + 2:c0 + 3], f64, ALU.mult, ALU.add, [Lb, sm, dst], [dst])
            A.release()
            if self.lvl < 0.65:
                return
            self.fox_attn(p, qT, kT, V, FQ, FK, obuf, PT)
            A.release()
        if self.lvl < 0.75:
            return
        A.mark()
        cosT = A.alloc([SEQ], F32)
        sinT = A.alloc([SEQ], F32)
        self.rope_tables(s, cosT, sinT)
        sqT = A.alloc([NT, 4, 128], BF16)
        skT = A.alloc([SEQ], BF16)
        Vs = A.alloc([NT, 2, 66], BF16)
        self.memset('pool', Vs.ap, 1.0, [Vs])
        specs = [('rope', (8 + 2 * c, 9 + 2 * c), (lambda cc, c=c: (sqT, sqT.ap[:, 4 * cc:4 * cc + 4, c, :])), (cosT, sinT))
                 for c in range(4)]
        specs.append(('rope', (16, 17), (lambda cc: (skT, skT.ap[:, cc * 512:(cc + 1) * 512])), (cosT, sinT)))
        self.proj_fm(hT, dr['wfm0'], specs, pj)

        def evac_s(t, pb):
            self.cp('act', Vs.ap[:, t, :, 0:64], pb.ap[:, 0:128].rearrange('p (h d) -> p h d', d=64), [pb], [bsub(Vs, t)])
        self.proj_tm(hT, dr['wtm0s'], 128, pj, evac_s)
        for g in range(2):
            heads = []
            for c in range(4):
                heads.append(dict(q=None, k=(skT, skT.ap[64 * g:64 * g + 64]),
                                  v=(lambda j, g=g: (bsub(Vs, j), Vs.ap[:, j, g])), bias=None))
            for i in range(NT):
                jl = [i - 1, i] if i > 0 else [i]
                self.attn_group(i, jl, heads, (lambda j, i=i: tri if j == i else prevb), 0.125,
                                pj, pO[i % 2], PT, obuf, 512 + 256 * g, sink=self.esink.ap[:, 4 * g:4 * g + 4],
                                qbatch=(bsub(sqT, i), sqT.ap[64 * g:64 * g + 64, i]))
        A.release()
        if self.lvl < 0.85:
            return
        self.out_proj(0, obuf, pj, ptb)
        A.release()

    def run_units(self, hT, units, wb, gTs, tmp):
        self.fn = 0
        steps = [(ui, c) for ui in range(len(units)) for c in range(4)]
        for k, (ui, c) in enumerate(steps):
            if c == 1 and ui + 1 < len(units):
                g2, u2, d2, nf2, _ = units[ui + 1]
                self.ffn_load(g2, u2, d2, nf2, wb[(ui + 1) % 2])
            self.ffn_gu(hT, units[ui][3], wb[ui % 2], gTs[k % 2], self.pj, tmp, c)
            if k >= 1:
                pu_, pc_ = steps[k - 1]
                self.ffn_down(units[pu_][3], wb[pu_ % 2], gTs[(k - 1) % 2], self.pj, tmp, pc_, units[pu_][4])
        pu_, pc_ = steps[-1]
        self.ffn_down(units[pu_][3], wb[pu_ % 2], gTs[(len(steps) - 1) % 2], self.pj, tmp, pc_, units[pu_][4])

    def alloc_ffn(self, l, first):
        A = self.A
        hT = A.alloc([8, SEQ], BF16)
        wb = [(A.alloc([6, 8, 128], BF16), A.alloc([6, 8, 128], BF16), A.alloc([6, D], BF16)) for _ in range(2)]
        self.ffn_load(first[0], first[1], first[2], first[3], wb[0])
        self.norm_hT2(l, 1, hT)
        gT = [A.alloc([6, 512], BF16) for _ in range(2)]
        tmp = [A.alloc([512], F32) for _ in range(4)]
        return hT, wb, gT, tmp

    def layer0_ffn(self, s):
        A, dr = self.A, self.dr
        A.mark()
        units = []
        f0 = 0
        for nf in (6, 6, 5, 5):
            units.append((dr['ffn_g'][f0:f0 + nf], dr['ffn_u'][f0:f0 + nf], dr['ffn_d'][f0:f0 + nf], nf, None))
            f0 += nf
        hT, wb, gT, tmp = self.alloc_ffn(0, units[0])
        self.run_units(hT, units, wb, gT, tmp)
        A.release()

    def layer1_mixer(self, s):
        A, dr = self.A, self.dr
        pj, pO, ptb = self.pj, self.pO, self.ptb
        A.mark()
        hT = A.alloc([8, SEQ], BF16)
        obuf = Buf(hT.space, hT.off, hT.nbytes,
                   hT.ap.rearrange('p a b -> p (a b)').rearrange('p (t d) -> p t d', d=D))
        A.mark()
        qT = A.alloc([2, NT, 4, 128], BF16)
        kT = A.alloc([2, SEQ], BF16)
        V = A.alloc([NT, 4, 66], BF16)
        iqT = A.alloc([4, SEQ], BF16)
        ikT = A.alloc([SEQ], BF16)
        iw = A.alloc([NT, 8], F32)
        A.mark()
        cosT = A.alloc([SEQ], BF16)
        sinT = A.alloc([SEQ], BF16)
        self.rope_tables(s, cosT, sinT)
        self.norm_hT2(1, 0, hT)
        self.memset('pool', V.ap, 1.0, [V])
        specs = []
        for c in range(8):
            specs.append(('rope', (2 * c, 2 * c + 1), (lambda cc, c=c: (bsub(qT, c // 4), qT.ap[:, c // 4, 4 * cc:4 * cc + 4, c % 4, :])), (cosT, sinT)))
        for c in range(2):
            specs.append(('rope', (16 + 2 * c, 17 + 2 * c), (lambda cc, c=c: (bsub(kT, c), kT.ap[:, c, cc * 512:(cc + 1) * 512])), (cosT, sinT)))
        for c in range(4):
            specs.append(('rope', (20 + 2 * c, 21 + 2 * c), (lambda cc, c=c: (bsub(iqT, c), iqT.ap[:, c, cc * 512:(cc + 1) * 512])), (cosT, sinT)))
        specs.append(('rope', (28, 29), (lambda cc: (ikT, ikT.ap[:, cc * 512:(cc + 1) * 512])), (cosT, sinT)))
        self.proj_fm(hT, dr['wfm1'], specs, pj)

        def evac(t, pb):
            self.cp('act', V.ap[:, t, :, 0:64], pb.ap[:, 0:256].rearrange('p (h d) -> p h d', d=64), [pb], [bsub(V, t)])
            self.cp('dve', iw.ap[:, t], pb.ap[:, 256:264], [pb], [iw])
        self.proj_tm(hT, dr['wtm1'], 264, pj, evac)
        A.release()
        sc = A.alloc([SEQ], F32)
        mb = A.alloc([SEQ], BF16)
        mbT = A.alloc([SEQ], BF16)
        bs = A.alloc([8], F32, align=64)
        TB = 25
        steps = A.alloc([TB + 1], F32, align=64)
        mids = A.alloc([TB + 1], F32, align=64)
        G = A.alloc([TB], F32, align=64)
        cand = A.alloc([TB], F32, align=64)
        rr = [A.alloc([512], F32) for _ in range(2)]
        Dm = A.alloc([8, 128], F32)
        PT = [A.alloc([512], BF16) for _ in range(2)]
        trineg = self.constf.ap[:, 3]
        osb = [A.alloc([260], F32, align=64) for _ in range(1)] * 2
        cnt = [0]

        def sc_of(t):
            if t % 2 == 1:
                nb = (t + 1) * 512
                ap = obuf.ap.rearrange('p t d -> p (t d)')[:, 0:nb // 2].bitcast(F32)
                return Buf(obuf.space, obuf.off, nb, ap)
            return sc

        ptbf = Buf('psum', ptb[1].off, 2048, self.P.base[:, ptb[1].off // 4:ptb[1].off // 4 + 512])
        pS_att = [pj[3], ptbf]
        identf = self.constf.ap[:, 0]
        lb = [pj[0], pj[1]]
        accb = pj[2]

        def indexer(i):
            L = (i + 1) * 128
            scb_ = sc_of(i)
            for h in range(8):
                self.ts('pool', Dm.ap[:, h], identf, iw.ap[:, i, h:h + 1], None, ALU.mult, None, [self.constf, iw], [bsub(Dm, h)])
            for c4 in range((L + 511) // 512):
                nc_ = min(512, L - 512 * c4)
                scs = (scb_, c4 * 2048, c4 * 2048 + nc_ * 4)
                sca = scb_.ap[:, c4 * 512:c4 * 512 + nc_]
                base = cnt[0]

                def logits(hi):
                    ps = lb[(base + hi) % 2]
                    hf = hi % 2
                    self.mm(ps.ap[:, 0:nc_], iqT.ap[64 * hf:64 * hf + 64, hi // 2, i * 128:(i + 1) * 128],
                            ikT.ap[64 * hf:64 * hf + 64, c4 * 512:c4 * 512 + nc_], True, True, [bsub(iqT, hi // 2), ikT], [ps])

                logits(0)
                for hi in range(8):
                    if hi + 1 < 8:
                        logits(hi + 1)
                    ps = lb[(base + hi) % 2]
                    r = rr[(base + hi) % 2]
                    self.act(r.ap[:, 0:nc_], ps.ap[:, 0:nc_], AF.Relu, [ps], [r])
                    self.mm(accb.ap[:, 0:nc_], Dm.ap[:, hi], r.ap[:, 0:nc_], hi == 0, hi == 7, [bsub(Dm, hi), r], [accb])
                cnt[0] += 8
                self.cp('act', sca, accb.ap[:, 0:nc_], [accb], [scs])

        def topk(i):
            L = (i + 1) * 128
            scb_ = sc_of(i)
            dg = (scb_, i * 512, (i + 1) * 512)
            scl = (scb_, 0, L * 4)
            mbl = (mb, 0, L * 2)
            sca_ = scb_.ap[:, 0:L]
            if i >= 2:
                self.S.op('dve', lambda e: e.tensor_reduce(out=bs.ap[:, 0:1], in_=sca_, axis=AX.X, op=ALU.min),
                          self.S._cells([scl]), self.S._cells([bs]))
            self.tt('pool', scb_.ap[:, i * 128:(i + 1) * 128], scb_.ap[:, i * 128:(i + 1) * 128], trineg, ALU.add, [dg, self.constf], [dg])
            if i >= 2:
                self.S.op('dve', lambda e: e.tensor_reduce(out=bs.ap[:, 1:2], in_=sca_, axis=AX.X, op=ALU.max),
                          self.S._cells([scl]), self.S._cells([bs]))
                self.tt('dve', bs.ap[:, 2:3], bs.ap[:, 1:2], bs.ap[:, 0:1], ALU.subtract, [bs], [bs])
                self.ts('dve', steps.ap, self.small.ap[:, 16:16 + TB + 1], bs.ap[:, 2:3], None, ALU.mult, None, [bs, self.small], [steps])
                self.tt('dve', mids.ap[:, 0:1], bs.ap[:, 0:1], steps.ap[:, 0:1], ALU.add, [bs, steps], [mids])
                for t in range(TB):
                    self.S.op('dve', lambda e, t=t: e.tensor_scalar(out=mb.ap[:, 0:L], in0=sca_, scalar1=mids.ap[:, t:t + 1], scalar2=0.0,
                                                                    op0=ALU.is_ge, op1=ALU.add, accum_out=bs.ap[:, 4:5]),
                              self.S._cells([scl, mids]), self.S._cells([mbl, bs]))
                    self.stt(G.ap[:, t:t + 1], bs.ap[:, 4:5], 255.5, steps.ap[:, t:t + 1], ALU.is_ge, ALU.mult, [bs, steps], [G])
                    self.stt(mids.ap[:, t + 1:t + 2], G.ap[:, t:t + 1], steps.ap[:, t + 1:t + 2], mids.ap[:, t:t + 1],
                             ALU.subtract, ALU.add, [G, steps, mids], [mids])
                self.ts('dve', G.ap, G.ap, 0.0, None, ALU.is_gt, None, [G], [G])
                self.tt('dve', cand.ap, mids.ap[:, 0:TB], G.ap, ALU.mult, [mids, G], [cand])
                self.ts('dve', G.ap, G.ap, -1.0, 1.0e30, ALU.add, ALU.mult, [G], [G])
                self.tt('dve', cand.ap, cand.ap, G.ap, ALU.add, [cand, G], [cand])
                self.S.op('dve', lambda e: e.tensor_reduce(out=bs.ap[:, 5:6], in_=cand.ap, axis=AX.X, op=ALU.max),
                          self.S._cells([cand]), self.S._cells([bs]))
                self.tt('dve', bs.ap[:, 0:1], bs.ap[:, 0:1], bs.ap[:, 5:6], ALU.max, [bs], [bs])
                self.ts('dve', mb.ap[:, 0:L], sca_, bs.ap[:, 0:1], NEG, ALU.is_lt, ALU.mult, [scl, bs], [mbl])
            else:
                self.ts('dve', mb.ap[:, 0:L], sca_, -1.0e29, NEG, ALU.is_le, ALU.mult, [scl], [mbl])

        def mask_T(i):
            for j0 in range(0, i + 1, 4):
                pb = ptb[0]
                nj = min(4, i + 1 - j0)
                for j in range(j0, j0 + nj):
                    self.tr(pb.ap[:, (j - j0) * 128:(j - j0 + 1) * 128], mb.ap[:, j * 128:(j + 1) * 128], self.identb.ap,
                            [(mb, j * 256, (j + 1) * 256), self.identb], [pb])
                self.cp('act', mbT.ap[:, j0 * 128:(j0 + nj) * 128], pb.ap[:, 0:nj * 128], [pb], [(mbT, j0 * 256, (j0 + nj) * 256)])

        def attend(i):
            for g in range(4):
                heads = []
                hf = g % 2
                for m in range(4):
                    heads.append(dict(q=None, k=(bsub(kT, g // 2), kT.ap[64 * hf:64 * hf + 64, g // 2]),
                                      v=(lambda j, g=g: (bsub(V, j), V.ap[:, j, g])), bias=None))
                self.attn_group(i, list(range(i + 1)), heads,
                                (lambda j: ((mbT, j * 256, (j + 1) * 256), mbT.ap[:, j * 128:(j + 1) * 128])), 0.125,
                                pS_att, pO[g % 2], PT, obuf, 256 * g, osb=osb[g % 2],
                                qbatch=(bsub(qT, g // 2), qT.ap[64 * hf:64 * hf + 64, g // 2, i]))

        order = list(range(NT - 1, -1, -1))
        indexer(order[0])
        topk(order[0])
        mask_T(order[0])
        indexer(order[1])
        for k in range(NT):
            cur = order[k]
            nxt = order[k + 1] if k + 1 < NT else None
            nn = order[k + 2] if k + 2 < NT else None
            if nn is not None:
                assert sc_of(nn).off != sc_of(nxt).off
                indexer(nn)
            if nxt is not None:
                topk(nxt)
            attend(cur)
            if nxt is not None:
                mask_T(nxt)
        A.release()
        self.out_proj(1, obuf, pj, ptb)
        A.release()

    def layer1_moe(self, s):
        A, dr = self.A, self.dr
        A.mark()
        hT, wb, gT, tmp = self.alloc_ffn(1, (dr['exp_g'][0, 0:6], dr['exp_u'][0, 0:6], dr['exp_d'][0, 0:6], 6))
        lg = A.alloc([NT, 8], F32)
        M8 = A.alloc([NT, 8], F32)
        comb = A.alloc([NT, 8], F32)
        sm4 = A.alloc([4, NT], F32)
        cm2 = A.alloc([NT, 8], F32)

        def evac(t, pb):
            self.tt('dve', lg.ap[:, t], pb.ap[:, 0:8], self.bc8.ap[:, 2], ALU.add, [pb, self.bc8], [lg])
            self.S.op('dve', lambda e, t=t: e.max(out=M8.ap[:, t], in_=lg.ap[:, t]), self.S._cells([lg]), self.S._cells([M8]))
        self.proj_tm(hT, dr['wrt'], 8, self.pj, evac)
        m1, m2 = M8.ap[:, :, 0], M8.ap[:, :, 1]
        d_, e2, g1, g2 = sm4.ap[:, 0], sm4.ap[:, 1], sm4.ap[:, 2], sm4.ap[:, 3]
        self.tt('dve', d_, m2, m1, ALU.subtract, [M8], [sm4])
        self.act(e2, d_, AF.Exp, [sm4], [sm4])
        self.ts('dve', d_, e2, 1.0, None, ALU.add, None, [sm4], [sm4])
        self.S.op('dve', lambda e: e.reciprocal(out=g1, in_=d_), self.S._cells([sm4]), self.S._cells([sm4]))
        self.tt('dve', g2, e2, g1, ALU.mult, [sm4], [sm4])
        self.tt('dve', d_, g1, g2, ALU.subtract, [sm4], [sm4])
        bc = lambda a: a.unsqueeze(2).to_broadcast([128, NT, 8])
        self.tt('dve', comb.ap, lg.ap, bc(m1), ALU.is_ge, [lg, M8], [comb])
        self.tt('dve', comb.ap, comb.ap, bc(d_), ALU.mult, [comb, sm4], [comb])
        self.tt('dve', cm2.ap, lg.ap, bc(m2), ALU.is_ge, [lg, M8], [cm2])
        self.tt('dve', cm2.ap, cm2.ap, bc(g2), ALU.mult, [cm2, sm4], [cm2])
        self.tt('dve', comb.ap, comb.ap, cm2.ap, ALU.add, [comb, cm2], [comb])
        units = []
        for e in range(NEXP):
            for (f0, nf) in ((0, 6), (6, 5)):
                units.append((dr['exp_g'][e, f0:f0 + nf], dr['exp_u'][e, f0:f0 + nf], dr['exp_d'][e, f0:f0 + nf], nf,
                              (lambda t, e=e: (comb, comb.ap[:, t, e:e + 1]))))
        self.run_units(hT, units, wb, gT, tmp)
        A.release()

    def final(self, s):
        A = self.A
        A.mark()
        sq = A.alloc([D], F32)
        yb = [A.alloc([D], F32) for _ in range(2)]
        self.fnw = A.alloc([D], F32)
        self.dma('sp', self.fnw.ap, self.dr['fnw'].partition_broadcast(128), (), [self.fnw])
        X = self.X
        for t in range(NT):
            y = yb[t % 2]
            if t % 4 == 0:
                self.rstd(t, 4, sq)
            self.stt(y.ap, X.ap[:, t], self.stat.ap[:, 1, t:t + 1], self.fnw.ap, ALU.mult, ALU.mult, [bsub(X, t), self.stat, self.fnw], [y])
            self.dma('sp', self.dr['out'][s, t * 128:(t + 1) * 128, :], y.ap, [y], ())
        A.release()

    def run_seq(self, s, stages=99):
        for t in range(NT):
            self.dma('sp', self.X.ap[:, t], self.dr['x'][s, t * 128:(t + 1) * 128, :], (), [bsub(self.X, t)])
        self.lvl = stages
        self.cur_s = s
        if stages >= 0.2:
            if s == 0:
                self.ada(0, 0)
            self.expand_gate(s, 0, 2)
        if stages >= 0.4:
            self.layer0_mixer(s)
        if stages >= 2:
            if s == 0:
                self.ada(0, 1)
            self.expand_gate(s, 0, 5)
            self.layer0_ffn(s)
        if stages >= 3:
            if s == 0:
                self.ada(1, 0)
            self.expand_gate(s, 1, 2)
            self.layer1_mixer(s)
        if stages >= 4:
            if s == 0:
                self.ada(1, 1)
            self.expand_gate(s, 1, 5)
            self.layer1_moe(s)
        if stages >= 5:
            self.final(s)
        else:
            for t in range(NT):
                self.dma('sp', self.dr['out'][s, t * 128:(t + 1) * 128, :], self.X.ap[:, t], [bsub(self.X, t)], ())


SB_BYTES = 207 * 1024


def build_program(nseq, shapes, stages=99):
    nc = bass.Bass("TRN2", target_bir_lowering=False)
    dr = {}
    for name, (shp, dt, kind) in shapes.items():
        dr[name] = nc.dram_tensor(name, list(shp), dt, kind=kind).ap()
    S = Sched(nc)
    with ExitStack() as es:
        sb = es.enter_context(nc.sbuf_tensor("arena", [128, SB_BYTES // 4], F32))
        ps = es.enter_context(nc.psum_tensor("psum", [128, 4096], F32))
        for k in S.sem_keys():
            nm = "s_" + "_".join(str(x) for x in (k if isinstance(k, tuple) else (k,)))
            S.sems[k] = es.enter_context(nc.semaphore(nm))
        A = Arena('sb', sb[:], SB_BYTES)
        P = Arena('psum', ps[:], 16384)
        kb = K(nc, S, A, P, dr, nseq)
        kb.pj = [P.alloc([512], F32, align=2048) for _ in range(4)]
        kb.pO = [P.alloc([512], F32, align=2048) for _ in range(2)]
        kb.ptb = [P.alloc([512], BF16, align=2048) for _ in range(2)]
        kb.setup()
        for s in range(nseq):
            kb.run_seq(s, stages)
        S.wait_all('sp')
        with nc.Block() as block:
            S.emit(block)
    return nc, S


def _chunks(W, col_lists):
    cols = np.concatenate(col_lists)
    g = W[:, cols]
    nch = len(col_lists)
    return np.ascontiguousarray(g.reshape(8, 128, nch, 128).transpose(2, 1, 0, 3))


def _tm(W, cols):
    g = W[:, cols]
    return np.ascontiguousarray(g.reshape(8, 128, len(cols)).transpose(1, 0, 2))


def _rot(cols):
    cols = np.asarray(cols).reshape(-1, 64)
    return np.concatenate([cols[:, 32:], cols[:, :32]], axis=1).reshape(-1)


def host_prepare(inp):
    f = lambda a: np.asarray(a, dtype=np.float32)
    ar = np.arange
    w0 = f(inp['e_w_in'])[0]
    o_fq, o_fk, o_fv, o_fg, o_sq, o_sk, o_sv = 0, 512, 1024, 1536, 1544, 2056, 2184
    cl = []
    for p in range(4):
        cl.append(o_fq + 128 * p + ar(128))
    for p in range(4):
        cl.append(o_fk + 128 * p + ar(128))
    sqc = [np.concatenate([o_sq + 64 * c + ar(64), o_sq + 64 * (4 + c) + ar(64)]) for c in range(4)]
    for c in sqc:
        cl += [c, _rot(c)]
    skc = o_sk + ar(128)
    cl += [skc, _rot(skc)]
    wfm0 = _chunks(w0, cl)
    wtm0f = np.stack([_tm(w0, np.concatenate([o_fv + 128 * p + ar(128), o_fg + 2 * p + ar(2)])) for p in range(4)])
    wtm0s = _tm(w0, o_sv + ar(128))
    w1 = f(inp['o_w_in'])[0]
    o_q, o_k, o_v, o_iq, o_ik, o_iw = 0, 1024, 1280, 1536, 2048, 2112
    LH = [0, 1, 2, 3, 8, 9, 10, 11]
    UH = [4, 5, 6, 7, 12, 13, 14, 15]
    qc = [np.concatenate([o_q + 64 * LH[c] + ar(64), o_q + 64 * UH[c] + ar(64)]) for c in range(8)]
    kc_ = [o_k + 128 * c + ar(128) for c in range(2)]
    iqc = [o_iq + 128 * c + ar(128) for c in range(4)]
    ikc = np.concatenate([o_ik + ar(64), o_ik + ar(64)])
    cl1 = []
    for c in qc + kc_ + iqc + [ikc]:
        cl1 += [c, _rot(c)]
    wfm1 = _chunks(w1, cl1)
    wtm1 = _tm(w1, np.concatenate([o_v + ar(256), o_iw + ar(8)]))
    ada = np.stack([f(inp['e_ada_w'])[0], f(inp['o_ada_w'])[0]])
    ada_w = np.ascontiguousarray(ada.reshape(2, 8, 128, 6, 8, 128).transpose(0, 3, 2, 4, 1, 5))
    ada_b_flat = np.stack([f(inp['e_ada_b'])[0], f(inp['o_ada_b'])[0]])
    adab = np.ascontiguousarray(ada_b_flat.reshape(2, 48, 128).transpose(2, 0, 1))
    nws = [f(inp['e_norm_mix'])[0], f(inp['e_norm_ffn'])[0], f(inp['o_norm_mix'])[0], f(inp['o_norm_ffn'])[0],
           f(inp['final_norm'])]
    normw = np.ascontiguousarray(np.stack(nws).reshape(5, 8, 128).transpose(2, 0, 1))
    bc8 = np.concatenate([f(inp['e_forget_b'])[0], f(inp['e_sinks'])[0], f(inp['o_router_b'])[0]])[None]
    wout = np.stack([f(inp['e_w_out'])[0], f(inp['o_w_out'])[0]])
    fg, fu, fd = f(inp['e_ffn_gate'])[0], f(inp['e_ffn_up'])[0], f(inp['e_ffn_down'])[0]
    ffc = [128 * c + ar(128) for c in range(22)]
    ffn_g = _chunks(fg, ffc)
    ffn_u = _chunks(fu, ffc)
    ffn_d = np.ascontiguousarray(fd.reshape(22, 128, 1024))
    xg, xu, xd = f(inp['o_exp_gate'])[0], f(inp['o_exp_up'])[0], f(inp['o_exp_down'])[0]
    exc = [128 * c + ar(128) for c in range(11)]
    exp_g = np.stack([_chunks(xg[e], exc) for e in range(NEXP)])
    exp_u = np.stack([_chunks(xu[e], exc) for e in range(NEXP)])
    exp_d = np.ascontiguousarray(xd.reshape(NEXP, 11, 128, 1024))
    wrt = _tm(f(inp['o_router_w'])[0], ar(8))
    p = ar(128)
    constf = np.zeros((128, 4, 128), np.float32)
    constf[:, 0] = np.eye(128)
    constf[:, 1] = (p[:, None] <= p[None, :])
    constf[:, 2] = 1.0
    constf[:, 3] = np.where(p[None, :] > p[:, None], -1.0e30, 0.0)
    constb = np.zeros((128, 3, 128), np.float32)
    constb[:, 0] = np.eye(128)
    constb[:, 1] = np.where(p[:, None] > p[None, :], NEG, 0.0)
    constb[:, 2] = np.where(p[:, None] > p[None, :], 0.0, NEG)
    small = np.zeros((128, 64), np.float32)
    half = 32
    inv_freq = (10000.0 ** (-np.arange(half, dtype=np.float32) / half)).astype(np.float32)
    small[:, 0] = inv_freq[p % 32]
    small[:, 1] = np.where((p % 64) < 32, -1.0, 1.0)
    r = p % 32
    small[:, 2] = (r == 0)
    small[:, 3] = (r == 1)
    small[:, 4] = (r == 2)
    small[:, 5] = (r >= 3) & (r < 6)
    small[:, 6] = -1.0 * (r == 3)
    small[:, 7] = -1.0 * (r == 4)
    small[:, 8] = -1.0 * (r == 5)
    small[:, 9] = (r < 3)
    for t in range(40):
        small[:, 16 + t] = 2.0 ** -(t + 1)
    shared = dict(wfm0=wfm0, wtm0f=wtm0f, wtm0s=wtm0s, wfm1=wfm1, wtm1=wtm1, ada_w=ada_w, ada_b_flat=ada_b_flat,
                  adab=adab, normw=normw, bc8=bc8, wout=wout, ffn_g=ffn_g, ffn_u=ffn_u, ffn_d=ffn_d,
                  exp_g=exp_g, exp_u=exp_u, exp_d=exp_d, wrt=wrt, constf=constf, constb=constb, small=small,
                  fnw=f(inp['final_norm'])[None])
    return shared


def per_core_inputs(inp, b0, nseq):
    x = np.ascontiguousarray(np.asarray(inp['x'], np.float32)[b0:b0 + nseq])
    pos = np.ascontiguousarray(np.asarray(inp['positions'], np.int32)[b0:b0 + nseq, None, :])
    c = np.asarray(inp['c'], np.float32)[b0:b0 + nseq]
    cT = np.ascontiguousarray(c.reshape(nseq, 8, 128).transpose(2, 1, 0))
    return dict(x=x, pos=pos, cT=cT)


def make_shapes(shared, pc, nseq):
    shapes = {}
    for k, v in list(shared.items()) + list(pc.items()):
        shapes[k] = (v.shape, I32 if v.dtype == np.int32 else F32, "ExternalInput")
    shapes['out'] = ((nseq, SEQ, D), F32, "ExternalOutput")
    return shapes


_CACHE = {}


def kernel(**inputs):
    ncores, nseq = 8, 2
    shared = host_prepare(inputs)
    maps = []
    for cix in range(ncores):
        pc = per_core_inputs(inputs, cix * nseq, nseq)
        m = dict(shared)
        m.update(pc)
        maps.append(m)
    if 'nc' not in _CACHE:
        _CACHE['nc'] = build_program(nseq, make_shapes(shared, maps[0], nseq))[0]
    res = run_bass_kernel_spmd(_CACHE['nc'], maps, core_ids=list(range(ncores)))
    out = np.concatenate([np.asarray(r['out'], np.float32) for r in res.results], axis=0)
    return out
```

```python
import math
from contextlib import ExitStack

import numpy as np
import concourse.bass as bass
import concourse.mybir as mybir
from concourse.bass_utils import run_bass_kernel_spmd

F32 = mybir.dt.float32
BF16 = mybir.dt.bfloat16
I32 = mybir.dt.int32
ALU = mybir.AluOpType
AF = mybir.ActivationFunctionType
AX = mybir.AxisListType

CELL = 512
PSUM_BANK = 2048


class Buf:
    def __init__(self, space, off, nbytes, ap):
        self.space = space
        self.off = off
        self.nbytes = nbytes
        self.ap = ap

    def cells(self, lo=None, hi=None):
        lo = self.off if lo is None else self.off + lo
        hi = self.off + self.nbytes if hi is None else self.off + hi
        g = CELL if self.space != 'psum' else PSUM_BANK
        return [(self.space, c) for c in range(lo // g, (hi - 1) // g + 1)]


class Sched:
    COMPUTE = ('pe', 'act', 'dve', 'pool')
    QUEUES = ('pe', 'act', 'dve', 'pool', 'sp')

    def __init__(self, nc):
        self.nc = nc
        self.ops = {e: [] for e in self.QUEUES}
        self.cnt = {e: 0 for e in self.COMPUTE}
        self.waited = {e: {} for e in self.QUEUES}
        self.cell = {}
        self.dma_slots = {'sp': 8, 'pool': 2, 'act': 4}
        self.dma_rr = {q: 0 for q in self.dma_slots}
        self.dma_val = {}
        self.sems = {}
        self.nops = 0

    def sem_keys(self):
        keys = list(self.COMPUTE)
        for q, n in self.dma_slots.items():
            keys += [('dma', q, i) for i in range(n)]
        return keys

    def _deps(self, eng, reads, writes):
        deps = {}

        def add(tok):
            if tok is None:
                return
            k, v = tok
            if eng == 'pe' and k == 'pe':
                return
            if deps.get(k, 0) < v:
                deps[k] = v

        for c in reads:
            st = self.cell.get(c)
            if st:
                add(st[0])
        for c in writes:
            st = self.cell.get(c)
            if st:
                add(st[0])
                for k, v in st[1].items():
                    add((k, v))
        out = []
        w = self.waited[eng]
        for k, v in deps.items():
            if w.get(k, 0) < v:
                w[k] = v
                out.append((k, v))
        return out

    def _commit(self, tok, reads, writes):
        k, v = tok
        for c in reads:
            st = self.cell.setdefault(c, [None, {}])
            if st[1].get(k, 0) < v:
                st[1][k] = v
        for c in writes:
            self.cell[c] = [tok, {}]

    @staticmethod
    def _cells(lst):
        out = []
        for b in lst:
            if isinstance(b, Buf):
                out += b.cells()
            elif isinstance(b, tuple) and isinstance(b[0], Buf):
                out += b[0].cells(b[1], b[2])
            else:
                out.append(b)
        return out

    def op(self, eng, fn, reads=(), writes=()):
        reads = self._cells(reads)
        writes = self._cells(writes)
        writes = writes + [c for c in reads if c[0] == 'psum' and c not in writes]
        waits = self._deps(eng, reads, writes)
        self.cnt[eng] += 1
        tok = (eng, self.cnt[eng])
        self.ops[eng].append((fn, waits, (eng, 1)))
        self._commit(tok, reads, writes)
        self.nops += 1
        return tok

    def dma(self, q, fn, reads=(), writes=()):
        reads = self._cells(reads)
        writes = self._cells(writes)
        slot = ('dma', q, self.dma_rr[q] % self.dma_slots[q])
        self.dma_rr[q] += 1
        waits = self._deps(q, reads, writes)
        prev = self.dma_val.get(slot, 0)
        if prev and self.waited[q].get(slot, 0) < prev:
            self.waited[q][slot] = prev
            waits.append((slot, prev))
        val = prev + 16
        self.dma_val[slot] = val
        tok = (slot, val)
        self.ops[q].append((fn, waits, (slot, 16)))
        self._commit(tok, reads, writes)
        self.nops += 1
        return tok

    def wait_all(self, q='sp'):
        waits = []
        for e in self.COMPUTE:
            if self.cnt[e] and self.waited[q].get(e, 0) < self.cnt[e]:
                waits.append((e, self.cnt[e]))
        for slot, v in self.dma_val.items():
            if self.waited[q].get(slot, 0) < v:
                waits.append((slot, v))
        self.ops[q].append((None, waits, None))

    def emit(self, block):
        sems = self.sems

        def run(engname):
            def body(engine):
                for fn, waits, inc in self.ops[engname]:
                    for k, v in waits:
                        engine.wait_ge(sems[k], v)
                    if fn is None:
                        continue
                    ins = fn(engine)
                    ins.then_inc(sems[inc[0]], inc[1])
            return body

        block.tensor(run('pe'))
        block.scalar(run('act'))
        block.vector(run('dve'))
        block.gpsimd(run('pool'))
        block.sync(run('sp'))


class Arena:
    def __init__(self, space, base_ap, nbytes):
        self.space = space
        self.base = base_ap
        self.nbytes = nbytes
        self.top = 0
        self.marks = []

    def alloc(self, shape, dtype, align=CELL):
        esz = 2 if dtype == BF16 else 4
        n = 1
        for s in shape:
            n *= s
        nb = n * esz
        off = (self.top + align - 1) // align * align
        assert off + nb <= self.nbytes, f"{self.space} arena overflow: need {off + nb} > {self.nbytes}"
        self.top = off + nb
        ap = self.base[:, off // 4:(off + nb + 3) // 4]
        if dtype != F32:
            ap = ap.bitcast(dtype)
            ap = ap[:, 0:n]
        if len(shape) > 1:
            names = ' '.join(f'a{i}' for i in range(len(shape)))
            kw = {f'a{i}': s for i, s in enumerate(shape[1:], start=1)}
            ap = ap.rearrange(f'p ({names}) -> p {names}', **kw)
        return Buf(self.space, off, nb, ap)

    def mark(self):
        self.marks.append(self.top)

    def release(self):
        self.top = self.marks.pop()


def _esz(dt):
    return 2 if dt == BF16 else 4


def bsub(b, i):
    shp = b.ap.shape
    n = 1
    for s in shp[2:]:
        n *= s
    rb = n * _esz(b.ap.dtype)
    return Buf(b.space, b.off + i * rb, rb, b.ap[:, i])


def bcols(b, lo, hi):
    shp = b.ap.shape
    n = 1
    for s in shp[2:]:
        n *= s
    rb = n * _esz(b.ap.dtype)
    return Buf(b.space, b.off + lo * rb, (hi - lo) * rb, b.ap[:, lo:hi])


D = 1024
SEQ = 2048
NT = 16
HD = 64
DFF = 2816
NEXP = 8
DFE = 1408
NEG = -30000.0


class K:
    def __init__(self, nc, S, A, P, dr, nseq):
        self.nc, self.S, self.A, self.P, self.dr, self.nseq = nc, S, A, P, dr, nseq

    def mm(self, out, lhsT, rhs, start, stop, R, W):
        self.S.op('pe', lambda e: e.matmul(out, lhsT=lhsT, rhs=rhs, start=start, stop=stop,
                                           skip_group_check=True), R, W)

    def tr(self, out, in_, ident, R, W):
        self.S.op('pe', lambda e: e.transpose(out=out, in_=in_, identity=ident), R, W)

    def act(self, out, in_, func, R, W, bias=None, scale=None, accum=None):
        kw = {}
        if bias is not None:
            kw['bias'] = bias
        if scale is not None:
            kw['scale'] = scale
        if accum is not None:
            kw['accum_out'] = accum
        self.S.op('act', lambda e: e.activation(out=out, in_=in_, func=func, **kw), R, W)

    def ts(self, eng, out, in0, s1, s2, op0, op1, R, W):
        if op1 is None:
            self.S.op(eng, lambda e: e.tensor_scalar(out=out, in0=in0, scalar1=s1, scalar2=None, op0=op0), R, W)
        else:
            self.S.op(eng, lambda e: e.tensor_scalar(out=out, in0=in0, scalar1=s1, scalar2=s2, op0=op0, op1=op1), R, W)

    def tt(self, eng, out, in0, in1, op, R, W):
        self.S.op(eng, lambda e: e.tensor_tensor(out=out, in0=in0, in1=in1, op=op), R, W)

    def stt(self, out, in0, scalar, in1, op0, op1, R, W):
        self.S.op('dve', lambda e: e.scalar_tensor_tensor(out=out, in0=in0, scalar=scalar, in1=in1, op0=op0, op1=op1), R, W)

    def cp(self, eng, out, in_, R, W):
        if eng == 'act':
            self.S.op('act', lambda e: e.copy(out=out, in_=in_), R, W)
        else:
            self.S.op(eng, lambda e: e.tensor_copy(out=out, in_=in_), R, W)

    def memset(self, eng, ap, val, W):
        self.S.op(eng, lambda e: e.memset(ap, val), (), W)

    def dma(self, q, out, in_, R, W):
        self.S.dma(q, lambda e: e.dma_start(out=out, in_=in_), R, W)

    def setup(self):
        A, dr = self.A, self.dr
        self.X = A.alloc([NT, D], F32)
        self.identb = A.alloc([128], BF16)
        self.constf = A.alloc([4, 128], F32)
        self.constb = A.alloc([3, 128], BF16)
        self.small = A.alloc([64], F32)
        self.normw = A.alloc([5, 8], F32)
        self.adab = A.alloc([2, 48], F32)
        self.bc8 = A.alloc([3, 8], F32)
        self.dma('sp', self.constf.ap, dr['constf'], (), [self.constf])
        self.dma('sp', self.small.ap, dr['small'], (), [self.small])
        self.dma('sp', self.normw.ap, dr['normw'], (), [self.normw])
        self.dma('sp', self.adab.ap, dr['adab'], (), [self.adab])
        self.dma('sp', self.bc8.ap, dr['bc8'].partition_broadcast(128), (), [self.bc8])
        self.dma('pool', self.constb.ap, dr['constb'], (), [self.constb])
        self.cp('dve', self.identb.ap, self.constf.ap[:, 0], [self.constf], [self.identb])
        self.esink = A.alloc([8], F32)
        self.negone = A.alloc([8], F32, align=64)
        self.memset('pool', self.negone.ap, -1.0, [self.negone])
        self.act(self.esink.ap, self.bc8.ap[:, 1], AF.Exp, [self.bc8], [self.esink])
        self.modT = A.alloc([self.nseq, 2, 6, 8], F32)
        self.gate = A.alloc([1, D], F32)
        self.cT = A.alloc([8, self.nseq], F32)
        self.scb = A.alloc([8, self.nseq], BF16)
        self.Dg = [A.alloc([128], F32) for _ in range(2)]
        self.stat = A.alloc([2, NT], F32)
        self.cosT = None

    def ada(self, l, part):
        A, dr = self.A, self.dr
        ns = self.nseq
        A.mark()
        if l == 0 and part == 0:
            self.dma('sp', self.cT.ap, dr['cT'], (), [self.cT])
            sg = A.alloc([8, ns], F32)
            self.act(sg.ap, self.cT.ap, AF.Silu, [self.cT], [sg])
            self.cp('dve', self.scb.ap, sg.ap, [sg], [self.scb])
        wst = [A.alloc([8, 8, 128], BF16) for _ in range(2)]
        ps1 = self.pj[0]
        for m in range(3 * part, 3 * part + 3):
            w = wst[m % 2]
            self.dma('pool', w.ap, dr['ada_w'][l, m], (), [w])
            for j in range(8):
                for kc in range(8):
                    self.mm(ps1.ap[:, j * ns:(j + 1) * ns], w.ap[:, j, kc], self.scb.ap[:, kc], kc == 0, kc == 7,
                            [w, self.scb], [ps1])
            pv = ps1.ap[:, 0:8 * ns].rearrange('p (j q) -> p q j', q=ns)
            for q in range(ns):
                self.tt('dve', self.modT.ap[:, q, l, m], pv[:, q], self.adab.ap[:, l, m * 8:(m + 1) * 8], ALU.add,
                        [ps1, self.adab], [self.modT])
                if m in (1, 4):
                    nw = self.normw.ap[:, 2 * l + (0 if m == 1 else 1)]
                    self.stt(self.modT.ap[:, q, l, m], self.modT.ap[:, q, l, m], 1.0, nw, ALU.add, ALU.mult,
                             [self.modT, self.normw], [self.modT])
        A.release()

    def expand_gate(self, s, l, m):
        ones = self.constf.ap[:, 2]
        identf = self.constf.ap[:, 0]
        for j in range(8):
            dg = self.Dg[j % 2]
            self.ts('dve', dg.ap, identf, self.modT.ap[:, s, l, m, j:j + 1], None, ALU.mult, None, [self.constf, self.modT], [dg])
            pb = self.pj[j // 4]
            self.mm(pb.ap[:, (j % 4) * 128:(j % 4 + 1) * 128], ones, dg.ap, True, True, [self.constf, dg], [pb])
        for hf in range(2):
            self.cp('act', self.gate.ap[:, 0, hf * 512:(hf + 1) * 512], self.pj[hf].ap, [self.pj[hf]], [self.gate])

    def rstd(self, t0, n, sq):
        X = self.X
        st = self.stat
        for t in range(t0, t0 + n):
            self.act(sq.ap, X.ap[:, t], AF.Square, [bsub(X, t)], [sq, st], accum=st.ap[:, 0, t:t + 1])
        self.ts('dve', st.ap[:, 1, t0:t0 + n], st.ap[:, 0, t0:t0 + n], 1.0 / D, 1e-6, ALU.mult, ALU.add, [st], [st])
        self.act(st.ap[:, 1, t0:t0 + n], st.ap[:, 1, t0:t0 + n], AF.Sqrt, [st], [st])
        self.S.op('dve', lambda e: e.reciprocal(out=st.ap[:, 1, t0:t0 + n], in_=st.ap[:, 1, t0:t0 + n]),
                  self.S._cells([st]), self.S._cells([st]))

    def norm_hT2(self, l, which, hT):
        A, P = self.A, self.P
        A.mark()
        sq = A.alloc([D], F32)
        xn = A.alloc([4, D], BF16)
        pt = self.ptb
        X = self.X
        msh = self.modT.ap[:, self.cur_s, l, 0 if which == 0 else 3]
        msc = self.modT.ap[:, self.cur_s, l, 1 if which == 0 else 4]
        for c in range(4):
            self.rstd(c * 4, 4, sq)
            for tl in range(4):
                t = c * 4 + tl
                self.ts('dve', xn.ap[:, tl], X.ap[:, t], self.stat.ap[:, 1, t:t + 1], None, ALU.mult, None,
                        [bsub(X, t), self.stat], [bsub(xn, tl)])
            for kc in range(8):
                pb = pt[kc % 2]
                for tl in range(4):
                    self.tr(pb.ap[:, tl * 128:(tl + 1) * 128], xn.ap[:, tl, kc * 128:(kc + 1) * 128], self.identb.ap,
                            [bsub(xn, tl), self.identb], [pb])
                if kc % 2 == 0:
                    self.act(hT.ap[:, kc, c * 512:(c + 1) * 512], pb.ap, AF.Identity, [pb, self.modT],
                             [(hT, (kc * SEQ + c * 512) * 2, (kc * SEQ + c * 512 + 512) * 2)],
                             bias=msh[:, kc:kc + 1], scale=msc[:, kc:kc + 1])
                else:
                    self.ts('dve', hT.ap[:, kc, c * 512:(c + 1) * 512], pb.ap, msc[:, kc:kc + 1], msh[:, kc:kc + 1], ALU.mult, ALU.add,
                            [pb, self.modT], [(hT, (kc * SEQ + c * 512) * 2, (kc * SEQ + c * 512 + 512) * 2)])
        A.release()

    def rope_tables(self, s, cosT, sinT):
        A = self.A
        A.mark()
        HS = SEQ // 2
        pi_ = A.alloc([HS], F32)
        ang = A.alloc([HS], F32)
        kf = A.alloc([HS], F32)
        pii = pi_.ap.bitcast(I32)
        sm = self.small
        C1 = 6.28125
        C2 = 2 * math.pi - C1

        def wrap(buf):
            self.ts('dve', kf.ap, buf.ap, math.pi, -2 * math.pi, ALU.is_gt, ALU.mult, [buf], [kf])
            self.tt('dve', buf.ap, buf.ap, kf.ap, ALU.add, [buf, kf], [buf])
            self.ts('dve', kf.ap, buf.ap, -math.pi, 2 * math.pi, ALU.is_lt, ALU.mult, [buf], [kf])
            self.tt('dve', buf.ap, buf.ap, kf.ap, ALU.add, [buf, kf], [buf])

        for hb in range(2):
            sl = slice(hb * HS, (hb + 1) * HS)
            self.dma('sp', pii, self.dr['pos'][s][:, sl].partition_broadcast(128), (), [pi_])
            self.cp('dve', ang.ap, pii, [pi_], [ang])
            self.ts('dve', ang.ap, ang.ap, sm.ap[:, 0:1], None, ALU.mult, None, [ang, sm], [ang])
            self.ts('dve', kf.ap, ang.ap, 1.0 / (2 * math.pi), None, ALU.mult, None, [ang], [kf])
            self.cp('dve', pii, kf.ap, [kf], [pi_])
            self.cp('dve', kf.ap, pii, [pi_], [kf])
            self.stt(ang.ap, kf.ap, -C1, ang.ap, ALU.mult, ALU.add, [kf, ang], [ang])
            self.stt(ang.ap, kf.ap, -C2, ang.ap, ALU.mult, ALU.add, [kf, ang], [ang])
            wrap(ang)
            self.act(sinT.ap[:, sl], ang.ap, AF.Sin, [ang, sm], [sinT], scale=sm.ap[:, 1:2])
            self.ts('dve', ang.ap, ang.ap, math.pi / 2, None, ALU.add, None, [ang], [ang])
            wrap(ang)
            self.act(cosT.ap[:, sl], ang.ap, AF.Sin, [ang], [cosT])
        A.release()

    @staticmethod
    def hT_rng(hT, t0, t1):
        return [(hT, (kc * SEQ + t0) * 2, (kc * SEQ + t1) * 2) for kc in range(8)]

    def proj_fm(self, hT, wsrc, specs, pbanks):
        A = self.A
        A.mark()
        nw = 3 if (A.nbytes - A.top) >= 3 * 4096 + 4 * 2048 + 1024 else 2
        wst = [A.alloc([2, 8, 128], BF16) for _ in range(nw)]
        nb = 2 if (A.nbytes - A.top) >= 4 * 2048 + 1024 else 1
        t1 = [A.alloc([512], F32) for _ in range(nb)]
        t2 = [A.alloc([512], F32) for _ in range(nb)]
        rn = 0
        n = 0

        def issue(si):
            cids = specs[si][1]
            w = wst[si % nw]
            if len(cids) == 2 and cids[1] == cids[0] + 1:
                self.dma('pool', w.ap, wsrc[cids[0]:cids[0] + 2].rearrange('c p k n -> p c k n'), (), [w])
            else:
                for ci, cid in enumerate(cids):
                    self.dma('pool', w.ap[:, ci], wsrc[cid], (), [bsub(w, ci)])

        for k in range(min(nw - 1, len(specs))):
            issue(k)
        for si, (kind, cids, dest, extra) in enumerate(specs):
            w = wst[si % nw]
            if si + nw - 1 < len(specs):
                issue(si + nw - 1)
            for c in range(4):
                pss = []
                for ci in range(len(cids)):
                    pb = pbanks[n % len(pbanks)]
                    n += 1
                    for kc in range(8):
                        self.mm(pb.ap, w.ap[:, ci, kc], hT.ap[:, kc, c * 512:(c + 1) * 512], kc == 0, kc == 7,
                                [bsub(w, ci)] + self.hT_rng(hT, c * 512, (c + 1) * 512), [pb])
                    pss.append(pb)
                dbuf, dap = dest(c)
                if kind == 'plain':
                    self.act(dap, pss[0].ap, AF.Copy, [pss[0]], [dbuf], scale=float(extra))
                else:
                    cosT, sinT = extra
                    a, b = t1[rn % nb], t2[rn % nb]
                    rn += 1
                    self.tt('dve', a.ap, pss[0].ap, cosT.ap[:, c * 512:(c + 1) * 512], ALU.mult, [pss[0], cosT], [a])
                    self.tt('dve', b.ap, pss[1].ap, sinT.ap[:, c * 512:(c + 1) * 512], ALU.mult, [pss[1], sinT], [b])
                    if len(dap.shape) == 3:
                        self.tt('pool', dap, a.ap.rearrange('p (t q) -> p t q', q=128), b.ap.rearrange('p (t q) -> p t q', q=128),
                                ALU.add, [a, b], [dbuf])
                    else:
                        self.tt('pool', dap, a.ap, b.ap, ALU.add, [a, b], [dbuf])
        A.release()

    def proj_tm(self, hT, wsrc, ncols, pbanks, evac):
        A = self.A
        A.mark()
        w = A.alloc([8, ncols], BF16)
        self.dma('pool', w.ap, wsrc, (), [w])
        for t in range(NT):
            pb = pbanks[t % len(pbanks)]
            for kc in range(8):
                self.mm(pb.ap[:, 0:ncols], hT.ap[:, kc, t * 128:(t + 1) * 128], w.ap[:, kc], kc == 0, kc == 7,
                        [w] + self.hT_rng(hT, t * 128, (t + 1) * 128), [pb])
            evac(t, pb)
        A.release()

    def attn_group(self, i, jlist, heads, maskfn, scale, pS, pO, PT, obuf, ocol0, sink=None, osb=None, qbatch=None):
        G = len(heads)
        nJ = len(jlist)

        def scores(jn):
            j = jlist[jn]
            ps = pS[jn % len(pS)]
            m = maskfn(j)
            started = False
            if qbatch is not None:
                out3 = ps.ap[:, 0:G * 128].rearrange('p (g q) -> p g q', q=128)
                self.mm(out3, self.identb.ap, m[1].unsqueeze(1).to_broadcast([128, G, 128]), True, False,
                        [self.identb, m[0]], [ps])
                kb_, ka_ = heads[0]['k']
                self.mm(out3, ka_[:, j * 128:(j + 1) * 128], qbatch[1], False, True, [kb_, qbatch[0]], [ps])
                return
            for hh, h in enumerate(heads):
                cols = ps.ap[:, hh * 128:(hh + 1) * 128]
                if m is not None:
                    self.mm(cols, self.identb.ap, m[1], not started, False, [self.identb, m[0]], [ps])
                    started = True
                qb, qa = h['q']
                kb, ka = h['k']
                self.mm(cols, ka[:, j * 128:(j + 1) * 128], qa[:, i * 128:(i + 1) * 128], not started,
                        h['bias'] is None, [kb, qb], [ps])
                started = True
                if h['bias'] is not None:
                    fkb, fka, fqb, fqa = h['bias']
                    self.mm(cols, fka[:, j * 128:(j + 1) * 128], fqa[:, i * 128:(i + 1) * 128], False, True,
                            [fkb, fqb], [ps])

        def exp_pv(jn):
            j = jlist[jn]
            ps = pS[jn % len(pS)]
            pt = PT[jn % len(PT)]
            self.act(pt.ap[:, 0:G * 128], ps.ap[:, 0:G * 128], AF.Exp, [ps], [pt], scale=float(scale))
            for hh, h in enumerate(heads):
                vb, va = h['v'](j)
                self.mm(pO.ap[:, hh * 65:(hh + 1) * 65], pt.ap[:, hh * 128:(hh + 1) * 128], va[:, 0:65],
                        jn == 0 and hh == 0, jn == nJ - 1, [pt, vb], [pO])

        scores(0)
        for jn in range(nJ):
            if jn + 1 < nJ:
                scores(jn + 1)
            exp_pv(jn)
        if osb is not None:
            ob = osb.ap[:, 0:G * 65]
            self.cp('act', ob, pO.ap[:, 0:G * 65], [pO], [osb])
            o3 = ob.rearrange('p (g d) -> p g d', d=65)
            od = obuf.ap[:, i, ocol0:ocol0 + G * 64].rearrange('p (g d) -> p g d', d=64)
            self.tt('pool', o3[:, :, 64], o3[:, :, 64], self.negone.ap[:, 0:G], ALU.pow, [osb, self.negone], [osb])
            self.tt('pool', od, o3[:, :, 0:64], o3[:, :, 64:65].to_broadcast([128, G, 64]), ALU.mult,
                    [osb], [(obuf, (i * D + ocol0) * 2, (i * D + ocol0 + G * 64) * 2)])
            return
        A = self.A
        A.mark()
        den = A.alloc([G], F32, align=64)
        ov = pO.ap[:, 0:G * 65].rearrange('p (g d) -> p g d', d=65)
        if sink is not None:
            self.tt('dve', den.ap, ov[:, :, 64], sink, ALU.add, [pO, self.esink], [den])
        else:
            self.cp('dve', den.ap, ov[:, :, 64], [pO], [den])
        self.S.op('dve', lambda e: e.reciprocal(out=den.ap, in_=den.ap), self.S._cells([den]), self.S._cells([den]))
        od = obuf.ap[:, i, ocol0:ocol0 + G * 64].rearrange('p (g d) -> p g d', d=64)
        self.tt('dve', od, ov[:, :, 0:64], den.ap.unsqueeze(2).to_broadcast([128, G, 64]), ALU.mult,
                [pO, den], [(obuf, (i * D + ocol0) * 2, (i * D + ocol0 + G * 64) * 2)])
        A.release()

    def fox_attn(self, p, qT, kT, V, FQ, FK, obuf, PT):
        pj, pO = self.pj, self.pO
        tri = self.constb.ap[:, 1]
        A = self.A
        rn = 0
        for hh in range(2):
            qa, ka = qT.ap[64 * hh:64 * hh + 64], kT.ap[64 * hh:64 * hh + 64]
            fqa, fka = FQ.ap[32 * hh:32 * hh + 6], FK.ap[32 * hh:32 * hh + 6]
            for c in range(4):
                po = pO[(2 * hh + c) % 2]
                nJ = 4 * c + 4

                def geom(j):
                    q0 = max(j, 4 * c)
                    off = (q0 - 4 * c) * 128
                    return q0, off, (4 * c + 4 - q0) * 128

                def scores(j, rn):
                    ps = pj[rn % 4]
                    q0, off, ncol = geom(j)
                    cols = ps.ap[:, off:off + ncol]
                    qs = slice(q0 * 128, (4 * c + 4) * 128)
                    self.mm(cols, ka[:, j * 128:(j + 1) * 128], qa[:, qs], True, False, [kT, qT], [ps])
                    diag = j >= 4 * c
                    self.mm(cols, fka[:, j * 128:(j + 1) * 128], fqa[:, qs], False, not diag, [FK, FQ], [ps])
                    if diag:
                        self.mm(ps.ap[:, off:off + 128], self.identb.ap, tri, False, True, [self.identb, self.constb], [ps])

                def exp_pv(j, rn):
                    ps = pj[rn % 4]
                    pt = PT[rn % 2]
                    q0, off, ncol = geom(j)
                    self.act(pt.ap[:, off:off + ncol], ps.ap[:, off:off + ncol], AF.Exp, [ps], [pt])
                    for t in range(q0, 4 * c + 4):
                        tl = t - 4 * c
                        self.mm(po.ap[:, tl * 65:(tl + 1) * 65], pt.ap[:, tl * 128:(tl + 1) * 128], V.ap[:, j, hh, 0:65],
                                j == 0 and tl == 0, j == t, [pt, bsub(V, j)], [po])

                scores(0, rn)
                for j in range(nJ):
                    if j + 1 < nJ:
                        scores(j + 1, rn + j + 1)
                    exp_pv(j, rn + j)
                rn += nJ
                A.mark()
                den = A.alloc([4], F32, align=64)
                ov = po.ap[:, 0:260].rearrange('p (g d) -> p g d', d=65)
                self.cp('dve', den.ap, ov[:, :, 64], [po], [den])
                self.S.op('dve', lambda e, den=den: e.reciprocal(out=den.ap, in_=den.ap), self.S._cells([den]), self.S._cells([den]))
                col0 = 128 * p + 64 * hh
                od = obuf.ap[:, 4 * c:4 * c + 4, col0:col0 + 64]
                self.tt('dve', od, ov[:, :, 0:64], den.ap.unsqueeze(2).to_broadcast([128, 4, 64]), ALU.mult,
                        [po, den], [(obuf, 4 * c * D * 2, (4 * c + 4) * D * 2)])
                A.release()

    def out_proj(self, l, obuf, pbanks, ptb):
        A = self.A
        A.mark()
        w = A.alloc([8, D], BF16)
        self.dma('pool', w.ap, self.dr['wout'][l].rearrange('(kc p) n -> p kc n', p=128), (), [w])
        oT = [A.alloc([8, 128], BF16) for _ in range(2)]
        tmp = [A.alloc([512], F32) for _ in range(2)]
        n = 0
        for t in range(NT):
            o_t = oT[t % 2]
            for k4 in range(2):
                pb = ptb[k4]
                for kk in range(4):
                    kc = 4 * k4 + kk
                    self.tr(pb.ap[:, kk * 128:(kk + 1) * 128], obuf.ap[:, t, kc * 128:(kc + 1) * 128], self.identb.ap,
                            [(obuf, t * D * 2, (t + 1) * D * 2), self.identb], [pb])
                self.cp('act', o_t.ap[:, 4 * k4:4 * k4 + 4].rearrange('p a b -> p (a b)'), pb.ap, [pb],
                        [(o_t, 4 * k4 * 256, (4 * k4 + 4) * 256)])
            for hf in range(2):
                pb = pbanks[n % len(pbanks)]
                tb = tmp[n % 2]
                n += 1
                for kc in range(8):
                    self.mm(pb.ap, o_t.ap[:, kc], w.ap[:, kc, hf * 512:(hf + 1) * 512], kc == 0, kc == 7, [o_t, w], [pb])
                self.tt('dve', tb.ap, pb.ap, self.gate.ap[:, 0, hf * 512:(hf + 1) * 512], ALU.mult, [pb, bsub(self.gate, 0)], [tb])
                xs = (self.X, (t * D + hf * 512) * 4, (t * D + hf * 512 + 512) * 4)
                xa = self.X.ap[:, t, hf * 512:(hf + 1) * 512]
                self.tt('pool', xa, xa, tb.ap, ALU.add, [xs, tb], [xs])
        A.release()

    def ffn_load(self, gsrc, usrc, dsrc, nf, wbuf):
        wg, wu, wd = wbuf
        self.dma('pool', wg.ap[:, 0:nf], gsrc.rearrange('f p k n -> p f k n'), (), [wg])
        self.dma('pool', wu.ap[:, 0:nf], usrc.rearrange('f p k n -> p f k n'), (), [wu])
        self.dma('pool', wd.ap[:, 0:nf], dsrc.rearrange('f p n -> p f n'), (), [wd])

    def ffn_gu(self, hT, nf, wbuf, gT, pbanks, tmp, c):
        wg, wu, wd = wbuf
        for f in range(nf):
            pg = pbanks[self.fn % len(pbanks)]
            pu = pbanks[(self.fn + 1) % len(pbanks)]
            self.fn += 2
            for kc in range(8):
                self.mm(pg.ap, wg.ap[:, f, kc], hT.ap[:, kc, c * 512:(c + 1) * 512], kc == 0, kc == 7,
                        [wg] + self.hT_rng(hT, c * 512, (c + 1) * 512), [pg])
            for kc in range(8):
                self.mm(pu.ap, wu.ap[:, f, kc], hT.ap[:, kc, c * 512:(c + 1) * 512], kc == 0, kc == 7,
                        [wu] + self.hT_rng(hT, c * 512, (c + 1) * 512), [pu])
            sg = tmp[2 + f % 2]
            sgb = sg.ap.bitcast(BF16)[:, 0:512]
            self.act(sgb, pg.ap, AF.Silu, [pg], [sg])
            self.tt('dve', gT.ap[:, f], pu.ap, sgb, ALU.mult, [pu, sg], [bsub(gT, f)])

    def ffn_down(self, nf, wbuf, gT, pbanks, tmp, c, comb):
        wg, wu, wd = wbuf
        for tl in range(4):
            t = c * 4 + tl
            for hf in range(2):
                pb = pbanks[self.fn % len(pbanks)]
                tb = tmp[self.fn % 2]
                self.fn += 1
                for f in range(nf):
                    self.mm(pb.ap, gT.ap[:, f, tl * 128:(tl + 1) * 128], wd.ap[:, f, hf * 512:(hf + 1) * 512],
                            f == 0, f == nf - 1, [gT, wd], [pb])
                gap = self.gate.ap[:, 0, hf * 512:(hf + 1) * 512]
                if comb is None:
                    self.tt('dve', tb.ap, pb.ap, gap, ALU.mult, [pb, bsub(self.gate, 0)], [tb])
                else:
                    cb, cap = comb(t)
                    self.stt(tb.ap, pb.ap, cap, gap, ALU.mult, ALU.mult, [pb, bsub(self.gate, 0), cb], [tb])
                xs = (self.X, (t * D + hf * 512) * 4, (t * D + hf * 512 + 512) * 4)
                xa = self.X.ap[:, t, hf * 512:(hf + 1) * 512]
                self.tt('pool', xa, xa, tb.ap, ALU.add, [xs, tb], [xs])

    def layer0_mixer(self, s):
        A, dr = self.A, self.dr
        pj, pO, ptb = self.pj, self.pO, self.ptb
        sm = self.small
        A.mark()
        hT = A.alloc([8, SEQ], BF16)
        obuf = A.alloc([NT, D], BF16)
        self.norm_hT2(0, 0, hT)
        if self.lvl < 0.45:
            return
        PT = [A.alloc([512], BF16) for _ in range(2)]
        tri = (self.constb, self.constb.ap[:, 1])
        prevb = (self.constb, self.constb.ap[:, 2])
        for p in range(4):
            A.mark()
            qT = A.alloc([SEQ], BF16)
            kT = A.alloc([SEQ], BF16)
            V = A.alloc([NT, 2, 66], BF16)
            FQ = A.alloc([SEQ], BF16)
            FK = A.alloc([SEQ], BF16)
            z = A.alloc([NT, 2], F32)
            self.memset('pool', V.ap, 1.0, [V])
            if self.lvl < 0.455:
                return
            if self.lvl < 0.47:
                return

            def evac(t, pb, V=V, z=z, p=p):
                if self.lvl < 0.49:
                    return
                if self.lvl != 0.494:
                    self.cp('act', V.ap[:, t, :, 0:64], pb.ap[:, 0:128].rearrange('p (h d) -> p h d', d=64), [pb], [bsub(V, t)])
                if self.lvl != 0.492:
                    self.tt('dve', z.ap[:, t], pb.ap[:, 128:130], self.bc8.ap[:, 0, 2 * p:2 * p + 2], ALU.add, [pb, self.bc8], [z])
            self.proj_tm(hT, dr['wtm0f'][p], 130, pj, evac)
            if self.lvl < 0.55:
                return
            A.mark()
            sp = A.alloc([NT, 2], F32)
            Lrep = A.alloc([NT, 64], F32)
            C = A.alloc([SEQ], F32)
            Hb = A.alloc([SEQ], BF16)
            Mb = A.alloc([SEQ], BF16)
            Lb = A.alloc([SEQ], BF16)
            self.act(sp.ap, z.ap, AF.Exp, [z], [sp], scale=-1.0)
            self.act(sp.ap, sp.ap, AF.Ln, [sp], [sp], bias=1.0)
            self.memset('pool', Lrep.ap, 0.0, [Lrep])
            for t in range(NT):
                for sl in range(2):
                    self.ts('dve', Lrep.ap[:, t, 32 * sl:32 * sl + 6], sp.ap[:, t, sl:sl + 1].to_broadcast([128, 6]), -1.0, None,
                            ALU.mult, None, [sp], [bsub(Lrep, t)])
            U = self.constf.ap[:, 1]
            for t in range(NT):
                pb = pj[t % 4]
                self.mm(pb.ap[0:64, 0:128], Lrep.ap[:, t], U, True, True, [bsub(Lrep, t), self.constf], [pb])
                cs = (C, t * 512, (t + 1) * 512)
                if t == 0:
                    self.cp('dve', C.ap[0:64, 0:128], pb.ap[0:64, 0:128], [pb], [cs])
                else:
                    self.ts('dve', C.ap[0:64, t * 128:(t + 1) * 128], pb.ap[0:64, 0:128], C.ap[0:64, t * 128 - 1:t * 128], None,
                            ALU.add, None, [pb, (C, (t - 1) * 512, t * 512)], [cs])
            c64, h64, m64, l64 = C.ap[0:64], Hb.ap[0:64], Mb.ap[0:64], Lb.ap[0:64]
            self.cp('dve', h64, c64, [C], [Hb])
            self.tt('dve', c64, c64, h64, ALU.subtract, [C, Hb], [C])
            self.cp('dve', m64, c64, [C], [Mb])
            self.tt('dve', c64, c64, m64, ALU.subtract, [C, Mb], [C])
            self.cp('dve', l64, c64, [C], [Lb])
            for (dst, c0) in ((FQ, 2), (FK, 6)):
                s64 = sm.ap[0:64]
                f64 = dst.ap[0:64]
                self.ts('dve', f64, h64, s64[:, c0:c0 + 1], s64[:, c0 + 3:c0 + 4], ALU.mult, ALU.add, [Hb, sm], [dst])
                self.stt(f64, m64, s64[:, c0 + 1:c0 + 2], f64, ALU.mult, ALU.add, [Mb, sm, dst], [dst])
                self.stt(f64, l64, s64[:, c0 + 2:c0 + 3], f64, ALU.mult, ALU.add, [Lb, sm, dst], [dst])
            self.proj_fm(hT, dr['wfm0'], [('plain', (p,), lambda c, b=qT: (b, b.ap[:, c * 512:(c + 1) * 512]), 0.125),
                                          ('plain', (4 + p,), lambda c, b=kT: (b, b.ap[:, c * 512:(c + 1) * 512]), 1.0)], pj)
            A.release()
            if self.lvl < 0.65:
                return
            self.fox_attn(p, qT, kT, V, FQ, FK, obuf, PT)
            A.release()
        if self.lvl < 0.75:
            return
        A.mark()
        cosT = A.alloc([SEQ], F32)
        sinT = A.alloc([SEQ], F32)
        self.rope_tables(s, cosT, sinT)
        sqT = A.alloc([NT, 4, 128], BF16)
        skT = A.alloc([SEQ], BF16)
        Vs = A.alloc([NT, 2, 66], BF16)
        self.memset('pool', Vs.ap, 1.0, [Vs])
        specs = [('rope', (8 + 2 * c, 9 + 2 * c), (lambda cc, c=c: (sqT, sqT.ap[:, 4 * cc:4 * cc + 4, c, :])), (cosT, sinT))
                 for c in range(4)]
        specs.append(('rope', (16, 17), (lambda cc: (skT, skT.ap[:, cc * 512:(cc + 1) * 512])), (cosT, sinT)))
        self.proj_fm(hT, dr['wfm0'], specs, pj)

        def evac_s(t, pb):
            self.cp('act', Vs.ap[:, t, :, 0:64], pb.ap[:, 0:128].rearrange('p (h d) -> p h d', d=64), [pb], [bsub(Vs, t)])
        self.proj_tm(hT, dr['wtm0s'], 128, pj, evac_s)
        for g in range(2):
            heads = []
            for c in range(4):
                heads.append(dict(q=None, k=(skT, skT.ap[64 * g:64 * g + 64]),
                                  v=(lambda j, g=g: (bsub(Vs, j), Vs.ap[:, j, g])), bias=None))
            for i in range(NT):
                jl = [i - 1, i] if i > 0 else [i]
                self.attn_group(i, jl, heads, (lambda j, i=i: tri if j == i else prevb), 0.125,
                                pj, pO[i % 2], PT, obuf, 512 + 256 * g, sink=self.esink.ap[:, 4 * g:4 * g + 4],
                                qbatch=(bsub(sqT, i), sqT.ap[64 * g:64 * g + 64, i]))
        A.release()
        if self.lvl < 0.85:
            return
        self.out_proj(0, obuf, pj, ptb)
        A.release()

    def run_units(self, hT, units, wb, gTs, tmp):
        self.fn = 0
        steps = [(ui, c) for ui in range(len(units)) for c in range(4)]
        for k, (ui, c) in enumerate(steps):
            if c == 1 and ui + 1 < len(units):
                g2, u2, d2, nf2, _ = units[ui + 1]
                self.ffn_load(g2, u2, d2, nf2, wb[(ui + 1) % 2])
            self.ffn_gu(hT, units[ui][3], wb[ui % 2], gTs[k % 2], self.pj, tmp, c)
            if k >= 1:
                pu_, pc_ = steps[k - 1]
                self.ffn_down(units[pu_][3], wb[pu_ % 2], gTs[(k - 1) % 2], self.pj, tmp, pc_, units[pu_][4])
        pu_, pc_ = steps[-1]
        self.ffn_down(units[pu_][3], wb[pu_ % 2], gTs[(len(steps) - 1) % 2], self.pj, tmp, pc_, units[pu_][4])

    def alloc_ffn(self, l, first):
        A = self.A
        hT = A.alloc([8, SEQ], BF16)
        wb = [(A.alloc([6, 8, 128], BF16), A.alloc([6, 8, 128], BF16), A.alloc([6, D], BF16)) for _ in range(2)]
        self.ffn_load(first[0], first[1], first[2], first[3], wb[0])
        self.norm_hT2(l, 1, hT)
        gT = [A.alloc([6, 512], BF16) for _ in range(2)]
        tmp = [A.alloc([512], F32) for _ in range(4)]
        return hT, wb, gT, tmp

    def layer0_ffn(self, s):
        A, dr = self.A, self.dr
        A.mark()
        units = []
        f0 = 0
        for nf in (6, 6, 5, 5):
            units.append((dr['ffn_g'][f0:f0 + nf], dr['ffn_u'][f0:f0 + nf], dr['ffn_d'][f0:f0 + nf], nf, None))
            f0 += nf
        hT, wb, gT, tmp = self.alloc_ffn(0, units[0])
        self.run_units(hT, units, wb, gT, tmp)
        A.release()

    def layer1_mixer(self, s):
        A, dr = self.A, self.dr
        pj, pO, ptb = self.pj, self.pO, self.ptb
        A.mark()
        hT = A.alloc([8, SEQ], BF16)
        obuf = Buf(hT.space, hT.off, hT.nbytes,
                   hT.ap.rearrange('p a b -> p (a b)').rearrange('p (t d) -> p t d', d=D))
        A.mark()
        qT = A.alloc([2, NT, 4, 128], BF16)
        kT = A.alloc([2, SEQ], BF16)
        V = A.alloc([NT, 4, 66], BF16)
        iqT = A.alloc([4, SEQ], BF16)
        ikT = A.alloc([SEQ], BF16)
        iw = A.alloc([NT, 8], F32)
        A.mark()
        cosT = A.alloc([SEQ], BF16)
        sinT = A.alloc([SEQ], BF16)
        self.rope_tables(s, cosT, sinT)
        self.norm_hT2(1, 0, hT)
        self.memset('pool', V.ap, 1.0, [V])
        specs = []
        for c in range(8):
            specs.append(('rope', (2 * c, 2 * c + 1), (lambda cc, c=c: (bsub(qT, c // 4), qT.ap[:, c // 4, 4 * cc:4 * cc + 4, c % 4, :])), (cosT, sinT)))
        for c in range(2):
            specs.append(('rope', (16 + 2 * c, 17 + 2 * c), (lambda cc, c=c: (bsub(kT, c), kT.ap[:, c, cc * 512:(cc + 1) * 512])), (cosT, sinT)))
        for c in range(4):
            specs.append(('rope', (20 + 2 * c, 21 + 2 * c), (lambda cc, c=c: (bsub(iqT, c), iqT.ap[:, c, cc * 512:(cc + 1) * 512])), (cosT, sinT)))
        specs.append(('rope', (28, 29), (lambda cc: (ikT, ikT.ap[:, cc * 512:(cc + 1) * 512])), (cosT, sinT)))
        self.proj_fm(hT, dr['wfm1'], specs, pj)

        def evac(t, pb):
            self.cp('act', V.ap[:, t, :, 0:64], pb.ap[:, 0:256].rearrange('p (h d) -> p h d', d=64), [pb], [bsub(V, t)])
            self.cp('dve', iw.ap[:, t], pb.ap[:, 256:264], [pb], [iw])
        self.proj_tm(hT, dr['wtm1'], 264, pj, evac)
        A.release()
        sc = A.alloc([SEQ], F32)
        mb = A.alloc([SEQ], BF16)
        mbT = A.alloc([SEQ], BF16)
        bs = A.alloc([8], F32, align=64)
        TB = 25
        steps = A.alloc([TB + 1], F32, align=64)
        mids = A.alloc([TB + 1], F32, align=64)
        G = A.alloc([TB], F32, align=64)
        cand = A.alloc([TB], F32, align=64)
        rr = [A.alloc([512], F32) for _ in range(2)]
        Dm = A.alloc([8, 128], F32)
        PT = [A.alloc([512], BF16) for _ in range(2)]
        trineg = self.constf.ap[:, 3]
        osb = [A.alloc([260], F32, align=64) for _ in range(1)] * 2
        cnt = [0]

        def sc_of(t):
            if t % 2 == 1:
                nb = (t + 1) * 512
                ap = obuf.ap.rearrange('p t d -> p (t d)')[:, 0:nb // 2].bitcast(F32)
                return Buf(obuf.space, obuf.off, nb, ap)
            return sc

        ptbf = Buf('psum', ptb[1].off, 2048, self.P.base[:, ptb[1].off // 4:ptb[1].off // 4 + 512])
        pS_att = [pj[3], ptbf]
        identf = self.constf.ap[:, 0]
        lb = [pj[0], pj[1]]
        accb = pj[2]

        def indexer(i):
            L = (i + 1) * 128
            scb_ = sc_of(i)
            for h in range(8):
                self.ts('pool', Dm.ap[:, h], identf, iw.ap[:, i, h:h + 1], None, ALU.mult, None, [self.constf, iw], [bsub(Dm, h)])
            for c4 in range((L + 511) // 512):
                nc_ = min(512, L - 512 * c4)
                scs = (scb_, c4 * 2048, c4 * 2048 + nc_ * 4)
                sca = scb_.ap[:, c4 * 512:c4 * 512 + nc_]
                base = cnt[0]

                def logits(hi):
                    ps = lb[(base + hi) % 2]
                    hf = hi % 2
                    self.mm(ps.ap[:, 0:nc_], iqT.ap[64 * hf:64 * hf + 64, hi // 2, i * 128:(i + 1) * 128],
                            ikT.ap[64 * hf:64 * hf + 64, c4 * 512:c4 * 512 + nc_], True, True, [bsub(iqT, hi // 2), ikT], [ps])

                logits(0)
                for hi in range(8):
                    if hi + 1 < 8:
                        logits(hi + 1)
                    ps = lb[(base + hi) % 2]
                    r = rr[(base + hi) % 2]
                    self.act(r.ap[:, 0:nc_], ps.ap[:, 0:nc_], AF.Relu, [ps], [r])
                    self.mm(accb.ap[:, 0:nc_], Dm.ap[:, hi], r.ap[:, 0:nc_], hi == 0, hi == 7, [bsub(Dm, hi), r], [accb])
                cnt[0] += 8
                self.cp('act', sca, accb.ap[:, 0:nc_], [accb], [scs])

        def topk(i):
            L = (i + 1) * 128
            scb_ = sc_of(i)
            dg = (scb_, i * 512, (i + 1) * 512)
            scl = (scb_, 0, L * 4)
            mbl = (mb, 0, L * 2)
            sca_ = scb_.ap[:, 0:L]
            if i >= 2:
                self.S.op('dve', lambda e: e.tensor_reduce(out=bs.ap[:, 0:1], in_=sca_, axis=AX.X, op=ALU.min),
                          self.S._cells([scl]), self.S._cells([bs]))
            self.tt('pool', scb_.ap[:, i * 128:(i + 1) * 128], scb_.ap[:, i * 128:(i + 1) * 128], trineg, ALU.add, [dg, self.constf], [dg])
            if i >= 2:
                self.S.op('dve', lambda e: e.tensor_reduce(out=bs.ap[:, 1:2], in_=sca_, axis=AX.X, op=ALU.max),
                          self.S._cells([scl]), self.S._cells([bs]))
                self.tt('dve', bs.ap[:, 2:3], bs.ap[:, 1:2], bs.ap[:, 0:1], ALU.subtract, [bs], [bs])
                self.ts('dve', steps.ap, self.small.ap[:, 16:16 + TB + 1], bs.ap[:, 2:3], None, ALU.mult, None, [bs, self.small], [steps])
                self.tt('dve', mids.ap[:, 0:1], bs.ap[:, 0:1], steps.ap[:, 0:1], ALU.add, [bs, steps], [mids])
                for t in range(TB):
                    self.S.op('dve', lambda e, t=t: e.tensor_scalar(out=mb.ap[:, 0:L], in0=sca_, scalar1=mids.ap[:, t:t + 1], scalar2=0.0,
                                                                    op0=ALU.is_ge, op1=ALU.add, accum_out=bs.ap[:, 4:5]),
                              self.S._cells([scl, mids]), self.S._cells([mbl, bs]))
                    self.stt(G.ap[:, t:t + 1], bs.ap[:, 4:5], 255.5, steps.ap[:, t:t + 1], ALU.is_ge, ALU.mult, [bs, steps], [G])
                    self.stt(mids.ap[:, t + 1:t + 2], G.ap[:, t:t + 1], steps.ap[:, t + 1:t + 2], mids.ap[:, t:t + 1],
                             ALU.subtract, ALU.add, [G, steps, mids], [mids])
                self.ts('dve', G.ap, G.ap, 0.0, None, ALU.is_gt, None, [G], [G])
                self.tt('dve', cand.ap, mids.ap[:, 0:TB], G.ap, ALU.mult, [mids, G], [cand])
                self.ts('dve', G.ap, G.ap, -1.0, 1.0e30, ALU.add, ALU.mult, [G], [G])
                self.tt('dve', cand.ap, cand.ap, G.ap, ALU.add, [cand, G], [cand])
                self.S.op('dve', lambda e: e.tensor_reduce(out=bs.ap[:, 5:6], in_=cand.ap, axis=AX.X, op=ALU.max),
                          self.S._cells([cand]), self.S._cells([bs]))
                self.tt('dve', bs.ap[:, 0:1], bs.ap[:, 0:1], bs.ap[:, 5:6], ALU.max, [bs], [bs])
                self.ts('dve', mb.ap[:, 0:L], sca_, bs.ap[:, 0:1], NEG, ALU.is_lt, ALU.mult, [scl, bs], [mbl])
            else:
                self.ts('dve', mb.ap[:, 0:L], sca_, -1.0e29, NEG, ALU.is_le, ALU.mult, [scl], [mbl])

        def mask_T(i):
            for j0 in range(0, i + 1, 4):
                pb = ptb[0]
                nj = min(4, i + 1 - j0)
                for j in range(j0, j0 + nj):
                    self.tr(pb.ap[:, (j - j0) * 128:(j - j0 + 1) * 128], mb.ap[:, j * 128:(j + 1) * 128], self.identb.ap,
                            [(mb, j * 256, (j + 1) * 256), self.identb], [pb])
                self.cp('act', mbT.ap[:, j0 * 128:(j0 + nj) * 128], pb.ap[:, 0:nj * 128], [pb], [(mbT, j0 * 256, (j0 + nj) * 256)])

        def attend(i):
            for g in range(4):
                heads = []
                hf = g % 2
                for m in range(4):
                    heads.append(dict(q=None, k=(bsub(kT, g // 2), kT.ap[64 * hf:64 * hf + 64, g // 2]),
                                      v=(lambda j, g=g: (bsub(V, j), V.ap[:, j, g])), bias=None))
                self.attn_group(i, list(range(i + 1)), heads,
                                (lambda j: ((mbT, j * 256, (j + 1) * 256), mbT.ap[:, j * 128:(j + 1) * 128])), 0.125,
                                pS_att, pO[g % 2], PT, obuf, 256 * g, osb=osb[g % 2],
                                qbatch=(bsub(qT, g // 2), qT.ap[64 * hf:64 * hf + 64, g // 2, i]))

        order = list(range(NT - 1, -1, -1))
        indexer(order[0])
        topk(order[0])
        mask_T(order[0])
        indexer(order[1])
        for k in range(NT):
            cur = order[k]
            nxt = order[k + 1] if k + 1 < NT else None
            nn = order[k + 2] if k + 2 < NT else None
            if nn is not None:
                assert sc_of(nn).off != sc_of(nxt).off
                indexer(nn)
            if nxt is not None:
                topk(nxt)
            attend(cur)
            if nxt is not None:
                mask_T(nxt)
        A.release()
        self.out_proj(1, obuf, pj, ptb)
        A.release()

    def layer1_moe(self, s):
        A, dr = self.A, self.dr
        A.mark()
        hT, wb, gT, tmp = self.alloc_ffn(1, (dr['exp_g'][0, 0:6], dr['exp_u'][0, 0:6], dr['exp_d'][0, 0:6], 6))
        lg = A.alloc([NT, 8], F32)
        M8 = A.alloc([NT, 8], F32)
        comb = A.alloc([NT, 8], F32)
        sm4 = A.alloc([4, NT], F32)
        cm2 = A.alloc([NT, 8], F32)

        def evac(t, pb):
            self.tt('dve', lg.ap[:, t], pb.ap[:, 0:8], self.bc8.ap[:, 2], ALU.add, [pb, self.bc8], [lg])
            self.S.op('dve', lambda e, t=t: e.max(out=M8.ap[:, t], in_=lg.ap[:, t]), self.S._cells([lg]), self.S._cells([M8]))
        self.proj_tm(hT, dr['wrt'], 8, self.pj, evac)
        m1, m2 = M8.ap[:, :, 0], M8.ap[:, :, 1]
        d_, e2, g1, g2 = sm4.ap[:, 0], sm4.ap[:, 1], sm4.ap[:, 2], sm4.ap[:, 3]
        self.tt('dve', d_, m2, m1, ALU.subtract, [M8], [sm4])
        self.act(e2, d_, AF.Exp, [sm4], [sm4])
        self.ts('dve', d_, e2, 1.0, None, ALU.add, None, [sm4], [sm4])
        self.S.op('dve', lambda e: e.reciprocal(out=g1, in_=d_), self.S._cells([sm4]), self.S._cells([sm4]))
        self.tt('dve', g2, e2, g1, ALU.mult, [sm4], [sm4])
        self.tt('dve', d_, g1, g2, ALU.subtract, [sm4], [sm4])
        bc = lambda a: a.unsqueeze(2).to_broadcast([128, NT, 8])
        self.tt('dve', comb.ap, lg.ap, bc(m1), ALU.is_ge, [lg, M8], [comb])
        self.tt('dve', comb.ap, comb.ap, bc(d_), ALU.mult, [comb, sm4], [comb])
        self.tt('dve', cm2.ap, lg.ap, bc(m2), ALU.is_ge, [lg, M8], [cm2])
        self.tt('dve', cm2.ap, cm2.ap, bc(g2), ALU.mult, [cm2, sm4], [cm2])
        self.tt('dve', comb.ap, comb.ap, cm2.ap, ALU.add, [comb, cm2], [comb])
        units = []
        for e in range(NEXP):
            for (f0, nf) in ((0, 6), (6, 5)):
                units.append((dr['exp_g'][e, f0:f0 + nf], dr['exp_u'][e, f0:f0 + nf], dr['exp_d'][e, f0:f0 + nf], nf,
                              (lambda t, e=e: (comb, comb.ap[:, t, e:e + 1]))))
        self.run_units(hT, units, wb, gT, tmp)
        A.release()

    def final(self, s):
        A = self.A
        A.mark()
        sq = A.alloc([D], F32)
        yb = [A.alloc([D], F32) for _ in range(2)]
        self.fnw = A.alloc([D], F32)
        self.dma('sp', self.fnw.ap, self.dr['fnw'].partition_broadcast(128), (), [self.fnw])
        X = self.X
        for t in range(NT):
            y = yb[t % 2]
            if t % 4 == 0:
                self.rstd(t, 4, sq)
            self.stt(y.ap, X.ap[:, t], self.stat.ap[:, 1, t:t + 1], self.fnw.ap, ALU.mult, ALU.mult, [bsub(X, t), self.stat, self.fnw], [y])
            self.dma('sp', self.dr['out'][s, t * 128:(t + 1) * 128, :], y.ap, [y], ())
        A.release()

    def run_seq(self, s, stages=99):
        for t in range(NT):
            self.dma('sp', self.X.ap[:, t], self.dr['x'][s, t * 128:(t + 1) * 128, :], (), [bsub(self.X, t)])
        self.lvl = stages
        self.cur_s = s
        if stages >= 0.2:
            if s == 0:
                self.ada(0, 0)
            self.expand_gate(s, 0, 2)
        if stages >= 0.4:
            self.layer0_mixer(s)
        if stages >= 2:
            if s == 0:
                self.ada(0, 1)
            self.expand_gate(s, 0, 5)
            self.layer0_ffn(s)
        if stages >= 3:
            if s == 0:
                self.ada(1, 0)
            self.expand_gate(s, 1, 2)
            self.layer1_mixer(s)
        if stages >= 4:
            if s == 0:
                self.ada(1, 1)
            self.expand_gate(s, 1, 5)
            self.layer1_moe(s)
        if stages >= 5:
            self.final(s)
        else:
            for t in range(NT):
                self.dma('sp', self.dr['out'][s, t * 128:(t + 1) * 128, :], self.X.ap[:, t], [bsub(self.X, t)], ())


SB_BYTES = 207 * 1024


def build_program(nseq, shapes, stages=99):
    nc = bass.Bass("TRN2", target_bir_lowering=False)
    dr = {}
    for name, (shp, dt, kind) in shapes.items():
        dr[name] = nc.dram_tensor(name, list(shp), dt, kind=kind).ap()
    S = Sched(nc)
    with ExitStack() as es:
        sb = es.enter_context(nc.sbuf_tensor("arena", [128, SB_BYTES // 4], F32))
        ps = es.enter_context(nc.psum_tensor("psum", [128, 4096], F32))
        for k in S.sem_keys():
            nm = "s_" + "_".join(str(x) for x in (k if isinstance(k, tuple) else (k,)))
            S.sems[k] = es.enter_context(nc.semaphore(nm))
        A = Arena('sb', sb[:], SB_BYTES)
        P = Arena('psum', ps[:], 16384)
        kb = K(nc, S, A, P, dr, nseq)
        kb.pj = [P.alloc([512], F32, align=2048) for _ in range(4)]
        kb.pO = [P.alloc([512], F32, align=2048) for _ in range(2)]
        kb.ptb = [P.alloc([512], BF16, align=2048) for _ in range(2)]
        kb.setup()
        for s in range(nseq):
            kb.run_seq(s, stages)
        S.wait_all('sp')
        with nc.Block() as block:
            S.emit(block)
    return nc, S


def _chunks(W, col_lists):
    cols = np.concatenate(col_lists)
    g = W[:, cols]
    nch = len(col_lists)
    return np.ascontiguousarray(g.reshape(8, 128, nch, 128).transpose(2, 1, 0, 3))


def _tm(W, cols):
    g = W[:, cols]
    return np.ascontiguousarray(g.reshape(8, 128, len(cols)).transpose(1, 0, 2))


def _rot(cols):
    cols = np.asarray(cols).reshape(-1, 64)
    return np.concatenate([cols[:, 32:], cols[:, :32]], axis=1).reshape(-1)


def host_prepare(inp):
    f = lambda a: np.asarray(a, dtype=np.float32)
    ar = np.arange
    w0 = f(inp['e_w_in'])[0]
    o_fq, o_fk, o_fv, o_fg, o_sq, o_sk, o_sv = 0, 512, 1024, 1536, 1544, 2056, 2184
    cl = []
    for p in range(4):
        cl.append(o_fq + 128 * p + ar(128))
    for p in range(4):
        cl.append(o_fk + 128 * p + ar(128))
    sqc = [np.concatenate([o_sq + 64 * c + ar(64), o_sq + 64 * (4 + c) + ar(64)]) for c in range(4)]
    for c in sqc:
        cl += [c, _rot(c)]
    skc = o_sk + ar(128)
    cl += [skc, _rot(skc)]
    wfm0 = _chunks(w0, cl)
    wtm0f = np.stack([_tm(w0, np.concatenate([o_fv + 128 * p + ar(128), o_fg + 2 * p + ar(2)])) for p in range(4)])
    wtm0s = _tm(w0, o_sv + ar(128))
    w1 = f(inp['o_w_in'])[0]
    o_q, o_k, o_v, o_iq, o_ik, o_iw = 0, 1024, 1280, 1536, 2048, 2112
    LH = [0, 1, 2, 3, 8, 9, 10, 11]
    UH = [4, 5, 6, 7, 12, 13, 14, 15]
    qc = [np.concatenate([o_q + 64 * LH[c] + ar(64), o_q + 64 * UH[c] + ar(64)]) for c in range(8)]
    kc_ = [o_k + 128 * c + ar(128) for c in range(2)]
    iqc = [o_iq + 128 * c + ar(128) for c in range(4)]
    ikc = np.concatenate([o_ik + ar(64), o_ik + ar(64)])
    cl1 = []
    for c in qc + kc_ + iqc + [ikc]:
        cl1 += [c, _rot(c)]
    wfm1 = _chunks(w1, cl1)
    wtm1 = _tm(w1, np.concatenate([o_v + ar(256), o_iw + ar(8)]))
    ada = np.stack([f(inp['e_ada_w'])[0], f(inp['o_ada_w'])[0]])
    ada_w = np.ascontiguousarray(ada.reshape(2, 8, 128, 6, 8, 128).transpose(0, 3, 2, 4, 1, 5))
    ada_b_flat = np.stack([f(inp['e_ada_b'])[0], f(inp['o_ada_b'])[0]])
    adab = np.ascontiguousarray(ada_b_flat.reshape(2, 48, 128).transpose(2, 0, 1))
    nws = [f(inp['e_norm_mix'])[0], f(inp['e_norm_ffn'])[0], f(inp['o_norm_mix'])[0], f(inp['o_norm_ffn'])[0],
           f(inp['final_norm'])]
    normw = np.ascontiguousarray(np.stack(nws).reshape(5, 8, 128).transpose(2, 0, 1))
    bc8 = np.concatenate([f(inp['e_forget_b'])[0], f(inp['e_sinks'])[0], f(inp['o_router_b'])[0]])[None]
    wout = np.stack([f(inp['e_w_out'])[0], f(inp['o_w_out'])[0]])
    fg, fu, fd = f(inp['e_ffn_gate'])[0], f(inp['e_ffn_up'])[0], f(inp['e_ffn_down'])[0]
    ffc = [128 * c + ar(128) for c in range(22)]
    ffn_g = _chunks(fg, ffc)
    ffn_u = _chunks(fu, ffc)
    ffn_d = np.ascontiguousarray(fd.reshape(22, 128, 1024))
    xg, xu, xd = f(inp['o_exp_gate'])[0], f(inp['o_exp_up'])[0], f(inp['o_exp_down'])[0]
    exc = [128 * c + ar(128) for c in range(11)]
    exp_g = np.stack([_chunks(xg[e], exc) for e in range(NEXP)])
    exp_u = np.stack([_chunks(xu[e], exc) for e in range(NEXP)])
    exp_d = np.ascontiguousarray(xd.reshape(NEXP, 11, 128, 1024))
    wrt = _tm(f(inp['o_router_w'])[0], ar(8))
    p = ar(128)
    constf = np.zeros((128, 4, 128), np.float32)
    constf[:, 0] = np.eye(128)
    constf[:, 1] = (p[:, None] <= p[None, :])
    constf[:, 2] = 1.0
    constf[:, 3] = np.where(p[None, :] > p[:, None], -1.0e30, 0.0)
    constb = np.zeros((128, 3, 128), np.float32)
    constb[:, 0] = np.eye(128)
    constb[:, 1] = np.where(p[:, None] > p[None, :], NEG, 0.0)
    constb[:, 2] = np.where(p[:, None] > p[None, :], 0.0, NEG)
    small = np.zeros((128, 64), np.float32)
    half = 32
    inv_freq = (10000.0 ** (-np.arange(half, dtype=np.float32) / half)).astype(np.float32)
    small[:, 0] = inv_freq[p % 32]
    small[:, 1] = np.where((p % 64) < 32, -1.0, 1.0)
    r = p % 32
    small[:, 2] = (r == 0)
    small[:, 3] = (r == 1)
    small[:, 4] = (r == 2)
    small[:, 5] = (r >= 3) & (r < 6)
    small[:, 6] = -1.0 * (r == 3)
    small[:, 7] = -1.0 * (r == 4)
    small[:, 8] = -1.0 * (r == 5)
    small[:, 9] = (r < 3)
    for t in range(40):
        small[:, 16 + t] = 2.0 ** -(t + 1)
    shared = dict(wfm0=wfm0, wtm0f=wtm0f, wtm0s=wtm0s, wfm1=wfm1, wtm1=wtm1, ada_w=ada_w, ada_b_flat=ada_b_flat,
                  adab=adab, normw=normw, bc8=bc8, wout=wout, ffn_g=ffn_g, ffn_u=ffn_u, ffn_d=ffn_d,
                  exp_g=exp_g, exp_u=exp_u, exp_d=exp_d, wrt=wrt, constf=constf, constb=constb, small=small,
                  fnw=f(inp['final_norm'])[None])
    return shared


def per_core_inputs(inp, b0, nseq):
    x = np.ascontiguousarray(np.asarray(inp['x'], np.float32)[b0:b0 + nseq])
    pos = np.ascontiguousarray(np.asarray(inp['positions'], np.int32)[b0:b0 + nseq, None, :])
    c = np.asarray(inp['c'], np.float32)[b0:b0 + nseq]
    cT = np.ascontiguousarray(c.reshape(nseq, 8, 128).transpose(2, 1, 0))
    return dict(x=x, pos=pos, cT=cT)


def make_shapes(shared, pc, nseq):
    shapes = {}
    for k, v in list(shared.items()) + list(pc.items()):
        shapes[k] = (v.shape, I32 if v.dtype == np.int32 else F32, "ExternalInput")
    shapes['out'] = ((nseq, SEQ, D), F32, "ExternalOutput")
    return shapes


_CACHE = {}


def kernel(**inputs):
    ncores, nseq = 8, 2
    shared = host_prepare(inputs)
    maps = []
    for cix in range(ncores):
        pc = per_core_inputs(inputs, cix * nseq, nseq)
        m = dict(shared)
        m.update(pc)
        maps.append(m)
    if 'nc' not in _CACHE:
        _CACHE['nc'] = build_program(nseq, make_shapes(shared, maps[0], nseq))[0]
    res = run_bass_kernel_spmd(_CACHE['nc'], maps, core_ids=list(range(ncores)))
    out = np.concatenate([np.asarray(r['out'], np.float32) for r in res.results], axis=0)
    return out
```
